# Optimizing a Trainium2 kernel written in Bass

```python
import jax
import jax.numpy as jnp
from jax import lax
import numpy as np

D_MODEL = 1024
BATCH = 8
SEQ = 4096
DEPTH = 4

GRID_W = 64
CTX_LEN = 256
Q_BLOCK = 128
ROPE_THETA = 10000.0
NORM_EPS = 1e-6
N_MOD = 6

MLA_HEADS = 4
MLA_Q_RANK = 384
MLA_KV_RANK = 256
MLA_NOPE = 128
MLA_ROPE = 64
MLA_V = 128
MLA_SCALE = (MLA_NOPE + MLA_ROPE) ** -0.5
GQA_HEADS = 4
GQA_KV_HEADS = 2
GQA_GROUP = GQA_HEADS // GQA_KV_HEADS
GQA_HEAD_DIM = 128
GQA_SCALE = GQA_HEAD_DIM ** -0.5
_S1 = MLA_Q_RANK
_S2 = _S1 + MLA_KV_RANK
_S3 = _S2 + MLA_ROPE
_S4 = _S3 + GQA_HEADS * GQA_HEAD_DIM
_S5 = _S4 + GQA_KV_HEADS * GQA_HEAD_DIM
ATTN_SPLITS = (_S1, _S2, _S3, _S4, _S5)
ATTN_IN = _S5 + GQA_KV_HEADS * GQA_HEAD_DIM
ATTN_OUT = MLA_HEADS * MLA_V + GQA_HEADS * GQA_HEAD_DIM

RWKV_HEAD = 64
RWKV_HEADS = D_MODEL // RWKV_HEAD
DECAY_LORA = 64
ICLR_LORA = 64
VRES_LORA = 32
GATE_LORA = 128
RWKV_GN_EPS = 64e-5
N_MIX = 6

N_EXPERTS = 32
TOP_K = 4
EXPERT_FF = D_MODEL
SWIGLU_ALPHA = 1.702
SWIGLU_LIMIT = 7.0

N_EVEN = (DEPTH + 1) // 2
N_ODD = DEPTH // 2
N_VRES = max(N_ODD - 1, 0)

kernel_name = 'hybrid_mla_gqa_rwkv7_moe_dit_trunk'


def rmsnorm(x, g):
    xf = x.astype(jnp.float32)
    y = xf * lax.rsqrt(jnp.mean(xf * xf, axis=-1, keepdims=True) + NORM_EPS)
    return (y * g.astype(jnp.float32)).astype(x.dtype)


def modulate(h, shift, scale):
    return h * (1 + scale) + shift


def axial_angles(n_tokens, rot_dim):
    rows = n_tokens // GRID_W
    row = jnp.repeat(jnp.arange(rows, dtype=jnp.float32), GRID_W)
    col = jnp.tile(jnp.arange(GRID_W, dtype=jnp.float32), rows)
    half = rot_dim // 2
    inv_freq = ROPE_THETA ** (-jnp.arange(0, half, 2, dtype=jnp.float32) / half)
    return row[:, None] * inv_freq[None, :], col[:, None] * inv_freq[None, :]


def _rotate(x, ang):
    cos = jnp.cos(ang)[None, :, None, :].astype(x.dtype)
    sin = jnp.sin(ang)[None, :, None, :].astype(x.dtype)
    x1, x2 = jnp.split(x, 2, axis=-1)
    return jnp.concatenate([x1 * cos - x2 * sin, x2 * cos + x1 * sin], axis=-1)


def axial_rope(x, angles):
    if angles is None:
        return x
    x_row, x_col = jnp.split(x, 2, axis=-1)
    return jnp.concatenate([_rotate(x_row, angles[0]), _rotate(x_col, angles[1])], axis=-1)


def softmax_attend(q, k, v, scale):
    s = jnp.einsum('bqhgd,bkhd->bhgqk', q, k).astype(jnp.float32) * scale
    p = jax.nn.softmax(s, axis=-1).astype(v.dtype)
    return jnp.einsum('bhgqk,bkhd->bqhgd', p, v)


def latent_attention(q, k, v, k_ctx, v_ctx, scale):
    B, T = q.shape[:2]
    k_all = jnp.concatenate([k_ctx, k], axis=1)
    v_all = jnp.concatenate([v_ctx, v], axis=1)
    qb = jnp.swapaxes(q.reshape(B, T // Q_BLOCK, Q_BLOCK, *q.shape[2:]), 0, 1)
    ob = lax.map(lambda q_blk: softmax_attend(q_blk, k_all, v_all, scale), qb)
    return jnp.swapaxes(ob, 0, 1).reshape(B, T, *ob.shape[3:])


def attn_mixer(h_lat, h_ctx, p, ang_mla, ang_gqa, with_ctx):
    def project(h, a_mla, a_gqa):
        B, T, _ = h.shape
        zq, zkv, zr, gq, gk, gv = jnp.split(h @ p['w_in'], ATTN_SPLITS, axis=-1)
        q = (rmsnorm(zq, p['q_norm']) @ p['w_uq']).reshape(B, T, MLA_HEADS, MLA_NOPE + MLA_ROPE)
        kv = (rmsnorm(zkv, p['kv_norm']) @ p['w_ukv']).reshape(B, T, MLA_HEADS, MLA_NOPE + MLA_V)
        q_nope, q_rope = jnp.split(q, [MLA_NOPE], axis=-1)
        k_nope, v_mla = jnp.split(kv, [MLA_NOPE], axis=-1)
        q_mla = jnp.concatenate([rmsnorm(q_nope, p['qn_g']), axial_rope(rmsnorm(q_rope, p['qr_g']), a_mla)], axis=-1)
        k_rope = axial_rope(rmsnorm(zr, p['kr_g'])[:, :, None, :], a_mla)
        k_mla = jnp.concatenate([rmsnorm(k_nope, p['kn_g']), jnp.broadcast_to(k_rope, (B, T, MLA_HEADS, MLA_ROPE))], axis=-1)
        q_gqa = axial_rope(rmsnorm(gq.reshape(B, T, GQA_HEADS, GQA_HEAD_DIM), p['gq_g']), a_gqa)
        k_gqa = axial_rope(rmsnorm(gk.reshape(B, T, GQA_KV_HEADS, GQA_HEAD_DIM), p['gk_g']), a_gqa)
        v_gqa = gv.reshape(B, T, GQA_KV_HEADS, GQA_HEAD_DIM)
        return ((q_mla[:, :, :, None, :], k_mla, v_mla),
                (q_gqa.reshape(B, T, GQA_KV_HEADS, GQA_GROUP, GQA_HEAD_DIM), k_gqa, v_gqa))

    def merge(o_mla, o_gqa):
        B, T = o_mla.shape[:2]
        return jnp.concatenate([o_mla.reshape(B, T, -1), o_gqa.reshape(B, T, -1)], axis=-1) @ p['w_out']

    (mq, mk, mv), (gq, gk, gv) = project(h_lat, ang_mla, ang_gqa)
    (cmq, cmk, cmv), (cgq, cgk, cgv) = project(h_ctx, None, None)
    o_lat = merge(latent_attention(mq, mk, mv, cmk, cmv, MLA_SCALE),
                  latent_attention(gq, gk, gv, cgk, cgv, GQA_SCALE))
    o_ctx = None
    if with_ctx:
        o_ctx = merge(softmax_attend(cmq, cmk, cmv, MLA_SCALE), softmax_attend(cgq, cgk, cgv, GQA_SCALE))
    return o_lat, o_ctx


def centred_shift(h):
    prev = jnp.pad(h[:, :-1], ((0, 0), (1, 0), (0, 0)))
    nxt = jnp.pad(h[:, 1:], ((0, 0), (0, 1), (0, 0)))
    return 0.5 * (prev + nxt)


def rwkv_features(h, p, v_first):
    B, T, _ = h.shape
    heads = lambda t: t.reshape(B, T, RWKV_HEADS, RWKV_HEAD)
    xx = centred_shift(h) - h
    xr, xw, xk, xv, xa, xg = (h + xx * p['mix'][m] for m in range(N_MIX))
    k = xk @ p['w_k']
    v = xv @ p['w_v']
    v_raw = v
    if v_first is not None:
        v = v + (v_first - v) * jax.nn.sigmoid(p['v0'] + (xv @ p['v1']) @ p['v2'])
    kk = heads(k * p['k_k']).astype(jnp.float32)
    kk = kk * lax.rsqrt(jnp.maximum(jnp.sum(kk * kk, axis=-1, keepdims=True), 1e-24))
    decay, k_dir, b_dir = [], [], []
    for d in range(2):
        z = (p['w0'][d] + jnp.tanh(xw @ p['w1'][d]) @ p['w2'][d]).astype(jnp.float32)
        decay.append(heads(jnp.exp(-jnp.exp(-jax.nn.softplus(-z) - 0.5))))
        a = jax.nn.sigmoid(p['a0'][d] + (xa @ p['a1'][d]) @ p['a2'][d])
        k_dir.append(heads(k * (1 + (a - 1) * p['k_a'])))
        b_dir.append(kk * heads(a).astype(jnp.float32))
    return dict(r=heads(xr @ p['w_r']), v=heads(v), kk=kk, xg=xg, decay=decay, k=k_dir, b=b_dir, v_raw=v_raw)


def wkv7_scan(state0, f, d, reverse):
    xs = tuple(jnp.swapaxes(t.astype(jnp.float32), 0, 1)
               for t in (f['r'], f['decay'][d], f['k'][d], f['v'], f['kk'], f['b'][d]))

    def step(S, inp):
        r, w, k, v, kk, b = inp
        sa = jnp.einsum('bhvk,bhk->bhv', S, -kk)
        S = S * w[:, :, None, :] + sa[..., None] * b[:, :, None, :] + v[..., None] * k[:, :, None, :]
        return S, jnp.einsum('bhvk,bhk->bhv', S, r)

    state, y = lax.scan(step, state0, xs, reverse=reverse)
    return state, jnp.swapaxes(y, 0, 1)


def rwkv_output(y, f, p):
    B, T, H, N = y.shape
    mu = jnp.mean(y, axis=-1, keepdims=True)
    var = jnp.mean(jnp.square(y - mu), axis=-1, keepdims=True)
    yn = (y - mu) * lax.rsqrt(var + RWKV_GN_EPS)
    yn = (yn * p['ln_w'].reshape(H, N) + p['ln_b'].reshape(H, N)).astype(f['v'].dtype)
    bonus = jnp.sum(f['r'] * (f['k'][0] + f['k'][1]) * p['r_k'], axis=-1, keepdims=True) * f['v']
    g = jax.nn.sigmoid(f['xg'] @ p['g1']) @ p['g2']
    return ((yn + bonus).reshape(B, T, H * N) * g) @ p['w_o']


def rwkv_mixer(h_lat, h_ctx, p, vf_lat, vf_ctx, with_ctx):
    f_lat = rwkv_features(h_lat, p, vf_lat)
    f_ctx = rwkv_features(h_ctx, p, vf_ctx)
    state0 = jnp.zeros((h_lat.shape[0], RWKV_HEADS, RWKV_HEAD, RWKV_HEAD), jnp.float32)
    y_lat, y_ctx = 0.0, 0.0
    for d, rev in enumerate((False, True)):
        s_ctx, yc = wkv7_scan(state0, f_ctx, d, rev)
        _, yl = wkv7_scan(s_ctx, f_lat, d, rev)
        y_lat = y_lat + yl
        y_ctx = y_ctx + yc
    o_lat = rwkv_output(y_lat, f_lat, p)
    o_ctx = rwkv_output(y_ctx, f_ctx, p) if with_ctx else None
    return o_lat, o_ctx, f_lat['v_raw'], f_ctx['v_raw']


def clamped_swiglu(u):
    u_glu, u_lin = u[..., ::2], u[..., 1::2]
    u_glu = jnp.minimum(u_glu, SWIGLU_LIMIT)
    u_lin = jnp.clip(u_lin, -SWIGLU_LIMIT, SWIGLU_LIMIT)
    return u_glu * jax.nn.sigmoid(SWIGLU_ALPHA * u_glu) * (u_lin + 1)


def moe(h, router_w, router_b, w1, b1, w2, b2):
    logits = (h @ router_w + router_b).astype(jnp.float32)
    top_val, top_idx = lax.top_k(logits, TOP_K)
    top_p = jax.nn.softmax(top_val, axis=-1)
    gates = jnp.einsum('nk,nke->ne', top_p, jax.nn.one_hot(top_idx, N_EXPERTS, dtype=jnp.float32)).astype(h.dtype)
    out = jnp.zeros_like(h)
    for e in range(N_EXPERTS):
        y = clamped_swiglu(h @ w1[e] + b1[e]) @ w2[e] + b2[e]
        out = out + gates[:, e:e + 1] * y
    return out


def setup_inputs(seed: int = 0) -> dict:
    key = jax.random.key(seed)
    ks = iter(jax.random.split(key, 64))

    def nrm(shape, scale):
        return jax.random.normal(next(ks), shape, jnp.float32) * scale

    def gain(shape):
        return 1.0 + nrm(shape, 0.02)

    def unif(shape, lo, hi):
        return jax.random.uniform(next(ks), shape, jnp.float32, lo, hi)

    D, E, F = D_MODEL, N_EXPERTS, EXPERT_FF
    H, N = RWKV_HEADS, RWKV_HEAD
    return {
        'x': nrm((BATCH, SEQ, D), 1.0),
        'c': nrm((BATCH, D), 1.0),
        'ctx': nrm((BATCH, CTX_LEN, D), 1.0),
        'c_ctx': nrm((D,), 1.0),
        'ada_w': nrm((DEPTH, D, N_MOD * D), 0.5 * D ** -0.5),
        'ada_b': nrm((DEPTH, N_MOD * D), 0.02),
        'norm_mix': gain((DEPTH, D)),
        'norm_ffn': gain((DEPTH, D)),
        'attn_w_in': nrm((N_EVEN, D, ATTN_IN), D ** -0.5),
        'mla_q_norm': gain((N_EVEN, MLA_Q_RANK)),
        'mla_w_uq': nrm((N_EVEN, MLA_Q_RANK, MLA_HEADS * (MLA_NOPE + MLA_ROPE)), MLA_Q_RANK ** -0.5),
        'mla_kv_norm': gain((N_EVEN, MLA_KV_RANK)),
        'mla_w_ukv': nrm((N_EVEN, MLA_KV_RANK, MLA_HEADS * (MLA_NOPE + MLA_V)), MLA_KV_RANK ** -0.5),
        'mla_qn_g': gain((N_EVEN, MLA_NOPE)),
        'mla_qr_g': gain((N_EVEN, MLA_ROPE)),
        'mla_kn_g': gain((N_EVEN, MLA_NOPE)),
        'mla_kr_g': gain((N_EVEN, MLA_ROPE)),
        'gqa_q_g': gain((N_EVEN, GQA_HEAD_DIM)),
        'gqa_k_g': gain((N_EVEN, GQA_HEAD_DIM)),
        'attn_w_out': nrm((N_EVEN, ATTN_OUT, D), ATTN_OUT ** -0.5),
        'rwkv_mix': unif((N_ODD, N_MIX, D), 0.0, 1.0),
        'rwkv_w_r': nrm((N_ODD, D, D), D ** -0.5),
        'rwkv_w_k': nrm((N_ODD, D, D), D ** -0.5),
        'rwkv_w_v': nrm((N_ODD, D, D), D ** -0.5),
        'rwkv_w_o': nrm((N_ODD, D, D), D ** -0.5),
        'rwkv_w0': unif((N_ODD, 2, D), -5.0, -1.0),
        'rwkv_w1': nrm((N_ODD, 2, D, DECAY_LORA), D ** -0.5),
        'rwkv_w2': nrm((N_ODD, 2, DECAY_LORA, D), 0.5 * DECAY_LORA ** -0.5),
        'rwkv_a0': nrm((N_ODD, 2, D), 0.1),
        'rwkv_a1': nrm((N_ODD, 2, D, ICLR_LORA), D ** -0.5),
        'rwkv_a2': nrm((N_ODD, 2, ICLR_LORA, D), 0.5 * ICLR_LORA ** -0.5),
        'rwkv_v0': nrm((N_VRES, D), 0.1),
        'rwkv_v1': nrm((N_VRES, D, VRES_LORA), D ** -0.5),
        'rwkv_v2': nrm((N_VRES, VRES_LORA, D), 0.5 * VRES_LORA ** -0.5),
        'rwkv_g1': nrm((N_ODD, D, GATE_LORA), D ** -0.5),
        'rwkv_g2': nrm((N_ODD, GATE_LORA, D), GATE_LORA ** -0.5),
        'rwkv_k_k': 0.85 + nrm((N_ODD, D), 0.02),
        'rwkv_k_a': gain((N_ODD, D)),
        'rwkv_r_k': nrm((N_ODD, H, N), 0.1),
        'rwkv_ln_w': gain((N_ODD, D)),
        'rwkv_ln_b': nrm((N_ODD, D), 0.02),
        'moe_router_w': nrm((DEPTH, D, E), D ** -0.5),
        'moe_router_b': nrm((DEPTH, E), 0.01),
        'moe_w1': nrm((DEPTH, E, D, 2 * F), D ** -0.5),
        'moe_b1': nrm((DEPTH, E, 2 * F), 0.02),
        'moe_w2': nrm((DEPTH, E, F, D), F ** -0.5),
        'moe_b2': nrm((DEPTH, E, D), 0.02),
    }


def reference(x, c, ctx, c_ctx, ada_w, ada_b, norm_mix, norm_ffn,
              attn_w_in, mla_q_norm, mla_w_uq, mla_kv_norm, mla_w_ukv, mla_qn_g, mla_qr_g, mla_kn_g, mla_kr_g,
              gqa_q_g, gqa_k_g, attn_w_out,
              rwkv_mix, rwkv_w_r, rwkv_w_k, rwkv_w_v, rwkv_w_o, rwkv_w0, rwkv_w1, rwkv_w2,
              rwkv_a0, rwkv_a1, rwkv_a2, rwkv_v0, rwkv_v1, rwkv_v2, rwkv_g1, rwkv_g2,
              rwkv_k_k, rwkv_k_a, rwkv_r_k, rwkv_ln_w, rwkv_ln_b,
              moe_router_w, moe_router_b, moe_w1, moe_b1, moe_w2, moe_b2):
    n_lat = x.shape[1]
    ang_mla = axial_angles(n_lat, MLA_ROPE)
    ang_gqa = axial_angles(n_lat, GQA_HEAD_DIM)
    cond_lat = jax.nn.silu(c)[:, None, :]
    cond_ctx = jax.nn.silu(c_ctx)[None, None, :]
    vf_lat, vf_ctx = None, None
    for i in range(DEPTH):
        with_ctx = i < DEPTH - 1
        m_lat = jnp.split(cond_lat @ ada_w[i] + ada_b[i], N_MOD, axis=-1)
        m_ctx = jnp.split(cond_ctx @ ada_w[i] + ada_b[i], N_MOD, axis=-1)
        h_lat = modulate(rmsnorm(x, norm_mix[i]), m_lat[0], m_lat[1])
        h_ctx = modulate(rmsnorm(ctx, norm_mix[i]), m_ctx[0], m_ctx[1])
        if i % 2 == 0:
            e = i // 2
            p = dict(w_in=attn_w_in[e], q_norm=mla_q_norm[e], w_uq=mla_w_uq[e], kv_norm=mla_kv_norm[e],
                     w_ukv=mla_w_ukv[e], qn_g=mla_qn_g[e], qr_g=mla_qr_g[e], kn_g=mla_kn_g[e], kr_g=mla_kr_g[e],
                     gq_g=gqa_q_g[e], gk_g=gqa_k_g[e], w_out=attn_w_out[e])
            o_lat, o_ctx = attn_mixer(h_lat, h_ctx, p, ang_mla, ang_gqa, with_ctx)
        else:
            j = i // 2
            p = dict(mix=rwkv_mix[j], w_r=rwkv_w_r[j], w_k=rwkv_w_k[j], w_v=rwkv_w_v[j], w_o=rwkv_w_o[j],
                     w0=rwkv_w0[j], w1=rwkv_w1[j], w2=rwkv_w2[j], a0=rwkv_a0[j], a1=rwkv_a1[j], a2=rwkv_a2[j],
                     g1=rwkv_g1[j], g2=rwkv_g2[j], k_k=rwkv_k_k[j], k_a=rwkv_k_a[j], r_k=rwkv_r_k[j],
                     ln_w=rwkv_ln_w[j], ln_b=rwkv_ln_b[j])
            if j > 0:
                p.update(v0=rwkv_v0[j - 1], v1=rwkv_v1[j - 1], v2=rwkv_v2[j - 1])
            o_lat, o_ctx, v_lat, v_ctx = rwkv_mixer(h_lat, h_ctx, p, vf_lat, vf_ctx, with_ctx)
            if j == 0:
                vf_lat, vf_ctx = v_lat, v_ctx
        x = x + m_lat[2] * o_lat
        ffn_lat = modulate(rmsnorm(x, norm_ffn[i]), m_lat[3], m_lat[4])
        moe_args = (moe_router_w[i], moe_router_b[i], moe_w1[i], moe_b1[i], moe_w2[i], moe_b2[i])
        if with_ctx:
            ctx = ctx + m_ctx[2] * o_ctx
            ffn_ctx = modulate(rmsnorm(ctx, norm_ffn[i]), m_ctx[3], m_ctx[4])
            n_tok = ffn_lat.shape[0] * ffn_lat.shape[1]
            tokens = jnp.concatenate([ffn_lat.reshape(-1, D_MODEL), ffn_ctx.reshape(-1, D_MODEL)], axis=0)
            out = moe(tokens, *moe_args)
            x = x + m_lat[5] * out[:n_tok].reshape(x.shape)
            ctx = ctx + m_ctx[5] * out[n_tok:].reshape(ctx.shape)
        else:
            x = x + m_lat[5] * moe(ffn_lat.reshape(-1, D_MODEL), *moe_args).reshape(x.shape)
    return x
```

```python
import numpy as np
from contextlib import ExitStack
import concourse.bass as bass
import concourse.mybir as mybir
from concourse.bass_utils import run_bass_kernel_spmd

F32 = mybir.dt.float32
BF16 = mybir.dt.bfloat16
AF = mybir.ActivationFunctionType
ALU = mybir.AluOpType
AX = mybir.AxisListType

D = 1024
NCH = 8
EPOCH = 12000


class Buf:
    __slots__ = ("name", "ap", "w", "r", "excl")

    def __init__(self, name, ap=None, excl=False):
        self.name = name
        self.ap = ap
        self.w = None
        self.r = []
        self.excl = excl

    def __getitem__(self, idx):
        return self.ap[idx]


class Sched:
    CE = ("pe", "act", "dve", "pool")

    def __init__(self, nc, n_dma_slots=8):
        self.nc = nc
        self.eng = {"pe": nc.tensor, "act": nc.scalar, "dve": nc.vector,
                    "pool": nc.gpsimd, "sp": nc.sync}
        self.sem, self.semcnt, self.gidx, self.nep = {}, {}, {}, {}
        for e in self.CE:
            self.sem[e] = nc.alloc_semaphore(f"s_{e}_0")
            self.semcnt[e] = 0
            self.gidx[e] = 0
            self.nep[e] = 0
        self.seen = {e: {} for e in self.eng}
        self.slots = {}
        for q in ("sp", "pool"):
            self.slots[q] = [[nc.alloc_semaphore(f"d_{q}_{i}"), 0, None] for i in range(n_dma_slots)]
        self.slot_i = {q: 0 for q in self.slots}
        self.dma_gid = 0
        self.last_tok = {e: None for e in self.CE}
        self.pending = {e: False for e in self.CE}
        self.ninstr = {e: 0 for e in self.eng}

    def _next_tok(self, e):
        return ("c", e, self.gidx[e] + 1, self.sem[e], self.semcnt[e] + 1)

    def _wait(self, e, tok):
        if tok is None:
            return
        kind, x, g, sem, val = tok
        if kind == "c":
            if self.seen[e].get(x, 0) >= g:
                return
            self.seen[e][x] = g
        else:
            if self.seen[e].get(("d", g)):
                return
            self.seen[e][("d", g)] = True
        self.eng[e].wait_ge(sem, val)
        self.ninstr[e] += 1

    def _deps(self, e, reads, writes):
        for b in reads:
            self._wait(e, b.w)
            if b.excl:
                for t in b.r:
                    if not (t[0] == "c" and t[1] == e):
                        self._wait(e, t)
        for b in writes:
            if b.w is not None and not (b.w[0] == "c" and b.w[1] == e):
                self._wait(e, b.w)
            for t in b.r:
                if t[0] == "c" and t[1] == e:
                    continue
                self._wait(e, t)

    def op(self, e, fn, reads=(), writes=(), inc=True):
        self._deps(e, reads, writes)
        tok = self._next_tok(e)
        ins = fn(self.eng[e])
        self.ninstr[e] += 1
        if inc:
            ins.then_inc(self.sem[e], 1)
            self.gidx[e] += 1
            self.semcnt[e] += 1
            self.last_tok[e] = tok
            self.pending[e] = False
        else:
            self.pending[e] = True
        for b in reads:
            b.r = [t for t in b.r if not (t[0] == "c" and t[1] == e)]
            b.r.append(tok)
        for b in writes:
            b.w = tok
            b.r = []
        if inc and self.semcnt[e] >= EPOCH:
            self.nep[e] += 1
            self.sem[e] = self.nc.alloc_semaphore(f"s_{e}_{self.nep[e]}")
            self.semcnt[e] = 0
        return ins

    def dma(self, q, out, in_, reads=(), writes=(), **kw):
        e = q
        self._deps(e, reads, writes)
        sl = self.slots[q][self.slot_i[q]]
        self.slot_i[q] = (self.slot_i[q] + 1) % len(self.slots[q])
        if sl[2] is not None:
            self._wait(e, sl[2])
        ins = self.eng[e].dma_start(out=out, in_=in_, **kw)
        self.ninstr[e] += 1
        sl[1] += 16
        ins.then_inc(sl[0], 16)
        self.dma_gid += 1
        tok = ("d", q, self.dma_gid, sl[0], sl[1])
        sl[2] = tok
        for b in reads:
            b.r.append(tok)
        for b in writes:
            b.w = tok
            b.r = []
        return tok

    def idma(self, out, out_offset, in_, in_offset, reads=(), writes=(), nrows=None):
        e = "pool"
        self._deps(e, reads, writes)
        sl = self.slots[e][self.slot_i[e]]
        self.slot_i[e] = (self.slot_i[e] + 1) % len(self.slots[e])
        if sl[2] is not None:
            self._wait(e, sl[2])
        kw = {}
        if nrows is not None:
            if not hasattr(self, "_bregs"):
                self._bregs = {}
            if nrows not in self._bregs:
                self._bregs[nrows] = self.eng[e].to_reg(nrows - 1)
            kw = dict(bounds_check=self._bregs[nrows], oob_is_err=False)
        ins = self.eng[e].indirect_dma_start(out=out, out_offset=out_offset, in_=in_, in_offset=in_offset, **kw)
        self.ninstr[e] += 1
        sl[1] += 16
        ins.then_inc(sl[0], 16)
        self.dma_gid += 1
        tok = ("d", e, self.dma_gid, sl[0], sl[1])
        sl[2] = tok
        for b in reads:
            b.r.append(tok)
        for b in writes:
            b.w = tok
            b.r = []
        return tok

    def barrier(self):
        toks = []
        for e in self.CE:
            assert not self.pending[e], f"engine {e} has un-inc'd trailing instructions"
            if self.last_tok[e] is not None:
                toks.append(self.last_tok[e])
        for q in self.slots:
            for sl in self.slots[q]:
                if sl[2] is not None:
                    toks.append(sl[2])
        for e in self.eng:
            for t in toks:
                if t[0] == "c" and t[1] == e:
                    continue
                self._wait(e, t)


class Rot:
    def __init__(self, P, stack, name, shape, dtype, n):
        self.bufs = []
        for i in range(n):
            t = stack.enter_context(P.nc.sbuf_tensor(f"{name}{i}_{P.uid()}", list(shape), dtype))
            self.bufs.append(Buf(f"{name}{i}", t))
        self.i = 0

    def next(self):
        b = self.bufs[self.i]
        self.i = (self.i + 1) % len(self.bufs)
        return b


class Cfg:
    def __init__(self, SEQ=4096, CTX=256, NE=32, DEPTH=4, TOPK=4, GRID_W=64):
        self.SEQ, self.CTX, self.NE, self.DEPTH, self.TOPK, self.GRID_W = SEQ, CTX, NE, DEPTH, TOPK, GRID_W
        self.N = SEQ + CTX
        assert CTX % 128 == 0 and CTX <= 512 and SEQ % 512 == 0
        self.NT = self.N // 128
        self.blocks = [(0, CTX, 1)] + [(CTX + i * 512, 512, 0) for i in range(SEQ // 512)]
        self.N_EVEN = (DEPTH + 1) // 2
        self.N_ODD = DEPTH // 2


class Prog:
    def __init__(self, cfg, debug=()):
        self.cfg = cfg
        self.debug = set(debug)
        self.nc = bass.Bass("TRN2", target_bir_lowering=False)
        self.s = Sched(self.nc)
        self._uid = 0
        self.inputs = {}
        self.root = ExitStack()
        nc = self.nc
        self.ps = [Buf(f"ps{i}", nc.alloc_psum_tensor(f"ps{i}", [128, 512], F32).ap(), excl=True) for i in range(8)]
        self.ps_i = 0
        self.ps_held = []

    def uid(self):
        self._uid += 1
        return self._uid

    def psum(self):
        while True:
            b = self.ps[self.ps_i]
            self.ps_i = (self.ps_i + 1) % 8
            if b not in self.ps_held:
                return b

    def psum_hold(self):
        b = self.psum()
        self.ps_held.append(b)
        return b

    def psum_release(self, b):
        self.ps_held.remove(b)

    def dram_in(self, name, shape, dtype=F32):
        t = self.nc.dram_tensor(name, list(shape), dtype, kind="ExternalInput").ap()
        self.inputs[name] = (tuple(shape), dtype)
        return t

    def dram_scratch(self, name, shape, dtype=F32):
        kind = "ExternalOutput" if name in self.debug else "Internal"
        return self.nc.dram_tensor(name, list(shape), dtype, kind=kind).ap()

    def sb(self, stack, name, shape, dtype=F32):
        t = stack.enter_context(self.nc.sbuf_tensor(f"{name}_{self.uid()}", list(shape), dtype))
        return Buf(name, t)

    def mm(self, out, lhsT, rhs, start, stop, reads=(), writes=(), inc=False, **kw):
        return self.s.op("pe", lambda e: e.matmul(out, lhsT, rhs, start=start, stop=stop, **kw),
                         reads=reads, writes=writes, inc=inc)

    def tr(self, out, in_, ident, reads=(), writes=(), inc=False):
        return self.s.op("pe", lambda e: e.transpose(out, in_, ident), reads=reads, writes=writes, inc=inc)

    def load(self, out_ap, in_ap, writes, reads=(), **kw):
        return self.s.dma("sp", out_ap, in_ap, reads=reads, writes=writes, **kw)

    def store(self, out_ap, in_ap, reads, writes=(), **kw):
        return self.s.dma("pool", out_ap, in_ap, reads=reads, writes=writes, **kw)

    def build(self):
        cfg, nc, s = self.cfg, self.nc, self.s
        L, N, NE = cfg.DEPTH, cfg.N, cfg.NE
        R = self.root
        self.xT0 = self.dram_in("xT0", [D, N])
        self.condT = self.dram_in("condT", [128, 2, NCH])
        self.ada_w = self.dram_in("ada_w", [L, D, 6 * D])
        self.ada_bT = self.dram_in("ada_bT", [L, 128, 48])
        self.normT = self.dram_in("normT", [L, 128, 2, NCH])
        self.cst = self.dram_in("cst", [128, 1024])
        self.router_w = self.dram_in("router_w", [L, 128, NCH, NE])
        self.router_b = self.dram_in("router_b", [L, 128, NE])
        self.w1 = self.dram_in("moe_w1", [L, NE, D, 2 * D])
        self.b1T = self.dram_in("moe_b1T", [L, NE, 128, 16])
        self.w2 = self.dram_in("moe_w2", [L, NE, D, D])
        self.b2 = self.dram_in("moe_b2", [L, NE, D])
        NEV = max(cfg.N_EVEN, 1)
        self.attn_w_in = self.dram_in("attn_w_in", [NEV, D, 1728])
        self.mla_w_uq = self.dram_in("mla_w_uq", [NEV, 384, 768])
        self.mla_w_ukv = self.dram_in("mla_w_ukv", [NEV, 256, 1024])
        self.attn_w_out = self.dram_in("attn_w_out", [NEV, D, D])
        self.attn_g = self.dram_in("attn_g", [NEV, 128, 16])
        self.ropeM = self.dram_in("ropeM", [64, 2, N])
        self.ropeG = self.dram_in("ropeG", [128, 2, N])
        NOD, NVR = max(cfg.N_ODD, 1), max(cfg.N_ODD - 1, 1)
        for nm in ("rw_wr", "rw_wk", "rw_wv", "rw_wo"):
            setattr(self, nm, self.dram_in(nm, [NOD, D, D]))
        self.rw_w1 = self.dram_in("rw_w1", [NOD, 2, D, 64])
        self.rw_w2 = self.dram_in("rw_w2", [NOD, 2, 64, D])
        self.rw_a1 = self.dram_in("rw_a1", [NOD, 2, D, 64])
        self.rw_a2 = self.dram_in("rw_a2", [NOD, 2, 64, D])
        self.rw_g1 = self.dram_in("rw_g1", [NOD, D, 128])
        self.rw_g2 = self.dram_in("rw_g2", [NOD, 128, D])
        self.rw_v1 = self.dram_in("rw_v1", [NVR, D, 32])
        self.rw_v2 = self.dram_in("rw_v2", [NVR, 32, D])
        self.rw_p = self.dram_in("rw_p", [NOD, 128, 16, NCH])
        self.rw_bc = self.dram_in("rw_bc", [NOD, 3, 128, D])
        self.cstR = self.dram_in("cstR", [128, 2176])
        self.cstM_d = self.dram_in("cstM", [128, 384])
        self.outT = self.nc.dram_tensor("outT", [D, cfg.SEQ], F32, kind="ExternalOutput").ap()
        self.XT = self.dram_scratch("XT", [D, N])
        self.C = self.sb(R, "cst", [128, 1024])
        self.ident = self.C[:, 0:128]
        self.ones = self.C[:, 128:256]
        self.blk1 = self.C[:, 256:384]
        self.onesb = self.sb(R, "onesb", [128, 128], BF16)
        self.mod = self.sb(R, "mod", [128, L, 2, 64])
        self.normS = self.sb(R, "normS", [128, L, 2, NCH])
        self.load(self.C[:], self.cst[:, :], writes=[self.C])
        self.cstMb = self.sb(R, "cstM", [128, 384])
        self.cstM = self.cstMb.ap
        self.load(self.cstMb[:], self.cstM_d[:, :], writes=[self.cstMb])
        self.eps_t = self.sb(R, "eps", [128, 1])
        s.op("dve", lambda e: e.memset(self.eps_t[:], 1e-6), writes=[self.eps_t])
        self.eps_ap = self.eps_t[:, 0:1]
        s.op("dve", lambda e: e.tensor_copy(self.onesb[:], self.ones), reads=[self.C], writes=[self.onesb])
        self.load(self.normS[:], self.normT.rearrange("l p a c -> p l a c"), writes=[self.normS])

        self.phase_mods()
        s.dma("sp", self.XT[:, :], self.xT0[:, :])
        s.barrier()
        for l in range(L):
            self.layer(l)
        s.dma("sp", self.outT[:, :], self.XT[:, cfg.CTX:cfg.N])
        s.barrier()
        self.root.close()
        return nc

    def mvec(self, l, col, j, c):
        k = j * 8 + c
        return self.mod[:, l, col, k:k + 1]

    def phase_mods(self):
        cfg, nc, s = self.cfg, self.nc, self.s
        L = cfg.DEPTH
        with ExitStack() as st:
            cond = self.sb(st, "cond", [128, 2, NCH])
            scond = self.sb(st, "scond", [128, NCH, 2])
            abT = self.sb(st, "abT", [128, L, 48])
            wrot = Rot(self, st, "adaw", [128, 6 * D], F32, 2)
            self.load(cond[:], self.condT[:, :, :], writes=[cond])
            self.load(abT[:], self.ada_bT.rearrange("l p k -> p l k"), writes=[abT])
            for col in range(2):
                s.op("act", lambda e, col=col: e.activation(scond[:, :, col], cond[:, col, :], AF.Silu),
                     reads=[cond], writes=[scond])
            for l in range(L):
                ps = self.psum()
                s.op("dve", lambda e: e.memset(ps[:, 0:96], 0.0), writes=[ps])
                for dc in range(NCH):
                    wt = wrot.next()
                    self.load(wt[:], self.ada_w[l, dc * 128:(dc + 1) * 128, :], writes=[wt])
                    for j in range(48):
                        self.mm(ps[:, 2 * j:2 * j + 2], wt[:, j * 128:(j + 1) * 128], scond[:, dc, :],
                                start=False, stop=False, reads=[wt, scond], writes=[ps],
                                inc=(j == 47), skip_group_check=True)
                for col in range(2):
                    s.op("dve", lambda e, col=col: e.tensor_tensor(
                        self.mod[:, l, col, 0:48], ps[:, col:96:2], abT[:, l, :], ALU.add),
                        reads=[ps, abT], writes=[self.mod])
                    for (dst, jsc, nrm) in ((48, 1, 0), (56, 4, 1)):
                        s.op("dve", lambda e, col=col, dst=dst, jsc=jsc, nrm=nrm: e.scalar_tensor_tensor(
                            self.mod[:, l, col, dst:dst + 8], self.mod[:, l, col, jsc * 8:jsc * 8 + 8], 1.0,
                            self.normS[:, l, nrm, :], ALU.add, ALU.mult),
                            reads=[self.mod, self.normS], writes=[self.mod])
            s.barrier()

    def norm_block(self, st_pools, l, which, t0, tb, col, out):
        s = self.s
        xr, sqr, rsr, tr_ = st_pools
        x32 = xr.next()
        self.load(x32[:, :, 0:tb], self.XT.rearrange("(c p) t -> p c t", p=128)[:, :, t0:t0 + tb], writes=[x32])
        sq = sqr.next()
        s.op("act", lambda e: e.activation(sq[:, :, 0:tb], x32[:, :, 0:tb], AF.Square), reads=[x32], writes=[sq])
        ps = self.psum()
        for c in range(NCH):
            self.mm(ps[:, 0:tb], self.onesb[:], sq[:, c, 0:tb], start=(c == 0), stop=(c == NCH - 1),
                    reads=[sq, self.onesb], writes=[ps], inc=(c == NCH - 1))
        rs = rsr.next()
        s.op("act", lambda e: e.activation(rs[:, 0:tb], ps[:, 0:tb], AF.Sqrt, bias=self.eps_ap, scale=1.0 / D),
             reads=[ps], writes=[rs])
        s.op("dve", lambda e: e.reciprocal(rs[:, 0:tb], rs[:, 0:tb]), reads=[rs], writes=[rs])
        jg, jsft = (6, 0) if which == 0 else (7, 3)
        for c in range(NCH):
            tmp = tr_.next()
            s.op("dve", lambda e: e.tensor_tensor(tmp[:, 0:tb], x32[:, c, 0:tb], rs[:, 0:tb], ALU.mult),
                 reads=[x32, rs], writes=[tmp])
            s.op("act", lambda e: e.activation(out[:, c, 0:tb], tmp[:, 0:tb], AF.Identity,
                                               bias=self.mvec(l, col, jsft, c), scale=self.mvec(l, col, jg, c)),
                 reads=[tmp, self.mod], writes=[out])
        return out

    def layer(self, l):
        cfg = self.cfg
        if "nomix" not in self.debug:
            if l % 2 == 0:
                self.phase_attn(l)
            else:
                self.phase_rwkv(l)
        self.phase_moe(l)

    def cast_load(self, dst_buf, dst_ap, src_ap, stg, eng="pool"):
        sg_ = stg.next()
        X = src_ap.shape[-1]
        self.load(sg_[:, 0:X], src_ap, writes=[sg_])
        self.s.op(eng, lambda e: e.tensor_copy(dst_ap, sg_[:, 0:X]), reads=[sg_], writes=[dst_buf])

    def phase_attn(self, l):
        cfg, nc, s = self.cfg, self.nc, self.s
        N, NT, CTX = cfg.N, cfg.NT, cfg.CTX
        ea = l // 2
        QT = self.dram_scratch(f"QT{l}", [12 * 128, N], BF16)
        KT = self.dram_scratch(f"KT{l}", [7 * 128, N], BF16)
        VT = self.dram_scratch(f"VT{l}", [N, 768], BF16)
        QTv = QT.rearrange("(s p) t -> p s t", p=128)
        KTv = KT.rearrange("(s p) t -> p s t", p=128)
        MLA_SCALE = 192.0 ** -0.5
        GQA_SCALE = 128.0 ** -0.5
        with ExitStack() as st:
            win = self.sb(st, "win", [128, NCH, 1728], BF16)
            wuq = self.sb(st, "wuq", [128, 3, 768], BF16)
            wukv = self.sb(st, "wukv", [128, 2, 1024], BF16)
            ag = self.sb(st, "ag", [128, 16])
            self.load(ag[:], self.attn_g[ea], writes=[ag])
            with ExitStack() as st0:
                stg = Rot(self, st0, "stgA", [128, 1728], F32, 2)
                for dc in range(NCH):
                    self.cast_load(win, win[:, dc, :], self.attn_w_in[ea, dc * 128:(dc + 1) * 128, :], stg)
                for rc in range(3):
                    self.cast_load(wuq, wuq[:, rc, :], self.mla_w_uq[ea, rc * 128:(rc + 1) * 128, :], stg)
                for rc in range(2):
                    self.cast_load(wukv, wukv[:, rc, :], self.mla_w_ukv[ea, rc * 128:(rc + 1) * 128, :], stg)
                s.barrier()
            pools = (Rot(self, st, "x32", [128, NCH, 512], F32, 1), Rot(self, st, "sq", [128, NCH, 512], BF16, 1),
                     Rot(self, st, "rs", [128, 512], F32, 2), Rot(self, st, "nt", [128, 512], F32, 2))
            hbr = Rot(self, st, "hb", [128, NCH, 512], BF16, 2)
            zsr = Rot(self, st, "zs", [128, 3, 512], F32, 1)
            zqr = Rot(self, st, "zsq", [128, 3, 512], BF16, 1)
            znr = Rot(self, st, "zn", [128, 3, 512], BF16, 2)
            xsr = Rot(self, st, "xs", [128, 512], F32, 3)
            sqr = Rot(self, st, "sq1", [128, 512], BF16, 2)
            rsr = Rot(self, st, "rs1", [128, 512], F32, 2)
            yr = Rot(self, st, "y", [128, 512], F32, 3)
            t1r = Rot(self, st, "t1", [128, 512], F32, 2)
            t2r = Rot(self, st, "t2", [128, 512], F32, 2)
            rMr = Rot(self, st, "rM", [64, 2, 512], F32, 2)
            rGr = Rot(self, st, "rG", [128, 2, 512], F32, 2)
            qbr = Rot(self, st, "qblk", [128, 12, 512], BF16, 1)
            kbr = Rot(self, st, "kblk", [128, 7, 512], BF16, 1)
            vbr = Rot(self, st, "vblk", [128, 4, 768], BF16, 1)
            for b_ in qbr.bufs + kbr.bufs:
                s.op("pool", lambda e: e.memset(b_[:], 0.0), writes=[b_])
            permM = self.C[0:64, 384:448]
            permG = self.C[:, 512:640]

            for (t0, tb, col) in cfg.blocks:
                hb = self.norm_block(pools, l, 0, t0, tb, col, hbr.next())
                rM, rG = rMr.next(), rGr.next()
                self.load(rM[:, :, 0:tb], self.ropeM[:, :, t0:t0 + tb], writes=[rM])
                self.load(rG[:, :, 0:tb], self.ropeG[:, :, t0:t0 + tb], writes=[rG])
                qblk, kblk, vblk = qbr.next(), kbr.next(), vbr.next()

                def proj(ps, M, w, col0, rhs, nk):
                    for dc in range(nk):
                        self.mm(ps[0:M, 0:tb], w[:, dc, col0:col0 + M], rhs[:, dc, 0:tb], start=(dc == 0),
                                stop=(dc == nk - 1), reads=[w, rhs], writes=[ps], inc=(dc == nk - 1))

                def rms_multi(nchunk, col0, gcol0):
                    zs, zsq, zn = zsr.next(), zqr.next(), znr.next()
                    for c in range(nchunk):
                        ps = self.psum()
                        proj(ps, 128, win, col0 + c * 128, hb, NCH)
                        s.op("act", lambda e: e.copy(zs[:, c, 0:tb], ps[:, 0:tb]), reads=[ps], writes=[zs])
                        s.op("act", lambda e: e.activation(zsq[:, c, 0:tb], ps[:, 0:tb], AF.Square), reads=[ps], writes=[zsq])
                    ps2 = self.psum()
                    for c in range(nchunk):
                        self.mm(ps2[:, 0:tb], self.onesb[:], zsq[:, c, 0:tb], start=(c == 0), stop=(c == nchunk - 1),
                                reads=[zsq, self.onesb], writes=[ps2], inc=(c == nchunk - 1))
                    rs = rsr.next()
                    s.op("act", lambda e: e.activation(rs[:, 0:tb], ps2[:, 0:tb], AF.Sqrt, bias=self.eps_ap,
                                                       scale=1.0 / (nchunk * 128)), reads=[ps2], writes=[rs])
                    s.op("dve", lambda e: e.reciprocal(rs[:, 0:tb], rs[:, 0:tb]), reads=[rs], writes=[rs])
                    for c in range(nchunk):
                        s.op("dve", lambda e: e.scalar_tensor_tensor(zn[:, c, 0:tb], zs[:, c, 0:tb], ag[:, gcol0 + c:gcol0 + c + 1],
                                                                    rs[:, 0:tb], ALU.mult, ALU.mult),
                             reads=[zs, ag, rs], writes=[zn])
                    return zn

                def rms_feat(ps, M, gcol, dst_buf=None, dst_ap=None):
                    xs, sq1, rs = xsr.next(), sqr.next(), rsr.next()
                    s.op("act", lambda e: e.copy(xs[0:M, 0:tb], ps[0:M, 0:tb]), reads=[ps], writes=[xs])
                    s.op("act", lambda e: e.activation(sq1[0:M, 0:tb], ps[0:M, 0:tb], AF.Square), reads=[ps], writes=[sq1])
                    ps2 = self.psum()
                    self.mm(ps2[0:M, 0:tb], self.onesb[0:M, 0:M], sq1[0:M, 0:tb], start=True, stop=True,
                            reads=[sq1, self.onesb], writes=[ps2], inc=True)
                    s.op("act", lambda e: e.activation(rs[0:M, 0:tb], ps2[0:M, 0:tb], AF.Sqrt, bias=self.eps_t[0:M, 0:1],
                                                       scale=1.0 / M), reads=[ps2], writes=[rs])
                    s.op("dve", lambda e: e.reciprocal(rs[0:M, 0:tb], rs[0:M, 0:tb]), reads=[rs], writes=[rs])
                    if dst_buf is None:
                        y = yr.next()
                        dst_buf, dst_ap = y, y[0:M, 0:tb]
                    s.op("dve", lambda e: e.scalar_tensor_tensor(dst_ap, xs[0:M, 0:tb], ag[0:M, gcol:gcol + 1], rs[0:M, 0:tb],
                                                                ALU.mult, ALU.mult), reads=[xs, ag, rs], writes=[dst_buf])
                    return dst_buf

                def rope(y, M, perm, rt, dst_buf, dst_ap):
                    ps = self.psum()
                    self.mm(ps[0:M, 0:tb], perm, y[0:M, 0:tb], start=True, stop=True, reads=[y, self.C], writes=[ps], inc=True)
                    t1, t2 = t1r.next(), t2r.next()
                    s.op("pool", lambda e: e.tensor_tensor(t1[0:M, 0:tb], y[0:M, 0:tb], rt[0:M, 0, 0:tb], ALU.mult),
                         reads=[y, rt], writes=[t1])
                    s.op("dve", lambda e: e.tensor_tensor(t2[0:M, 0:tb], ps[0:M, 0:tb], rt[0:M, 1, 0:tb], ALU.mult),
                         reads=[ps, rt], writes=[t2])
                    s.op("dve", lambda e: e.tensor_tensor(dst_ap, t1[0:M, 0:tb], t2[0:M, 0:tb], ALU.add),
                         reads=[t1, t2], writes=[dst_buf])

                zqn = rms_multi(3, 0, 0)
                for h in range(4):
                    ps = self.psum()
                    proj(ps, 128, wuq, h * 192, zqn, 3)
                    rms_feat(ps, 128, 5, qblk, qblk[:, h, 0:tb])
                    ps = self.psum()
                    proj(ps, 64, wuq, h * 192 + 128, zqn, 3)
                    y = rms_feat(ps, 64, 6)
                    rope(y, 64, permM, rM, qblk, qblk[0:64, 4 + h, 0:tb])
                zkvn = rms_multi(2, 384, 3)
                for h in range(4):
                    ps = self.psum()
                    proj(ps, 128, wukv, h * 256, zkvn, 2)
                    rms_feat(ps, 128, 7, kblk, kblk[:, h, 0:tb])
                ps = self.psum()
                proj(ps, 64, win, 640, hb, NCH)
                y = rms_feat(ps, 64, 8)
                rope(y, 64, permM, rM, kblk, kblk[0:64, 4, 0:tb])
                for h in range(4):
                    ps = self.psum()
                    proj(ps, 128, win, 704 + h * 128, hb, NCH)
                    y = rms_feat(ps, 128, 9)
                    rope(y, 128, permG, rG, qblk, qblk[:, 8 + h, 0:tb])
                for g in range(2):
                    ps = self.psum()
                    proj(ps, 128, win, 1216 + g * 128, hb, NCH)
                    y = rms_feat(ps, 128, 10)
                    rope(y, 128, permG, rG, kblk, kblk[:, 5 + g, 0:tb])
                for sub in range(tb // 128):
                    ps = self.psum()
                    for h in range(4):
                        for rc in range(2):
                            self.mm(ps[:, h * 128:(h + 1) * 128], zkvn[:, rc, sub * 128:(sub + 1) * 128],
                                    wukv[:, rc, h * 256 + 128:h * 256 + 256], start=(rc == 0), stop=(rc == 1),
                                    reads=[zkvn, wukv], writes=[ps], inc=(h == 3 and rc == 1))
                    s.op("act", lambda e: e.copy(vblk[:, sub, 0:512], ps[:, :]), reads=[ps], writes=[vblk])
                    ps = self.psum()
                    for dc in range(NCH):
                        self.mm(ps[:, 0:256], hb[:, dc, sub * 128:(sub + 1) * 128], win[:, dc, 1472:1728],
                                start=(dc == 0), stop=(dc == NCH - 1), reads=[hb, win], writes=[ps], inc=(dc == NCH - 1))
                    s.op("act", lambda e: e.copy(vblk[:, sub, 512:768], ps[:, 0:256]), reads=[ps], writes=[vblk])
                nsub = tb // 128
                self.store(QTv[:, :, t0:t0 + tb], qblk[:, :, 0:tb], reads=[qblk])
                self.store(KTv[:, :, t0:t0 + tb], kblk[:, :, 0:tb], reads=[kblk])
                self.store(VT[t0:t0 + tb, :].rearrange("(a p) f -> p a f", p=128), vblk[:, 0:nsub, :], reads=[vblk])
            s.barrier()
        with ExitStack() as st:
            Kr = self.sb(st, "Kres", [128, 7, N], BF16)
            Vr = self.sb(st, "Vres", [128, NT, 768], BF16)
            wo = self.sb(st, "wo", [128, NCH, D], BF16)
            stg = Rot(self, st, "stgB", [128, D], F32, 2)
            self.load(Kr[:], KTv, writes=[Kr])
            self.load(Vr[:], VT.rearrange("(a p) f -> p a f", p=128), writes=[Vr])
            for hc in range(NCH):
                self.cast_load(wo, wo[:, hc, :], self.attn_w_out[ea, hc * 128:(hc + 1) * 128, :], stg)
            qr_ = Rot(self, st, "qin", [128, 12, 512], BF16, 1)
            ptr = Rot(self, st, "pT", [128, 512], BF16, 4)
            aor = Rot(self, st, "ao", [128, NCH, 512], BF16, 1)
            rdr = Rot(self, st, "rden", [128, 512], F32, 2)
            xur = Rot(self, st, "xa", [128, NCH, 512], F32, 1)
            for (t0, tb, col) in cfg.blocks:
                nkt = (CTX // 128) if col == 1 else NT
                qin = qr_.next()
                self.load(qin[:, :, 0:tb], QTv[:, :, t0:t0 + tb], writes=[qin])
                ao = aor.next()
                for hd in range(8):
                    pso, psd = self.psum_hold(), self.psum_hold()
                    for kt in range(nkt):
                        ks = slice(kt * 128, (kt + 1) * 128)
                        ps = self.psum()
                        if hd < 4:
                            self.mm(ps[:, 0:tb], Kr[:, hd, ks], qin[:, hd, 0:tb], start=True, stop=False,
                                    reads=[Kr, qin], writes=[ps], inc=False)
                            self.mm(ps[:, 0:tb], Kr[0:64, 4, ks], qin[0:64, 4 + hd, 0:tb], start=False, stop=True,
                                    reads=[Kr, qin], writes=[ps], inc=True)
                            scale, vs = MLA_SCALE, hd
                        else:
                            self.mm(ps[:, 0:tb], Kr[:, 5 + (hd - 4) // 2, ks], qin[:, 8 + hd - 4, 0:tb], start=True, stop=True,
                                    reads=[Kr, qin], writes=[ps], inc=True)
                            scale, vs = GQA_SCALE, 4 + (hd - 4) // 2
                        pT = ptr.next()
                        s.op("act", lambda e: e.activation(pT[:, 0:tb], ps[:, 0:tb], AF.Exp, scale=scale), reads=[ps], writes=[pT])
                        self.mm(pso[:, 0:tb], Vr[:, kt, vs * 128:(vs + 1) * 128], pT[:, 0:tb], start=(kt == 0), stop=(kt == nkt - 1),
                                reads=[Vr, pT], writes=[pso], inc=False)
                        self.mm(psd[:, 0:tb], self.onesb[:], pT[:, 0:tb], start=(kt == 0), stop=(kt == nkt - 1),
                                reads=[self.onesb, pT], writes=[psd], inc=True)
                    rd = rdr.next()
                    s.op("dve", lambda e: e.reciprocal(rd[:, 0:tb], psd[:, 0:tb]), reads=[psd], writes=[rd])
                    s.op("dve", lambda e: e.tensor_tensor(ao[:, hd, 0:tb], pso[:, 0:tb], rd[:, 0:tb], ALU.mult),
                         reads=[pso, rd], writes=[ao])
                    self.psum_release(pso)
                    self.psum_release(psd)
                xa = xur.next()
                xv = self.XT.rearrange("(c p) t -> p c t", p=128)[:, :, t0:t0 + tb]
                self.load(xa[:, :, 0:tb], xv, writes=[xa])
                for fc in range(NCH):
                    ps = self.psum()
                    for hd in range(8):
                        self.mm(ps[:, 0:tb], wo[:, hd, fc * 128:(fc + 1) * 128], ao[:, hd, 0:tb], start=(hd == 0), stop=(hd == 7),
                                reads=[wo, ao], writes=[ps], inc=(hd == 7))
                    s.op("dve", lambda e: e.scalar_tensor_tensor(xa[:, fc, 0:tb], ps[:, 0:tb], self.mvec(l, col, 2, fc),
                                                                xa[:, fc, 0:tb], ALU.mult, ALU.add),
                         reads=[ps, xa, self.mod], writes=[xa])
                self.store(xv, xa[:, :, 0:tb], reads=[xa])
            s.barrier()

    def phase_rwkv(self, l):
        cfg, nc, s = self.cfg, self.nc, self.s
        N, NT, CTX = cfg.N, cfg.NT, cfg.CTX
        j = l // 2
        C0 = 0.6065306597126334
        fmv = lambda a: a.rearrange("(c p) t -> p c t", p=128)
        HT = self.dram_scratch(f"HTr{l}", [D, N])
        RT = self.dram_scratch(f"RT{l}", [D, N])
        SG = [self.dram_scratch(f"SG{l}_{d}", [D, N]) for d in range(2)]
        KD = [self.dram_scratch(f"KD{l}_{d}", [D, N]) for d in range(2)]
        BD = [self.dram_scratch(f"BD{l}_{d}", [D, N]) for d in range(2)]
        NKK = self.dram_scratch(f"NKK{l}", [D, N])
        GT = self.dram_scratch(f"GT{l}", [D, N], BF16)
        VTOK = self.dram_scratch(f"VTOK{l}", [N, D])
        RK = self.dram_scratch(f"RK{l}", [N, 16])
        YD = [self.dram_scratch(f"YD{l}_{d}", [N, D]) for d in range(2)]
        if j == 0:
            self.VRAW = self.dram_scratch("VRAW", [N, D])
        with ExitStack() as st:
            pools = (Rot(self, st, "x32", [128, NCH, 512], F32, 2), Rot(self, st, "sq", [128, NCH, 512], BF16, 2),
                     Rot(self, st, "rs", [128, 512], F32, 2), Rot(self, st, "nt", [128, 512], F32, 3))
            h32r = Rot(self, st, "h32", [128, NCH, 512], F32, 2)
            for (t0, tb, col) in cfg.blocks:
                h32 = self.norm_block(pools, l, 0, t0, tb, col, h32r.next())
                self.store(fmv(HT)[:, :, t0:t0 + tb], h32[:, :, 0:tb], reads=[h32])
            s.barrier()
        if "stopA0" in self.debug:
            return
        with ExitStack() as st:
            wr = self.sb(st, "wr", [128, NCH, D], BF16)
            wk = self.sb(st, "wk", [128, NCH, D], BF16)
            wv = self.sb(st, "wv", [128, NCH, D], BF16)
            w1b = [self.sb(st, f"w1b{d}", [128, NCH, 64], BF16) for d in range(2)]
            a1b = [self.sb(st, f"a1b{d}", [128, NCH, 64], BF16) for d in range(2)]
            w2b = [self.sb(st, f"w2b{d}", [64, D], BF16) for d in range(2)]
            a2b = [self.sb(st, f"a2b{d}", [64, D], BF16) for d in range(2)]
            g1b = self.sb(st, "g1b", [128, NCH, 128], BF16)
            g2b = self.sb(st, "g2b", [128, D], BF16)
            v1b = self.sb(st, "v1b", [128, NCH, 32], BF16)
            v2b = self.sb(st, "v2b", [32, D], BF16)
            pp = self.sb(st, "rwp", [128, 16, NCH])
            v0bc = self.sb(st, "v0bc", [128, D])
            hsel = self.sb(st, "hsel", [128, NCH, 16])
            self.load(pp[:], self.rw_p[j], writes=[pp])
            self.load(hsel[:], self.cstR[:, 2048:2176].rearrange("p (c h) -> p c h", c=NCH), writes=[hsel])
            s.op("dve", lambda e: e.tensor_scalar(pp[:, 12, :], pp[:, 11, :], -1.0, 1.0, ALU.mult, ALU.add), reads=[pp], writes=[pp])
            s.op("dve", lambda e: e.tensor_scalar(pp[:, 14, :], pp[:, 10, :], -1.0, None, ALU.mult), reads=[pp], writes=[pp])
            pv = lambda i, c: pp[:, i, c:c + 1]
            with ExitStack() as st0:
                stg = Rot(self, st0, "stgR", [128, D], F32, 2)
                for (dst, src) in ((wr, self.rw_wr), (wk, self.rw_wk), (wv, self.rw_wv)):
                    for dc in range(NCH):
                        self.cast_load(dst, dst[:, dc, :], src[j, dc * 128:(dc + 1) * 128, :], stg)
                for d in range(2):
                    for (dst, src) in ((w1b[d], self.rw_w1), (a1b[d], self.rw_a1)):
                        sg_ = stg.next()
                        self.load(sg_[:, 0:512].rearrange("p (c m) -> p c m", c=NCH),
                                  src[j, d].rearrange("(c p) m -> p c m", p=128), writes=[sg_])
                        s.op("pool", lambda e: e.tensor_copy(dst[:], sg_[:, 0:512].rearrange("p (c m) -> p c m", c=NCH)),
                             reads=[sg_], writes=[dst])
                    for (dst, src) in ((w2b[d], self.rw_w2), (a2b[d], self.rw_a2)):
                        sg_ = stg.next()
                        self.load(sg_[0:64, :], src[j, d], writes=[sg_])
                        s.op("pool", lambda e: e.tensor_copy(dst[:], sg_[0:64, :]), reads=[sg_], writes=[dst])
                sg_ = stg.next()
                self.load(sg_[:, :].rearrange("p (c m) -> p c m", c=NCH), self.rw_g1[j].rearrange("(c p) m -> p c m", p=128), writes=[sg_])
                s.op("pool", lambda e: e.tensor_copy(g1b[:], sg_[:, :].rearrange("p (c m) -> p c m", c=NCH)), reads=[sg_], writes=[g1b])
                self.cast_load(g2b, g2b[:], self.rw_g2[j], stg)
                if j > 0:
                    sg_ = stg.next()
                    self.load(sg_[:, 0:256].rearrange("p (c m) -> p c m", c=NCH),
                              self.rw_v1[j - 1].rearrange("(c p) m -> p c m", p=128), writes=[sg_])
                    s.op("pool", lambda e: e.tensor_copy(v1b[:], sg_[:, 0:256].rearrange("p (c m) -> p c m", c=NCH)),
                         reads=[sg_], writes=[v1b])
                    sg_ = stg.next()
                    self.load(sg_[0:32, :], self.rw_v2[j - 1], writes=[sg_])
                    s.op("pool", lambda e: e.tensor_copy(v2b[:], sg_[0:32, :]), reads=[sg_], writes=[v2b])
                    self.load(v0bc[:], self.rw_bc[j, 2], writes=[v0bc])
                s.barrier()
            TB = 256
            hhr = Rot(self, st, "hh", [128, NCH, TB + 2], F32, 2)
            xxr = Rot(self, st, "xx", [128, NCH, TB], F32, 1)
            xmr = Rot(self, st, "xm", [128, NCH, TB], BF16, 2)
            rTr = Rot(self, st, "rT", [128, NCH, TB], F32, 1)
            kTr = Rot(self, st, "kT", [128, NCH, TB], F32, 1)
            ksr = Rot(self, st, "ksum", [128, NCH, TB], F32, 1)
            o2r = Rot(self, st, "o2", [128, TB], F32, 6)
            nkr = Rot(self, st, "nkk", [128, TB], F32, NCH)
            lor = Rot(self, st, "lo", [128, TB], BF16, 3)
            vtr = Rot(self, st, "vt", [128, 2, D], F32, 1)
            vfr = Rot(self, st, "vf", [128, 2, D], F32, 1)
            sgr = Rot(self, st, "vsg", [128, 512], F32, 2)
            gbr = Rot(self, st, "gb", [128, NCH, TB], BF16, 1)
            rkr = Rot(self, st, "rk", [128, 2, 16], F32, 2)
            blocks = []
            for (s0, s1, col) in ((0, CTX, 1), (CTX, N, 0)):
                t = s0
                while t < s1:
                    tb = min(TB, s1 - t)
                    blocks.append((t, tb, col, s0, s1))
                    t += tb
            for (t0, tb, col, s0, s1) in blocks:
                hh = hhr.next()
                s.op("pool", lambda e: e.memset(hh[:, :, :], 0.0), writes=[hh])
                lo, hi = max(t0 - 1, s0), min(t0 + tb + 1, s1)
                off = lo - (t0 - 1)
                self.load(hh[:, :, off:off + hi - lo], fmv(HT)[:, :, lo:hi], writes=[hh])
                hc = hh[:, :, 1:tb + 1]
                xx = xxr.next()
                s.op("dve", lambda e: e.tensor_tensor(xx[:, :, 0:tb], hh[:, :, 0:tb], hh[:, :, 2:tb + 2], ALU.add), reads=[hh], writes=[xx])
                s.op("dve", lambda e: e.scalar_tensor_tensor(xx[:, :, 0:tb], xx[:, :, 0:tb], 0.5, hc, ALU.mult, ALU.subtract),
                     reads=[xx, hh], writes=[xx])

                def mix(m):
                    xm = xmr.next()
                    for c in range(NCH):
                        s.op("dve", lambda e: e.scalar_tensor_tensor(xm[:, c, 0:tb], xx[:, c, 0:tb], pv(m, c), hh[:, c, 1:tb + 1],
                                                                    ALU.mult, ALU.add), reads=[xx, pp, hh], writes=[xm])
                    return xm

                def proj_fm(w, xm, fc, M=128, col0=None):
                    ps = self.psum()
                    c0_ = fc * 128 if col0 is None else col0
                    for dc in range(NCH):
                        self.mm(ps[0:M, 0:tb], w[:, dc, c0_:c0_ + M], xm[:, dc, 0:tb], start=(dc == 0), stop=(dc == NCH - 1),
                                reads=[w, xm], writes=[ps], inc=(dc == NCH - 1))
                    return ps

                xm = mix(0)
                rT = rTr.next()
                for fc in range(NCH):
                    ps = proj_fm(wr, xm, fc)
                    s.op("act", lambda e: e.copy(rT[:, fc, 0:tb], ps[:, 0:tb]), reads=[ps], writes=[rT])
                self.store(fmv(RT)[:, :, t0:t0 + tb], rT[:, :, 0:tb], reads=[rT])
                xm = mix(2)
                kT = kTr.next()
                for fc in range(NCH):
                    ps = proj_fm(wk, xm, fc)
                    s.op("act", lambda e: e.copy(kT[:, fc, 0:tb], ps[:, 0:tb]), reads=[ps], writes=[kT])
                nkk = []
                for fc in range(NCH):
                    sq = o2r.next()
                    s.op("act", lambda e: e.activation(sq[:, 0:tb], kT[:, fc, 0:tb], AF.Square, scale=pv(10, fc)), reads=[kT, pp], writes=[sq])
                    ps = self.psum()
                    self.mm(ps[:, 0:tb], self.blk1, sq[:, 0:tb], start=True, stop=True, reads=[self.C, sq], writes=[ps], inc=True)
                    rn = o2r.next()
                    s.op("dve", lambda e: e.tensor_scalar(rn[:, 0:tb], ps[:, 0:tb], 1e-24, None, ALU.max), reads=[ps], writes=[rn])
                    s.op("act", lambda e: e.activation(rn[:, 0:tb], rn[:, 0:tb], AF.Sqrt), reads=[rn], writes=[rn])
                    s.op("dve", lambda e: e.reciprocal(rn[:, 0:tb], rn[:, 0:tb]), reads=[rn], writes=[rn])
                    nk = nkr.next()
                    s.op("dve", lambda e: e.scalar_tensor_tensor(nk[:, 0:tb], kT[:, fc, 0:tb], pv(14, fc), rn[:, 0:tb], ALU.mult, ALU.mult),
                         reads=[kT, pp, rn], writes=[nk])
                    self.store(NKK[fc * 128:(fc + 1) * 128, t0:t0 + tb], nk[:, 0:tb], reads=[nk])
                    nkk.append(nk)
                    for d in range(2):
                        pass
                xm = mix(1)
                for d in range(2):
                    ps = proj_fm(w1b[d], xm, 0, M=64, col0=0)
                    lw = lor.next()
                    s.op("act", lambda e: e.activation(lw[0:64, 0:tb], ps[0:64, 0:tb], AF.Tanh), reads=[ps], writes=[lw])
                    for fc in range(NCH):
                        ps2 = self.psum()
                        self.mm(ps2[:, 0:tb], w2b[d][0:64, fc * 128:(fc + 1) * 128], lw[0:64, 0:tb], start=True, stop=True,
                                reads=[w2b[d], lw], writes=[ps2], inc=True)
                        sg = o2r.next()
                        s.op("act", lambda e: e.activation(sg[:, 0:tb], ps2[:, 0:tb], AF.Sigmoid, bias=pv(6 + d, fc)), reads=[ps2, pp], writes=[sg])
                        self.store(SG[d][fc * 128:(fc + 1) * 128, t0:t0 + tb], sg[:, 0:tb], reads=[sg])
                xm = mix(4)
                ksum = ksr.next()
                for d in range(2):
                    ps = proj_fm(a1b[d], xm, 0, M=64, col0=0)
                    la = lor.next()
                    s.op("act", lambda e: e.copy(la[0:64, 0:tb], ps[0:64, 0:tb]), reads=[ps], writes=[la])
                    for fc in range(NCH):
                        ps2 = self.psum()
                        self.mm(ps2[:, 0:tb], a2b[d][0:64, fc * 128:(fc + 1) * 128], la[0:64, 0:tb], start=True, stop=True,
                                reads=[a2b[d], la], writes=[ps2], inc=True)
                        a_ = o2r.next()
                        s.op("act", lambda e: e.activation(a_[:, 0:tb], ps2[:, 0:tb], AF.Sigmoid, bias=pv(8 + d, fc)), reads=[ps2, pp], writes=[a_])
                        kd = o2r.next()
                        s.op("dve", lambda e: e.tensor_scalar(kd[:, 0:tb], a_[:, 0:tb], pv(11, fc), pv(12, fc), ALU.mult, ALU.add),
                             reads=[a_, pp], writes=[kd])
                        s.op("dve", lambda e: e.tensor_tensor(kd[:, 0:tb], kd[:, 0:tb], kT[:, fc, 0:tb], ALU.mult), reads=[kd, kT], writes=[kd])
                        self.store(KD[d][fc * 128:(fc + 1) * 128, t0:t0 + tb], kd[:, 0:tb], reads=[kd])
                        if d == 0:
                            s.op("pool", lambda e: e.tensor_copy(ksum[:, fc, 0:tb], kd[:, 0:tb]), reads=[kd], writes=[ksum])
                        else:
                            s.op("pool", lambda e: e.tensor_tensor(ksum[:, fc, 0:tb], ksum[:, fc, 0:tb], kd[:, 0:tb], ALU.add),
                                 reads=[kd, ksum], writes=[ksum])
                        bt_ = o2r.next()
                        s.op("dve", lambda e: e.scalar_tensor_tensor(bt_[:, 0:tb], nkk[fc][:, 0:tb], -1.0, a_[:, 0:tb], ALU.mult, ALU.mult),
                             reads=[nkk[fc], a_], writes=[bt_])
                        self.store(BD[d][fc * 128:(fc + 1) * 128, t0:t0 + tb], bt_[:, 0:tb], reads=[bt_])
                for fc in range(NCH):
                    s.op("dve", lambda e: e.scalar_tensor_tensor(ksum[:, fc, 0:tb], ksum[:, fc, 0:tb], pv(13, fc), rT[:, fc, 0:tb],
                                                                ALU.mult, ALU.mult), reads=[ksum, pp, rT], writes=[ksum])
                rk = rkr.next()
                for sub in range(tb // 128):
                    ps = self.psum()
                    for fc in range(NCH):
                        self.mm(ps[:, 0:16], ksum[:, fc, sub * 128:(sub + 1) * 128], hsel[:, fc, :], start=(fc == 0), stop=(fc == NCH - 1),
                                reads=[ksum, hsel], writes=[ps], inc=(fc == NCH - 1))
                    s.op("act", lambda e: e.copy(rk[:, sub, :], ps[:, 0:16]), reads=[ps], writes=[rk])
                self.store(RK[t0:t0 + tb, :].rearrange("(a p) h -> p a h", p=128), rk[:, 0:tb // 128, :], reads=[rk])
                xm = mix(3)
                vt = vtr.next()
                for sub in range(tb // 128):
                    for hf in range(2):
                        ps = self.psum()
                        for dc in range(NCH):
                            self.mm(ps[:, :], xm[:, dc, sub * 128:(sub + 1) * 128], wv[:, dc, hf * 512:(hf + 1) * 512],
                                    start=(dc == 0), stop=(dc == NCH - 1), reads=[xm, wv], writes=[ps], inc=(dc == NCH - 1))
                        s.op("act", lambda e: e.copy(vt[:, sub, hf * 512:(hf + 1) * 512], ps[:, :]), reads=[ps], writes=[vt])
                nsub = tb // 128
                tokv = lambda a: a[t0:t0 + tb, :].rearrange("(a p) f -> p a f", p=128)
                if j == 0:
                    self.store(tokv(self.VRAW), vt[:, 0:nsub, :], reads=[vt])
                else:
                    vf = vfr.next()
                    self.load(vf[:, 0:nsub, :], tokv(self.VRAW), writes=[vf])
                    ps = proj_fm(v1b, xm, 0, M=32, col0=0)
                    l32 = lor.next()
                    s.op("act", lambda e: e.copy(l32[0:32, 0:tb], ps[0:32, 0:tb]), reads=[ps], writes=[l32])
                    for sub in range(nsub):
                        for hf in range(2):
                            hs = slice(hf * 512, (hf + 1) * 512)
                            ps2 = self.psum()
                            self.mm(ps2[:, :], l32[0:32, sub * 128:(sub + 1) * 128], v2b[0:32, hs], start=True, stop=True,
                                    reads=[l32, v2b], writes=[ps2], inc=True)
                            sg = sgr.next()
                            s.op("dve", lambda e: e.tensor_tensor(sg[:, :], ps2[:, :], v0bc[:, hs], ALU.add), reads=[ps2, v0bc], writes=[sg])
                            s.op("act", lambda e: e.activation(sg[:, :], sg[:, :], AF.Sigmoid), reads=[sg], writes=[sg])
                            s.op("dve", lambda e: e.tensor_tensor(vf[:, sub, hs], vf[:, sub, hs], vt[:, sub, hs], ALU.subtract), reads=[vf, vt], writes=[vf])
                            s.op("dve", lambda e: e.tensor_tensor(vf[:, sub, hs], vf[:, sub, hs], sg[:, :], ALU.mult), reads=[vf, sg], writes=[vf])
                            s.op("dve", lambda e: e.tensor_tensor(vt[:, sub, hs], vt[:, sub, hs], vf[:, sub, hs], ALU.add), reads=[vf, vt], writes=[vt])
                self.store(tokv(VTOK), vt[:, 0:nsub, :], reads=[vt])
                xm = mix(5)
                ps = proj_fm(g1b, xm, 0, M=128, col0=0)
                lg = lor.next()
                s.op("act", lambda e: e.activation(lg[:, 0:tb], ps[:, 0:tb], AF.Sigmoid), reads=[ps], writes=[lg])
                gb = gbr.next()
                for fc in range(NCH):
                    ps2 = self.psum()
                    self.mm(ps2[:, 0:tb], g2b[:, fc * 128:(fc + 1) * 128], lg[:, 0:tb], start=True, stop=True, reads=[g2b, lg], writes=[ps2], inc=True)
                    s.op("act", lambda e: e.copy(gb[:, fc, 0:tb], ps2[:, 0:tb]), reads=[ps2], writes=[gb])
                self.store(fmv(GT)[:, :, t0:t0 + tb], gb[:, :, 0:tb], reads=[gb])
            s.barrier()
        if "stopA1" in self.debug:
            return
        with ExitStack() as st:
            msk = self.sb(st, "msk", [128, 2048])
            self.load(msk[:], self.cstR[:, 0:2048], writes=[msk])
            ST = self.sb(st, "ST", [128, NCH, 64])
            onesf = self.ones
            ldr = {k: Rot(self, st, f"ld{k}", [128, NCH, 128], F32, 2) for k in ("r", "sg", "kd", "nkk", "bd")}
            vr_ = Rot(self, st, "ldv", [128, D], F32, 2)
            csr = Rot(self, st, "cs", [128, NCH, 128], F32, 1)
            Pr = Rot(self, st, "P", [128, NCH, 128], F32, 1)
            Pir = Rot(self, st, "Pi", [128, NCH, 128], F32, 1)
            Pxr = Rot(self, st, "Px", [128, NCH, 128], F32, 1)
            ARr = Rot(self, st, "AR", [128, NCH, 256], F32, 1)
            BTr = Rot(self, st, "BT", [128, NCH, 128], F32, 1)
            KTr = Rot(self, st, "KTt", [128, NCH, 128], F32, 1)
            Btr = Rot(self, st, "Btok", [128, D], F32, 1)
            Ktr = Rot(self, st, "Ktok", [128, D], F32, 1)
            Mh = [self.sb(st, f"Mh{h}", [128, 512]) for h in range(16)]
            A4 = [[self.sb(st, f"A4_{g}_{i}", [128, 512]) for i in range(2)] for g in range(4)]
            AT4 = [[self.sb(st, f"AT4_{g}_{i}", [128, 512]) for i in range(2)] for g in range(4)]
            TT4 = [self.sb(st, f"TT4_{g}", [128, 512]) for g in range(4)]
            Wsb = self.sb(st, "Wsb", [128, D])
            Usb = self.sb(st, "Usb", [128, D])
            Ysb = self.sb(st, "Ysb", [128, D])
            pcb = self.sb(st, "pcb", [128, NCH, 64])
            tot = self.sb(st, "tot", [128, NCH])
            stmp = self.sb(st, "stmp", [128, NCH, 64])
            nctx = CTX // 128
            for d in range(2):
                order = list(range(NT)) if d == 0 else (list(range(nctx - 1, -1, -1)) + list(range(NT - 1, nctx - 1, -1)))
                s.op("dve", lambda e: e.memset(ST[:], 0.0), writes=[ST])
                m512 = msk[:, d * 512:(d + 1) * 512]
                mA4 = msk[:, 1024 + d * 512:1024 + (d + 1) * 512]
                for ci in order:
                    ts_ = slice(ci * 128, (ci + 1) * 128)
                    ld = {k: ldr[k].next() for k in ldr}
                    for k, src in (("r", RT), ("sg", SG[d]), ("kd", KD[d]), ("nkk", NKK), ("bd", BD[d])):
                        self.load(ld[k][:], fmv(src)[:, :, ts_], writes=[ld[k]])
                    V = vr_.next()
                    self.load(V[:], VTOK[ts_, :], writes=[V])
                    cs, P_, Pi, Px = csr.next(), Pr.next(), Pir.next(), Pxr.next()
                    for fc in range(NCH):
                        s.op("dve", lambda e: e.tensor_tensor_scan(cs[:, fc, :], onesf, ld["sg"][:, fc, :], 0.0, ALU.mult, ALU.add),
                             reads=[self.C, ld["sg"]], writes=[cs])
                    if d == 1:
                        s.op("dve", lambda e: e.tensor_copy(tot[:], cs[:, :, 127]), reads=[cs], writes=[tot])
                        for fc in range(NCH):
                            s.op("dve", lambda e: e.scalar_tensor_tensor(cs[:, fc, :], cs[:, fc, :], -1.0, ld["sg"][:, fc, :], ALU.mult, ALU.add),
                                 reads=[cs, ld["sg"]], writes=[cs])
                            s.op("dve", lambda e: e.tensor_scalar(cs[:, fc, :], cs[:, fc, :], tot[:, fc:fc + 1], None, ALU.add),
                                 reads=[cs, tot], writes=[cs])
                    last = 127 if d == 0 else 0
                    s.op("act", lambda e: e.activation(P_[:], cs[:], AF.Exp, scale=-C0), reads=[cs], writes=[P_])
                    s.op("act", lambda e: e.activation(Pi[:], cs[:], AF.Exp, scale=C0), reads=[cs], writes=[Pi])
                    s.op("dve", lambda e: e.tensor_tensor(Px[:], cs[:], ld["sg"][:], ALU.subtract), reads=[cs, ld["sg"]], writes=[Px])
                    s.op("act", lambda e: e.activation(Px[:], Px[:], AF.Exp, scale=-C0), reads=[Px], writes=[Px])
                    AR, BT, KTt = ARr.next(), BTr.next(), KTr.next()
                    s.op("dve", lambda e: e.tensor_tensor(AR[:, :, 0:128], ld["nkk"][:], Px[:], ALU.mult), reads=[ld["nkk"], Px], writes=[AR])
                    s.op("pool", lambda e: e.tensor_tensor(AR[:, :, 128:256], ld["r"][:], P_[:], ALU.mult), reads=[ld["r"], P_], writes=[AR])
                    s.op("dve", lambda e: e.tensor_tensor(BT[:], ld["bd"][:], Pi[:], ALU.mult), reads=[ld["bd"], Pi], writes=[BT])
                    s.op("pool", lambda e: e.tensor_tensor(KTt[:], ld["kd"][:], Pi[:], ALU.mult), reads=[ld["kd"], Pi], writes=[KTt])
                    for fc in range(NCH):
                        s.op("dve", lambda e: e.tensor_scalar(pcb[:, fc, :], onesf[:, 0:64], P_[:, fc, last:last + 1], None, ALU.mult),
                             reads=[self.C, P_], writes=[pcb])
                    if "B1" in self.debug:
                        continue
                    Btok, Ktok = Btr.next(), Ktr.next()
                    for (src, dst) in ((BT, Btok), (KTt, Ktok)):
                        for half in range(2):
                            ps = self.psum()
                            for q in range(4):
                                self.tr(ps[:, q * 128:(q + 1) * 128], src[:, half * 4 + q, :], self.ident, reads=[src, self.C], writes=[ps], inc=(q == 3))
                            s.op("act", lambda e: e.copy(dst[:, half * 512:(half + 1) * 512], ps[:, :]), reads=[ps], writes=[dst])
                    if "B2" in self.debug:
                        continue
                    for h in range(16):
                        fc, sl = h // 2, slice((h % 2) * 64, (h % 2) * 64 + 64)
                        ps = self.psum()
                        self.mm(ps[:, 0:256], BT[sl, fc, :], AR[sl, fc, :], start=True, stop=True, reads=[BT, AR], writes=[ps], inc=False)
                        self.mm(ps[:, 256:512], KTt[sl, fc, :], AR[sl, fc, :], start=True, stop=True, reads=[KTt, AR], writes=[ps], inc=True)
                        s.op("dve", lambda e: e.tensor_tensor(Mh[h][:], ps[:, :], m512, ALU.mult), reads=[ps, msk], writes=[Mh[h]])
                    if "B3" in self.debug:
                        continue
                    cur = [0, 0, 0, 0]
                    hof = lambda g, q: (g // 2) * 8 + 2 * q + (g % 2)
                    for g in range(4):
                        ps = self.psum()
                        for q in range(4):
                            h = hof(g, q)
                            fc, sl = h // 2, slice((h % 2) * 64, (h % 2) * 64 + 64)
                            self.mm(ps[:, q * 128:(q + 1) * 128], AR[sl, fc, 0:128], BT[sl, fc, :], start=True, stop=True,
                                    reads=[AR, BT], writes=[ps], inc=(q == 3))
                        s.op("dve", lambda e: e.tensor_tensor(A4[g][0][:], ps[:, :], mA4, ALU.mult), reads=[ps, msk], writes=[A4[g][0]])
                        for q in range(4):
                            h = hof(g, q)
                            s.op("pool", lambda e: e.tensor_copy(AT4[g][0][:, q * 128:(q + 1) * 128], Mh[h][:, 0:128]), reads=[Mh[h]], writes=[AT4[g][0]])
                            s.op("pool", lambda e: e.tensor_tensor(TT4[g][:, q * 128:(q + 1) * 128], Mh[h][:, 0:128], self.ident, ALU.add),
                                 reads=[Mh[h], self.C], writes=[TT4[g]])
                    if "B4" in self.debug:
                        continue
                    for lev in range(6):
                        for g in range(4):
                            A_, AT_ = A4[g][cur[g]], AT4[g][cur[g]]
                            An, ATn = A4[g][1 - cur[g]], AT4[g][1 - cur[g]]
                            psA = self.psum()
                            for q in range(4):
                                qs = slice(q * 128, (q + 1) * 128)
                                self.mm(psA[:, qs], AT_[:, qs], A_[:, qs], start=True, stop=True, reads=[A_, AT_], writes=[psA], inc=(q == 3))
                            s.op("act", lambda e: e.copy(An[:], psA[:, :]), reads=[psA], writes=[An])
                            if lev < 5:
                                psT = self.psum()
                                for q in range(4):
                                    qs = slice(q * 128, (q + 1) * 128)
                                    self.mm(psT[:, qs], A_[:, qs], AT_[:, qs], start=True, stop=True, reads=[A_, AT_], writes=[psT], inc=(q == 3))
                                s.op("act", lambda e: e.copy(ATn[:], psT[:, :]), reads=[psT], writes=[ATn])
                            psU = self.psum()
                            for q in range(4):
                                qs = slice(q * 128, (q + 1) * 128)
                                self.mm(psU[:, qs], An[:, qs], TT4[g][:, qs], start=True, stop=True, reads=[An, TT4[g]], writes=[psU], inc=(q == 3))
                            s.op("dve", lambda e: e.tensor_tensor(TT4[g][:], psU[:, :], TT4[g][:], ALU.add), reads=[psU, TT4[g]], writes=[TT4[g]])
                            cur[g] = 1 - cur[g]
                    if "B5" in self.debug:
                        continue
                    hp = lambda h: (h // 2, slice((h % 2) * 64, (h % 2) * 64 + 64), slice(h * 64, (h + 1) * 64))
                    for b in range(2):
                        ps = self.psum()
                        for q in range(8):
                            h = b * 8 + q
                            fc, sl, hs = hp(h)
                            qs = slice(q * 64, (q + 1) * 64)
                            self.mm(ps[:, qs], AR[sl, fc, 0:128], ST[sl, fc, :], start=True, stop=False, reads=[AR, ST], writes=[ps], inc=False)
                            self.mm(ps[:, qs], Mh[h][:, 256:384], V[:, hs], start=False, stop=True, reads=[Mh[h], V], writes=[ps], inc=(q == 7))
                        s.op("act", lambda e: e.copy(Wsb[:, b * 512:(b + 1) * 512], ps[:, :]), reads=[ps], writes=[Wsb])
                    for b in range(2):
                        ps = self.psum()
                        for q in range(8):
                            h = b * 8 + q
                            g, qq = 2 * (h // 8) + (h % 2), (h % 8) // 2
                            self.mm(ps[:, q * 64:(q + 1) * 64], TT4[g][:, qq * 128:(qq + 1) * 128], Wsb[:, h * 64:(h + 1) * 64],
                                    start=True, stop=True, reads=[TT4[g], Wsb], writes=[ps], inc=(q == 7))
                        s.op("act", lambda e: e.copy(Usb[:, b * 512:(b + 1) * 512], ps[:, :]), reads=[ps], writes=[Usb])
                    for b in range(2):
                        ps = self.psum()
                        for q in range(8):
                            h = b * 8 + q
                            fc, sl, hs = hp(h)
                            qs = slice(q * 64, (q + 1) * 64)
                            self.mm(ps[:, qs], AR[sl, fc, 128:256], ST[sl, fc, :], start=True, stop=False, reads=[AR, ST], writes=[ps], inc=False)
                            self.mm(ps[:, qs], Mh[h][:, 128:256], Usb[:, hs], start=False, stop=False, reads=[Mh[h], Usb], writes=[ps], inc=False)
                            self.mm(ps[:, qs], Mh[h][:, 384:512], V[:, hs], start=False, stop=True, reads=[Mh[h], V], writes=[ps], inc=(q == 7))
                        s.op("act", lambda e: e.copy(Ysb[:, b * 512:(b + 1) * 512], ps[:, :]), reads=[ps], writes=[Ysb])
                    self.store(YD[d][ts_, :], Ysb[:], reads=[Ysb])
                    for b in range(2):
                        ps = self.psum()
                        for q in range(8):
                            h = b * 8 + q
                            fc, sl, hs = hp(h)
                            qs = slice(q * 64, (q + 1) * 64)
                            self.mm(ps[:, qs], Btok[:, fc * 128:(fc + 1) * 128], Usb[:, hs], start=True, stop=False, reads=[Btok, Usb], writes=[ps], inc=False)
                            self.mm(ps[:, qs], Ktok[:, fc * 128:(fc + 1) * 128], V[:, hs], start=False, stop=True, reads=[Ktok, V], writes=[ps], inc=(q == 7))
                        psv = ps[:, :].rearrange("p (f two v) -> p f two v", two=2, v=64)
                        for par in range(2):
                            sl = slice(par * 64, par * 64 + 64)
                            fcs = slice(b * 4, b * 4 + 4)
                            s.op("dve", lambda e: e.tensor_tensor(stmp[sl, fcs, :], psv[sl, :, par, :], ST[sl, fcs, :], ALU.add),
                                 reads=[ps, ST], writes=[stmp])
                            s.op("dve", lambda e: e.tensor_tensor(ST[sl, fcs, :], stmp[sl, fcs, :], pcb[sl, fcs, :], ALU.mult),
                                 reads=[stmp, pcb], writes=[ST])
            s.barrier()
        if "stopB" in self.debug:
            return
        with ExitStack() as st:
            wo = self.sb(st, "rwo", [128, NCH, D], BF16)
            lnw = self.sb(st, "lnw", [128, D])
            lnb = self.sb(st, "lnb", [128, D])
            self.load(lnw[:], self.rw_bc[j, 0], writes=[lnw])
            self.load(lnb[:], self.rw_bc[j, 1], writes=[lnb])
            with ExitStack() as st0:
                stg = Rot(self, st0, "stgO", [128, D], F32, 2)
                for dc in range(NCH):
                    self.cast_load(wo, wo[:, dc, :], self.rw_wo[j, dc * 128:(dc + 1) * 128, :], stg)
                s.barrier()
            y0r = Rot(self, st, "y0", [128, D], F32, 2)
            y1r = Rot(self, st, "y1", [128, D], F32, 2)
            vr_ = Rot(self, st, "vC", [128, D], F32, 2)
            rkr = Rot(self, st, "rkC", [128, 16], F32, 2)
            str_ = Rot(self, st, "stat", [128, 64], F32, 2)
            sqr = Rot(self, st, "sqC", [128, D], F32, 1)
            gtr = Rot(self, st, "gtC", [128, NCH, 512], BF16, 1)
            zgr = Rot(self, st, "zgT", [128, NCH, 512], BF16, 1)
            xur = Rot(self, st, "xC", [128, NCH, 512], F32, 1)
            h3 = lambda a: a.rearrange("p (h n) -> p h n", n=64)
            for (t0, tb, col) in cfg.blocks:
                gt, zg = gtr.next(), zgr.next()
                self.load(gt[:, :, 0:tb], fmv(GT)[:, :, t0:t0 + tb], writes=[gt])
                for sub in range(tb // 128):
                    ts_ = slice(t0 + sub * 128, t0 + (sub + 1) * 128)
                    y0, y1, v, rk, sa, sq = y0r.next(), y1r.next(), vr_.next(), rkr.next(), str_.next(), sqr.next()
                    self.load(y0[:], YD[0][ts_, :], writes=[y0])
                    self.load(y1[:], YD[1][ts_, :], writes=[y1])
                    self.load(v[:], VTOK[ts_, :], writes=[v])
                    self.load(rk[:], RK[ts_, :], writes=[rk])
                    s.op("dve", lambda e: e.tensor_tensor(y0[:], y0[:], y1[:], ALU.add), reads=[y0, y1], writes=[y0])
                    s.op("dve", lambda e: e.tensor_reduce(sa[:, 0:16], h3(y0[:]), AX.X, ALU.add), reads=[y0], writes=[sa])
                    s.op("dve", lambda e: e.tensor_scalar(sa[:, 0:16], sa[:, 0:16], 1.0 / 64, None, ALU.mult), reads=[sa], writes=[sa])
                    s.op("dve", lambda e: e.tensor_tensor(h3(y0[:]), h3(y0[:]), sa[:, 0:16].unsqueeze(2).broadcast_to([128, 16, 64]), ALU.subtract),
                         reads=[y0, sa], writes=[y0])
                    s.op("pool", lambda e: e.tensor_tensor(sq[:], y0[:], y0[:], ALU.mult), reads=[y0], writes=[sq])
                    s.op("dve", lambda e: e.tensor_reduce(sa[:, 16:32], h3(sq[:]), AX.X, ALU.add), reads=[sq], writes=[sa])
                    s.op("dve", lambda e: e.tensor_scalar(sa[:, 16:32], sa[:, 16:32], 1.0 / 64, 64e-5, ALU.mult, ALU.add), reads=[sa], writes=[sa])
                    s.op("act", lambda e: e.activation(sa[:, 16:32], sa[:, 16:32], AF.Sqrt), reads=[sa], writes=[sa])
                    s.op("dve", lambda e: e.reciprocal(sa[:, 16:32], sa[:, 16:32]), reads=[sa], writes=[sa])
                    s.op("dve", lambda e: e.tensor_tensor(h3(y0[:]), h3(y0[:]), sa[:, 16:32].unsqueeze(2).broadcast_to([128, 16, 64]), ALU.mult),
                         reads=[y0, sa], writes=[y0])
                    s.op("pool", lambda e: e.tensor_tensor(y0[:], y0[:], lnw[:], ALU.mult), reads=[y0, lnw], writes=[y0])
                    s.op("pool", lambda e: e.tensor_tensor(y0[:], y0[:], lnb[:], ALU.add), reads=[y0, lnb], writes=[y0])
                    s.op("dve", lambda e: e.tensor_tensor(h3(v[:]), h3(v[:]), rk[:, 0:16].unsqueeze(2).broadcast_to([128, 16, 64]), ALU.mult),
                         reads=[v, rk], writes=[v])
                    s.op("dve", lambda e: e.tensor_tensor(y0[:], y0[:], v[:], ALU.add), reads=[y0, v], writes=[y0])
                    for half in range(2):
                        ps = self.psum()
                        for q in range(4):
                            c = half * 4 + q
                            self.tr(ps[:, q * 128:(q + 1) * 128], y0[:, c * 128:(c + 1) * 128], self.ident, reads=[y0, self.C], writes=[ps], inc=(q == 3))
                        s.op("dve", lambda e: e.tensor_tensor(zg[:, half * 4:half * 4 + 4, sub * 128:(sub + 1) * 128],
                                                             ps[:, :].rearrange("p (q t) -> p q t", q=4),
                                                             gt[:, half * 4:half * 4 + 4, sub * 128:(sub + 1) * 128], ALU.mult),
                             reads=[ps, gt], writes=[zg])
                xa = xur.next()
                xv = fmv(self.XT)[:, :, t0:t0 + tb]
                self.load(xa[:, :, 0:tb], xv, writes=[xa])
                for fo in range(NCH):
                    ps = self.psum()
                    for fc in range(NCH):
                        self.mm(ps[:, 0:tb], wo[:, fc, fo * 128:(fo + 1) * 128], zg[:, fc, 0:tb], start=(fc == 0), stop=(fc == NCH - 1),
                                reads=[wo, zg], writes=[ps], inc=(fc == NCH - 1))
                    s.op("dve", lambda e: e.scalar_tensor_tensor(xa[:, fo, 0:tb], ps[:, 0:tb], self.mvec(l, col, 2, fo), xa[:, fo, 0:tb],
                                                                ALU.mult, ALU.add), reads=[ps, xa, self.mod], writes=[xa])
                self.store(xv, xa[:, :, 0:tb], reads=[xa])
            s.barrier()

    def phase_moe(self, l):
        cfg, nc, s = self.cfg, self.nc, self.s
        NE, NT, N, TOPK = cfg.NE, cfg.NT, cfg.N, cfg.TOPK
        T = 512
        NSLOT = (N * TOPK + NE * (T - 1)) // T
        I32 = mybir.dt.int32
        BIG = 1.0e7
        fmv = lambda a: a.rearrange("(c p) t -> p c t", p=128)
        HTOK = self.dram_scratch(f"HTOK{l}", [N, D])
        HS = self.dram_scratch("HS", [NSLOT * T, D]) if l == 0 else self.HS
        YS = self.dram_scratch("YS", [NSLOT * T, D]) if l == 0 else self.YS
        self.HS, self.YS = HS, YS
        w1rows = self.w1.rearrange("l e d f -> (l e d) f")
        w2rows = self.w2.rearrange("l e d f -> (l e d) f")
        b1rows = self.b1T.rearrange("l e p k -> (l e p) k")
        with ExitStack() as st:
            gates = self.sb(st, "gates", [128, NT, NE])
            Mall = self.sb(st, "Mall", [128, NT, NE])
            POS = self.sb(st, "POS", [128, NT, 4], I32)
            GK = self.sb(st, "GK", [128, NT, 4])
            idxw = self.sb(st, "idxw", [128, NSLOT, NCH], I32)
            idxb = self.sb(st, "idxb", [128, NSLOT], I32)
            rw = self.sb(st, "rw", [128, NCH, NE])
            rb = self.sb(st, "rb", [128, NE])
            b2 = self.sb(st, "b2", [NE, D])
            off = self.sb(st, "off", [128, NE])
            self.load(rw[:], self.router_w[l], writes=[rw])
            self.load(rb[:], self.router_b[l], writes=[rb])
            self.load(b2[:], self.b2[l], writes=[b2])
            with ExitStack() as st2:
                pools = (Rot(self, st2, "x32", [128, NCH, 512], F32, 2), Rot(self, st2, "sq", [128, NCH, 512], BF16, 2),
                         Rot(self, st2, "rs", [128, 512], F32, 2), Rot(self, st2, "nt", [128, 512], F32, 3))
                h32r = Rot(self, st2, "h32", [128, NCH, 512], F32, 2)
                hrr = Rot(self, st2, "hrow", [128, D], F32, 3)
                smr = Rot(self, st2, "sm", [128, 64], F32, 4)
                cnt = self.psum_hold()
                htok_b = [Buf(f"htok{ti}") for ti in range(NT)]
                first = True
                for (t0, tb, col) in cfg.blocks:
                    h32 = self.norm_block(pools, l, 1, t0, tb, col, h32r.next())
                    for sub in range(tb // 128):
                        ti = t0 // 128 + sub
                        ps = self.psum()
                        for c in range(NCH):
                            self.mm(ps[:, 0:NE], h32[:, c, sub * 128:(sub + 1) * 128], rw[:, c, :],
                                    start=(c == 0), stop=(c == NCH - 1), reads=[h32, rw], writes=[ps],
                                    inc=(c == NCH - 1))
                        sm = smr.next()
                        lg = sm[:, 0:NE]
                        g = gates[:, ti, :]
                        M = Mall[:, ti, :]
                        s.op("dve", lambda e: e.tensor_tensor(lg, ps[:, 0:NE], rb[:], ALU.add), reads=[ps, rb], writes=[sm])
                        s.op("dve", lambda e: e.max(sm[:, 32:40], lg), reads=[sm], writes=[sm])
                        s.op("dve", lambda e: e.tensor_scalar(sm[:, 40:41], sm[:, 32:33], -1.0, None, ALU.mult),
                             reads=[sm], writes=[sm])
                        s.op("act", lambda e: e.activation(g, lg, AF.Exp, bias=sm[:, 40:41], scale=1.0),
                             reads=[sm], writes=[gates])
                        k = TOPK - 1
                        s.op("dve", lambda e: e.tensor_scalar(M, lg, sm[:, 32 + k:33 + k], None, ALU.is_ge), reads=[sm], writes=[Mall])
                        s.op("dve", lambda e: e.tensor_tensor(g, g, M, ALU.mult), reads=[Mall, gates], writes=[gates])
                        s.op("dve", lambda e: e.tensor_reduce(sm[:, 41:42], g, AX.X, ALU.add), reads=[gates], writes=[sm])
                        s.op("dve", lambda e: e.reciprocal(sm[:, 41:42], sm[:, 41:42]), reads=[sm], writes=[sm])
                        s.op("dve", lambda e: e.tensor_scalar(g, g, sm[:, 41:42], None, ALU.mult),
                             reads=[sm, gates], writes=[gates])
                        last = (t0 + tb == N) and (sub == tb // 128 - 1)
                        self.mm(cnt[:, 0:NE], self.ones, M, start=first, stop=last, reads=[self.C, Mall], writes=[cnt], inc=True)
                        first = False
                        hrow = hrr.next()
                        for half in range(2):
                            ps2 = self.psum()
                            for q in range(4):
                                c = half * 4 + q
                                self.tr(ps2[:, q * 128:(q + 1) * 128], h32[:, c, sub * 128:(sub + 1) * 128], self.ident,
                                        reads=[h32, self.C], writes=[ps2], inc=(q == 3))
                            s.op("act", lambda e: e.copy(hrow[:, half * 512:(half + 1) * 512], ps2[:, :]), reads=[ps2], writes=[hrow])
                        self.store(HTOK[ti * 128:(ti + 1) * 128, :], hrow[:], reads=[hrow], writes=[htok_b[ti]])
                nf = self.sb(st2, "nf", [128, NE])
                ni = self.sb(st2, "ni", [128, NE], I32)
                npf = self.sb(st2, "npf", [128, NE])
                ends = self.sb(st2, "ends", [128, NE])
                cmp_ = self.sb(st2, "cmp", [128, NSLOT, NE])
                ejf = self.sb(st2, "ejf", [128, NSLOT])
                ixf = self.sb(st2, "ixf", [128, NSLOT, NCH])
                s.op("dve", lambda e: e.tensor_scalar(nf[:], cnt[:, 0:NE], float(T - 1), None, ALU.add), reads=[cnt], writes=[nf])
                self.psum_release(cnt)
                s.op("dve", lambda e: e.tensor_copy(ni[:], nf[:]), reads=[nf], writes=[ni])
                s.op("dve", lambda e: e.tensor_scalar(ni[:], ni[:], 9, 9, ALU.arith_shift_right, ALU.logical_shift_left), reads=[ni], writes=[ni])
                s.op("dve", lambda e: e.tensor_copy(npf[:], ni[:]), reads=[ni], writes=[npf])
                s.op("dve", lambda e: e.tensor_tensor_scan(ends[:], self.ones[:, 0:NE], npf[:], 0.0, ALU.mult, ALU.add),
                     reads=[self.C, npf], writes=[ends])
                s.op("dve", lambda e: e.tensor_tensor(off[:], ends[:], npf[:], ALU.subtract), reads=[ends, npf], writes=[off])
                starts = self.cstM[:, 0:NSLOT]
                s.op("dve", lambda e: e.tensor_tensor(cmp_[:], ends[:].unsqueeze(1).broadcast_to([128, NSLOT, NE]),
                                                     starts.unsqueeze(2).broadcast_to([128, NSLOT, NE]), ALU.is_le),
                     reads=[ends, self.cstMb], writes=[cmp_])
                s.op("dve", lambda e: e.tensor_reduce(ejf[:], cmp_[:], AX.X, ALU.add), reads=[cmp_], writes=[ejf])
                s.op("dve", lambda e: e.tensor_scalar(ejf[:], ejf[:], float(NE - 1), float(l * NE), ALU.min, ALU.add), reads=[ejf], writes=[ejf])
                pb = self.cstM[:, 128:136]
                s.op("dve", lambda e: e.tensor_scalar(ixf[:, :, 0], ejf[:], 128.0, self.cstM[:, 128:129], ALU.mult, ALU.add),
                     reads=[ejf, self.cstMb], writes=[ixf])
                s.op("dve", lambda e: e.tensor_copy(idxb[:], ixf[:, :, 0]), reads=[ixf], writes=[idxb])
                s.op("dve", lambda e: e.tensor_scalar(ejf[:], ejf[:], float(D), None, ALU.mult), reads=[ejf], writes=[ejf])
                s.op("dve", lambda e: e.tensor_tensor(ixf[:], ejf[:].unsqueeze(2).broadcast_to([128, NSLOT, NCH]),
                                                     pb.unsqueeze(1).broadcast_to([128, NSLOT, NCH]), ALU.add),
                     reads=[ejf, self.cstMb], writes=[ixf])
                s.op("dve", lambda e: e.tensor_copy(idxw[:], ixf[:]), reads=[ixf], writes=[idxw])
                tpr = Rot(self, st2, "tp", [128, 4 * NE], F32, 2)
                pfr = Rot(self, st2, "pf", [128, 16], F32, 2)
                SUf = self.cstM[:, 256:384]
                for ti in range(NT):
                    M = Mall[:, ti, :]
                    ps = self.psum()
                    self.mm(ps[:, 0:NE], SUf, M, start=True, stop=True, reads=[self.cstMb, Mall], writes=[ps], inc=False)
                    self.mm(ps[:, 64:64 + NE], self.ones, M, start=True, stop=True, reads=[self.C, Mall], writes=[ps], inc=True)
                    tp, pf = tpr.next(), pfr.next()
                    posf, t1, npm, tq = tp[:, 0:NE], tp[:, NE:2 * NE], tp[:, 2 * NE:3 * NE], tp[:, 3 * NE:4 * NE]
                    s.op("dve", lambda e: e.tensor_tensor(posf, ps[:, 0:NE], off[:], ALU.add), reads=[ps, off], writes=[tp])
                    s.op("dve", lambda e: e.tensor_tensor(off[:], ps[:, 64:64 + NE], off[:], ALU.add), reads=[ps, off], writes=[off])
                    s.op("dve", lambda e: e.tensor_tensor(t1, posf, M, ALU.mult), reads=[tp, Mall], writes=[tp])
                    s.op("dve", lambda e: e.tensor_scalar(npm, M, -1.0, BIG, ALU.add, ALU.mult), reads=[Mall], writes=[tp])
                    s.op("dve", lambda e: e.tensor_tensor(npm, npm, t1, ALU.subtract), reads=[tp], writes=[tp])
                    s.op("dve", lambda e: e.max(pf[:, 0:8], npm), reads=[tp], writes=[pf])
                    s.op("dve", lambda e: e.tensor_scalar(pf[:, 8:12], pf[:, 0:4], -1.0, None, ALU.mult), reads=[pf], writes=[pf])
                    s.op("dve", lambda e: e.tensor_copy(POS[:, ti, :], pf[:, 8:12]), reads=[pf], writes=[POS])
                    for k in range(TOPK):
                        s.op("dve", lambda e: e.scalar_tensor_tensor(tq, npm, pf[:, k:k + 1], gates[:, ti, :], ALU.is_equal, ALU.mult),
                             reads=[tp, pf, gates], writes=[tp])
                        s.op("dve", lambda e: e.tensor_reduce(GK[:, ti, k:k + 1], tq, AX.X, ALU.add), reads=[tp], writes=[GK])
                    hrow = hrr.next()
                    self.load(hrow[:], HTOK[ti * 128:(ti + 1) * 128, :], reads=[htok_b[ti]], writes=[hrow])
                    for k in range(TOPK):
                        s.idma(HS[:, :], bass.IndirectOffsetOnAxis(ap=POS[:, ti, k:k + 1], axis=0), hrow[:], None,
                               reads=[hrow, POS], nrows=NSLOT * T)
                s.barrier()
            with ExitStack() as st2:
                w1b = [[self.sb(st2, f"w1b{b}_{dc}", [128, 2 * D], BF16) for dc in range(NCH)] for b in range(2)]
                w2b = [[self.sb(st2, f"w2b{b}_{fc}", [128, D], BF16) for fc in range(NCH)] for b in range(2)]
                b1s = [self.sb(st2, f"b1s{b}", [128, 16]) for b in range(2)]
                stg = Rot(self, st2, "stg", [128, 2 * D], F32, 2)
                stg2 = Rot(self, st2, "stg2", [128, D], F32, 2)
                Xr = Rot(self, st2, "Xs", [128, 4, D], F32, 1)
                hTr = Rot(self, st2, "hTs", [128, NCH, 512], BF16, 1)
                ysr = Rot(self, st2, "ys", [128, 4, D], F32, 1)
                ur = Rot(self, st2, "u", [128, 512], F32, 2)
                sgr = Rot(self, st2, "sg", [128, 512], F32, 2)
                lnr = Rot(self, st2, "ln", [128, 512], F32, 2)
                actr = Rot(self, st2, "actT", [128, NCH, 512], BF16, 1)
                for j in range(NSLOT):
                    bsel = j % 2
                    s.idma(b1s[bsel][:], None, b1rows[:, :], bass.IndirectOffsetOnAxis(ap=idxb[:, j:j + 1], axis=0),
                           reads=[idxb], writes=[b1s[bsel]], nrows=cfg.DEPTH * NE * 128)
                    for dc in range(NCH):
                        sg_ = stg.next()
                        s.idma(sg_[:], None, w1rows[:, :], bass.IndirectOffsetOnAxis(ap=idxw[:, j, dc:dc + 1], axis=0),
                               reads=[idxw], writes=[sg_], nrows=cfg.DEPTH * NE * D)
                        s.op("pool", lambda e: e.tensor_copy(w1b[bsel][dc][:], sg_[:]), reads=[sg_], writes=[w1b[bsel][dc]])
                    for fc in range(NCH):
                        sg_ = stg2.next()
                        s.idma(sg_[:], None, w2rows[:, :], bass.IndirectOffsetOnAxis(ap=idxw[:, j, fc:fc + 1], axis=0),
                               reads=[idxw], writes=[sg_], nrows=cfg.DEPTH * NE * D)
                        s.op("pool", lambda e: e.tensor_copy(w2b[bsel][fc][:], sg_[:]), reads=[sg_], writes=[w2b[bsel][fc]])
                    X = Xr.next()
                    self.load(X[:], HS[j * T:(j + 1) * T, :].rearrange("(a p) f -> p a f", p=128), writes=[X])
                    hT = hTr.next()
                    for c in range(NCH):
                        ps = self.psum()
                        for sub in range(4):
                            self.tr(ps[:, sub * 128:(sub + 1) * 128], X[:, sub, c * 128:(c + 1) * 128], self.ident,
                                    reads=[X, self.C], writes=[ps], inc=(sub == 3))
                        s.op("act", lambda e: e.copy(hT[:, c, :], ps[:, :]), reads=[ps], writes=[hT])
                    actT = actr.next()
                    tb = T
                    for i in range(NCH):
                        psg, psl = self.psum(), self.psum()
                        for (pp, joff) in ((psg, 0), (psl, D)):
                            for dc in range(NCH):
                                self.mm(pp[:, 0:tb], w1b[bsel][dc][:, joff + i * 128: joff + (i + 1) * 128],
                                        hT[:, dc, 0:tb], start=(dc == 0), stop=(dc == NCH - 1),
                                        reads=[w1b[bsel][dc], hT], writes=[pp], inc=(dc == NCH - 1))
                        u, sg, ln = ur.next(), sgr.next(), lnr.next()
                        s.op("dve", lambda e: e.tensor_scalar(u[:, 0:tb], psg[:, 0:tb], b1s[bsel][:, i:i + 1], 7.0, ALU.add, ALU.min),
                             reads=[psg, b1s[bsel]], writes=[u])
                        s.op("act", lambda e: e.activation(sg[:, 0:tb], u[:, 0:tb], AF.Sigmoid, scale=1.702), reads=[u], writes=[sg])
                        s.op("dve", lambda e: e.tensor_scalar(ln[:, 0:tb], psl[:, 0:tb], b1s[bsel][:, 8 + i:9 + i], 7.0, ALU.add, ALU.min),
                             reads=[psl, b1s[bsel]], writes=[ln])
                        s.op("dve", lambda e: e.tensor_scalar(ln[:, 0:tb], ln[:, 0:tb], -7.0, 1.0, ALU.max, ALU.add), reads=[ln], writes=[ln])
                        s.op("dve", lambda e: e.tensor_tensor(u[:, 0:tb], u[:, 0:tb], sg[:, 0:tb], ALU.mult), reads=[u, sg], writes=[u])
                        s.op("dve", lambda e: e.tensor_tensor(actT[:, i, 0:tb], u[:, 0:tb], ln[:, 0:tb], ALU.mult), reads=[u, ln], writes=[actT])
                    ys = ysr.next()
                    for sub in range(4):
                        for hf in range(2):
                            ps = self.psum()
                            for fc in range(NCH):
                                self.mm(ps[:, :], actT[:, fc, sub * 128:(sub + 1) * 128], w2b[bsel][fc][:, hf * 512:(hf + 1) * 512],
                                        start=(fc == 0), stop=(fc == NCH - 1), reads=[actT, w2b[bsel][fc]], writes=[ps],
                                        inc=(fc == NCH - 1))
                            s.op("act", lambda e: e.copy(ys[:, sub, hf * 512:(hf + 1) * 512], ps[:, :]), reads=[ps], writes=[ys])
                    self.store(YS[j * T:(j + 1) * T, :].rearrange("(a p) f -> p a f", p=128), ys[:], reads=[ys])
                s.barrier()
            with ExitStack() as st2:
                accr = Rot(self, st2, "acc", [128, D], F32, 2)
                rwr = Rot(self, st2, "yrow", [128, D], F32, 4)
                gT = self.sb(st2, "gT", [NE, 128])
                xur = Rot(self, st2, "xu", [128, NCH, 128], F32, 2)
                for ti in range(NT):
                    acc = accr.next()
                    ps = self.psum()
                    self.tr(ps[0:NE, 0:128], gates[:, ti, :], self.ident, reads=[gates, self.C], writes=[ps], inc=True)
                    s.op("dve", lambda e: e.tensor_copy(gT[:], ps[0:NE, 0:128]), reads=[ps], writes=[gT])
                    for hf in range(2):
                        ps2 = self.psum()
                        self.mm(ps2[:, :], gT[:], b2[:, hf * 512:(hf + 1) * 512], start=True, stop=True,
                                reads=[gT, b2], writes=[ps2], inc=True)
                        s.op("act", lambda e: e.copy(acc[:, hf * 512:(hf + 1) * 512], ps2[:, :]), reads=[ps2], writes=[acc])
                    for k in range(TOPK):
                        yr_ = rwr.next()
                        s.idma(yr_[:], None, YS[:, :], bass.IndirectOffsetOnAxis(ap=POS[:, ti, k:k + 1], axis=0),
                               reads=[POS], writes=[yr_], nrows=NSLOT * T)
                        s.op("dve", lambda e: e.scalar_tensor_tensor(acc[:], yr_[:], GK[:, ti, k:k + 1], acc[:], ALU.mult, ALU.add),
                             reads=[yr_, GK, acc], writes=[acc])
                    col = 1 if ti * 128 < cfg.CTX else 0
                    xu = xur.next()
                    xv = fmv(self.XT)[:, :, ti * 128:(ti + 1) * 128]
                    self.load(xu[:], xv, writes=[xu])
                    for half in range(2):
                        ps = self.psum()
                        for q in range(4):
                            c = half * 4 + q
                            self.tr(ps[:, q * 128:(q + 1) * 128], acc[:, c * 128:(c + 1) * 128], self.ident,
                                    reads=[acc, self.C], writes=[ps], inc=(q == 3))
                        for q in range(4):
                            c = half * 4 + q
                            s.op("dve", lambda e: e.scalar_tensor_tensor(xu[:, c, :], ps[:, q * 128:(q + 1) * 128], self.mvec(l, col, 5, c),
                                                                        xu[:, c, :], ALU.mult, ALU.add), reads=[ps, xu, self.mod], writes=[xu])
                    self.store(xv, xu[:], reads=[xu])
                s.barrier()


def make_consts():
    c = np.zeros((128, 1024), np.float32)
    c[:, 0:128] = np.eye(128, dtype=np.float32)
    c[:, 128:256] = 1.0
    c[0:64, 256:320] = 1.0
    c[64:128, 320:384] = 1.0
    for m in range(64):
        c[(m + 16) if (m % 32) < 16 else (m - 16), 384 + m] = 1.0
    for m in range(128):
        c[(m + 32) if (m % 64) < 32 else (m - 32), 512 + m] = 1.0
    return c


def make_consts_r():
    c = np.zeros((128, 2176), np.float32)
    i = np.arange(128)
    SU = (i[:, None] < i[None, :]).astype(np.float32)
    UI = (i[:, None] <= i[None, :]).astype(np.float32)
    SL = (i[:, None] > i[None, :]).astype(np.float32)
    LI = (i[:, None] >= i[None, :]).astype(np.float32)
    c[:, 0:512] = np.concatenate([SU, UI, SU, UI], axis=1)
    c[:, 512:1024] = np.concatenate([SL, LI, SL, LI], axis=1)
    c[:, 1024:1536] = np.concatenate([SL] * 4, axis=1)
    c[:, 1536:2048] = np.concatenate([SU] * 4, axis=1)
    hs = np.zeros((128, NCH, 16), np.float32)
    for p in range(128):
        for fc in range(NCH):
            hs[p, fc, fc * 2 + p // 64] = 1.0
    c[:, 2048:2176] = hs.reshape(128, 128)
    return c


def rope_table(cfg, rot_dim):
    n, gw = cfg.SEQ, cfg.GRID_W
    rows = n // gw
    row = np.repeat(np.arange(rows, dtype=np.float32), gw)
    colp = np.tile(np.arange(gw, dtype=np.float32), rows)
    half = rot_dim // 2
    inv = (np.float32(10000.0) ** (-np.arange(0, half, 2, dtype=np.float32) / np.float32(half))).astype(np.float32)
    ar = (row[:, None] * inv[None, :]).astype(np.float32)
    ac = (colp[:, None] * inv[None, :]).astype(np.float32)
    q = rot_dim // 4
    t = np.zeros((rot_dim, 2, cfg.N), np.float32)
    t[:, 0, :cfg.CTX] = 1.0
    for blk, ang in ((0, ar), (1, ac)):
        cs, sn = np.cos(ang).T.astype(np.float32), np.sin(ang).T.astype(np.float32)
        b0 = blk * half
        t[b0:b0 + q, 0, cfg.CTX:] = cs
        t[b0 + q:b0 + 2 * q, 0, cfg.CTX:] = cs
        t[b0:b0 + q, 1, cfg.CTX:] = -sn
        t[b0 + q:b0 + 2 * q, 1, cfg.CTX:] = sn
    return t


def fm(v):
    v = np.asarray(v, np.float32)
    return np.ascontiguousarray(np.swapaxes(v.reshape(v.shape[:-1] + (NCH, 128)), -1, -2))


def prep_shared(cfg, inp):
    L, NE = cfg.DEPTH, cfg.NE
    sh = {}
    sh["ada_w"] = np.ascontiguousarray(inp["ada_w"], np.float32)
    sh["ada_bT"] = np.ascontiguousarray(np.swapaxes(np.asarray(inp["ada_b"], np.float32).reshape(L, 48, 128), 1, 2))
    sh["normT"] = np.ascontiguousarray(np.stack([fm(inp["norm_mix"]), fm(inp["norm_ffn"])], axis=2))
    sh["cst"] = make_consts()
    rw = np.asarray(inp["moe_router_w"], np.float32).reshape(L, NCH, 128, NE)
    sh["router_w"] = np.ascontiguousarray(rw.transpose(0, 2, 1, 3))
    sh["router_b"] = np.ascontiguousarray(np.broadcast_to(np.asarray(inp["moe_router_b"], np.float32)[:, None, :], (L, 128, NE)))
    w1 = np.asarray(inp["moe_w1"], np.float32)
    sh["moe_w1"] = np.ascontiguousarray(np.concatenate([w1[..., 0::2], w1[..., 1::2]], axis=-1))
    b1 = np.asarray(inp["moe_b1"], np.float32)
    b1d = np.concatenate([b1[..., 0::2], b1[..., 1::2]], axis=-1).reshape(L, NE, 16, 128)
    sh["moe_b1T"] = np.ascontiguousarray(np.swapaxes(b1d, -1, -2))
    sh["moe_w2"] = np.ascontiguousarray(inp["moe_w2"], np.float32)
    sh["moe_b2"] = np.ascontiguousarray(inp["moe_b2"], np.float32)
    NEV = max(cfg.N_EVEN, 1)
    for k in ("attn_w_in", "mla_w_uq", "mla_w_ukv", "attn_w_out"):
        sh[k] = np.ascontiguousarray(inp[k], np.float32)
    ag = np.zeros((NEV, 128, 16), np.float32)
    f32 = lambda k: np.asarray(inp[k], np.float32)
    ag[:, :, 0:3] = np.swapaxes(f32("mla_q_norm").reshape(NEV, 3, 128), 1, 2)
    ag[:, :, 3:5] = np.swapaxes(f32("mla_kv_norm").reshape(NEV, 2, 128), 1, 2)
    ag[:, :, 5] = f32("mla_qn_g")
    ag[:, 0:64, 6] = f32("mla_qr_g")
    ag[:, :, 7] = f32("mla_kn_g")
    ag[:, 0:64, 8] = f32("mla_kr_g")
    ag[:, :, 9] = f32("gqa_q_g")
    ag[:, :, 10] = f32("gqa_k_g")
    sh["attn_g"] = ag
    sh["ropeM"] = rope_table(cfg, 64)
    sh["ropeG"] = rope_table(cfg, 128)
    NOD, NVR = max(cfg.N_ODD, 1), max(cfg.N_ODD - 1, 1)

    def pad0(a, n):
        a = np.asarray(a, np.float32)
        if a.shape[0] >= n:
            return np.ascontiguousarray(a)
        return np.ascontiguousarray(np.concatenate([a, np.zeros((n - a.shape[0],) + a.shape[1:], np.float32)], axis=0))
    for dst, src in (("rw_wr", "rwkv_w_r"), ("rw_wk", "rwkv_w_k"), ("rw_wv", "rwkv_w_v"), ("rw_wo", "rwkv_w_o"),
                     ("rw_w1", "rwkv_w1"), ("rw_w2", "rwkv_w2"), ("rw_a1", "rwkv_a1"), ("rw_a2", "rwkv_a2"),
                     ("rw_g1", "rwkv_g1"), ("rw_g2", "rwkv_g2")):
        sh[dst] = pad0(inp[src], NOD)
    sh["rw_v1"] = pad0(inp["rwkv_v1"], NVR)
    sh["rw_v2"] = pad0(inp["rwkv_v2"], NVR)
    J = cfg.N_ODD
    rp = np.zeros((NOD, 128, 16, NCH), np.float32)
    bc = np.zeros((NOD, 3, 128, D), np.float32)
    if J > 0:
        rp[:J, :, 0:6, :] = np.swapaxes(fm(inp["rwkv_mix"]), 1, 2)
        rp[:J, :, 6:8, :] = np.swapaxes(fm(inp["rwkv_w0"]), 1, 2)
        rp[:J, :, 8:10, :] = np.swapaxes(fm(inp["rwkv_a0"]), 1, 2)
        rp[:J, :, 10, :] = fm(inp["rwkv_k_k"])
        rp[:J, :, 11, :] = fm(inp["rwkv_k_a"])
        rp[:J, :, 13, :] = fm(np.asarray(inp["rwkv_r_k"], np.float32).reshape(J, D))
        bc[:J, 0] = np.asarray(inp["rwkv_ln_w"], np.float32)[:, None, :]
        bc[:J, 1] = np.asarray(inp["rwkv_ln_b"], np.float32)[:, None, :]
        for jj in range(1, J):
            bc[jj, 2] = np.asarray(inp["rwkv_v0"], np.float32)[jj - 1][None, :]
    sh["rw_p"] = rp
    sh["rw_bc"] = bc
    sh["cstR"] = make_consts_r()
    cm = np.zeros((128, 384), np.float32)
    cm[:, 0:128] = (np.arange(128, dtype=np.float32) * 512.0)[None, :]
    cm[:, 128:136] = np.arange(128, dtype=np.float32)[:, None] + 128.0 * np.arange(8, dtype=np.float32)[None, :]
    ii = np.arange(128)
    cm[:, 256:384] = (ii[:, None] < ii[None, :]).astype(np.float32)
    sh["cstM"] = cm
    return sh


def prep_core(cfg, inp, b):
    pc = {}
    xT = np.concatenate([np.asarray(inp["ctx"][b], np.float32).T, np.asarray(inp["x"][b], np.float32).T], axis=1)
    pc["xT0"] = np.ascontiguousarray(xT)
    cond = np.stack([fm(inp["c"][b]), fm(inp["c_ctx"])], axis=1)
    pc["condT"] = np.ascontiguousarray(cond)
    return pc


def run(cfg, inp, n_cores, debug=()):
    P = Prog(cfg, debug=debug)
    nc = P.build()
    sh = prep_shared(cfg, inp)
    in_maps = []
    for b in range(n_cores):
        m = dict(sh)
        m.update(prep_core(cfg, inp, b))
        for k, (shape, dt) in P.inputs.items():
            assert tuple(m[k].shape) == shape, (k, m[k].shape, shape)
        in_maps.append({k: m[k] for k in P.inputs})
    res = run_bass_kernel_spmd(nc, in_maps, core_ids=list(range(n_cores)))
    return res, P


def kernel(**inputs):
    cfg = Cfg()
    res, P = run(cfg, inputs, 8)
    out = np.stack([np.ascontiguousarray(r["outT"].T) for r in res.results], axis=0)
    return out.astype(np.float32)
```

```python
import numpy as np
from contextlib import ExitStack
import concourse.bass as bass
import concourse.mybir as mybir
from concourse.bass_utils import run_bass_kernel_spmd

F32 = mybir.dt.float32
F32R = mybir.dt.float32r
BF16 = mybir.dt.bfloat16
AF = mybir.ActivationFunctionType
ALU = mybir.AluOpType
AX = mybir.AxisListType

D = 1024
NCH = 8
EPOCH = 12000


class Buf:
    __slots__ = ("name", "ap", "w", "r", "excl")

    def __init__(self, name, ap=None, excl=False):
        self.name = name
        self.ap = ap
        self.w = None
        self.r = []
        self.excl = excl

    def __getitem__(self, idx):
        return self.ap[idx]


class Sched:
    CE = ("pe", "act", "dve", "pool")

    def __init__(self, nc, n_dma_slots=8):
        self.nc = nc
        self.eng = {"pe": nc.tensor, "act": nc.scalar, "dve": nc.vector,
                    "pool": nc.gpsimd, "sp": nc.sync}
        self.sem, self.semcnt, self.gidx, self.nep = {}, {}, {}, {}
        for e in self.CE:
            self.sem[e] = nc.alloc_semaphore(f"s_{e}_0")
            self.semcnt[e] = 0
            self.gidx[e] = 0
            self.nep[e] = 0
        self.seen = {e: {} for e in self.eng}
        self.slots = {}
        for q in ("sp", "pool"):
            self.slots[q] = [[nc.alloc_semaphore(f"d_{q}_{i}"), 0, None] for i in range(n_dma_slots)]
        self.slot_i = {q: 0 for q in self.slots}
        self.dma_gid = 0
        self.last_tok = {e: None for e in self.CE}
        self.pending = {e: False for e in self.CE}
        self.ninstr = {e: 0 for e in self.eng}

    def _next_tok(self, e):
        return ("c", e, self.gidx[e] + 1, self.sem[e], self.semcnt[e] + 1)

    def _wait(self, e, tok):
        if tok is None:
            return
        kind, x, g, sem, val = tok
        if kind == "c":
            if self.seen[e].get(x, 0) >= g:
                return
            self.seen[e][x] = g
        else:
            if self.seen[e].get(("d", g)):
                return
            self.seen[e][("d", g)] = True
        self.eng[e].wait_ge(sem, val)
        self.ninstr[e] += 1

    def _deps(self, e, reads, writes):
        for b in reads:
            self._wait(e, b.w)
            if b.excl:
                for t in b.r:
                    if not (t[0] == "c" and t[1] == e):
                        self._wait(e, t)
        for b in writes:
            if b.w is not None and not (b.w[0] == "c" and b.w[1] == e):
                self._wait(e, b.w)
            for t in b.r:
                if t[0] == "c" and t[1] == e:
                    continue
                self._wait(e, t)

    def op(self, e, fn, reads=(), writes=(), inc=True):
        self._deps(e, reads, writes)
        tok = self._next_tok(e)
        ins = fn(self.eng[e])
        self.ninstr[e] += 1
        if inc:
            ins.then_inc(self.sem[e], 1)
            self.gidx[e] += 1
            self.semcnt[e] += 1
            self.last_tok[e] = tok
            self.pending[e] = False
        else:
            self.pending[e] = True
        for b in reads:
            b.r = [t for t in b.r if not (t[0] == "c" and t[1] == e)]
            b.r.append(tok)
        for b in writes:
            b.w = tok
            b.r = []
        if inc and self.semcnt[e] >= EPOCH:
            self.nep[e] += 1
            self.sem[e] = self.nc.alloc_semaphore(f"s_{e}_{self.nep[e]}")
            self.semcnt[e] = 0
        return ins

    def dma(self, q, out, in_, reads=(), writes=(), **kw):
        e = q
        self._deps(e, reads, writes)
        sl = self.slots[q][self.slot_i[q]]
        self.slot_i[q] = (self.slot_i[q] + 1) % len(self.slots[q])
        if sl[2] is not None:
            self._wait(e, sl[2])
        ins = self.eng[e].dma_start(out=out, in_=in_, **kw)
        self.ninstr[e] += 1
        sl[1] += 16
        ins.then_inc(sl[0], 16)
        self.dma_gid += 1
        tok = ("d", q, self.dma_gid, sl[0], sl[1])
        sl[2] = tok
        for b in reads:
            b.r.append(tok)
        for b in writes:
            b.w = tok
            b.r = []
        return tok

    def idma(self, out, out_offset, in_, in_offset, reads=(), writes=(), nrows=None):
        e = "pool"
        self._deps(e, reads, writes)
        sl = self.slots[e][self.slot_i[e]]
        self.slot_i[e] = (self.slot_i[e] + 1) % len(self.slots[e])
        if sl[2] is not None:
            self._wait(e, sl[2])
        kw = {}
        if nrows is not None:
            if not hasattr(self, "_bregs"):
                self._bregs = {}
            if nrows not in self._bregs:
                self._bregs[nrows] = self.eng[e].to_reg(nrows - 1)
            kw = dict(bounds_check=self._bregs[nrows], oob_is_err=False)
        ins = self.eng[e].indirect_dma_start(out=out, out_offset=out_offset, in_=in_, in_offset=in_offset, **kw)
        self.ninstr[e] += 1
        sl[1] += 16
        ins.then_inc(sl[0], 16)
        self.dma_gid += 1
        tok = ("d", e, self.dma_gid, sl[0], sl[1])
        sl[2] = tok
        for b in reads:
            b.r.append(tok)
        for b in writes:
            b.w = tok
            b.r = []
        return tok

    def barrier(self):
        toks = []
        for e in self.CE:
            assert not self.pending[e], f"engine {e} has un-inc'd trailing instructions"
            if self.last_tok[e] is not None:
                toks.append(self.last_tok[e])
        for q in self.slots:
            for sl in self.slots[q]:
                if sl[2] is not None:
                    toks.append(sl[2])
        for e in self.eng:
            for t in toks:
                if t[0] == "c" and t[1] == e:
                    continue
                self._wait(e, t)


class Rot:
    def __init__(self, P, stack, name, shape, dtype, n):
        self.bufs = []
        for i in range(n):
            t = stack.enter_context(P.nc.sbuf_tensor(f"{name}{i}_{P.uid()}", list(shape), dtype))
            self.bufs.append(Buf(f"{name}{i}", t))
        self.i = 0

    def next(self):
        b = self.bufs[self.i]
        self.i = (self.i + 1) % len(self.bufs)
        return b


class Cfg:
    def __init__(self, SEQ=4096, CTX=256, NE=32, DEPTH=4, TOPK=4, GRID_W=64):
        self.SEQ, self.CTX, self.NE, self.DEPTH, self.TOPK, self.GRID_W = SEQ, CTX, NE, DEPTH, TOPK, GRID_W
        self.N = SEQ + CTX
        assert CTX % 128 == 0 and CTX <= 512 and SEQ % 512 == 0
        self.NT = self.N // 128
        self.blocks = [(0, CTX, 1)] + [(CTX + i * 512, 512, 0) for i in range(SEQ // 512)]
        self.N_EVEN = (DEPTH + 1) // 2
        self.N_ODD = DEPTH // 2


class Prog:
    def __init__(self, cfg, debug=()):
        self.cfg = cfg
        self.debug = set(debug)
        self.nc = bass.Bass("TRN2", target_bir_lowering=False)
        self.s = Sched(self.nc)
        self._uid = 0
        self.inputs = {}
        self.root = ExitStack()
        nc = self.nc
        self.ps = [Buf(f"ps{i}", nc.alloc_psum_tensor(f"ps{i}", [128, 512], F32).ap(), excl=True) for i in range(8)]
        self.ps_i = 0
        self.ps_held = []
        self.f32r = False

    def uid(self):
        self._uid += 1
        return self._uid

    def psum(self):
        while True:
            b = self.ps[self.ps_i]
            self.ps_i = (self.ps_i + 1) % 8
            if b not in self.ps_held:
                return b

    def psum_hold(self):
        b = self.psum()
        self.ps_held.append(b)
        return b

    def psum_release(self, b):
        self.ps_held.remove(b)

    def dram_in(self, name, shape, dtype=F32):
        t = self.nc.dram_tensor(name, list(shape), dtype, kind="ExternalInput").ap()
        self.inputs[name] = (tuple(shape), dtype)
        return t

    def dram_scratch(self, name, shape, dtype=F32):
        kind = "ExternalOutput" if name in self.debug else "Internal"
        return self.nc.dram_tensor(name, list(shape), dtype, kind=kind).ap()

    def sb(self, stack, name, shape, dtype=F32):
        t = stack.enter_context(self.nc.sbuf_tensor(f"{name}_{self.uid()}", list(shape), dtype))
        return Buf(name, t)

    def mm(self, out, lhsT, rhs, start, stop, reads=(), writes=(), inc=False, **kw):
        if self.f32r and lhsT.dtype == F32:
            lhsT, rhs = lhsT.bitcast(F32R), rhs.bitcast(F32R)
        return self.s.op("pe", lambda e: e.matmul(out, lhsT, rhs, start=start, stop=stop, **kw),
                         reads=reads, writes=writes, inc=inc)

    def tr(self, out, in_, ident, reads=(), writes=(), inc=False):
        return self.s.op("pe", lambda e: e.transpose(out, in_, ident), reads=reads, writes=writes, inc=inc)

    def load(self, out_ap, in_ap, writes, reads=(), **kw):
        return self.s.dma("sp", out_ap, in_ap, reads=reads, writes=writes, **kw)

    def store(self, out_ap, in_ap, reads, writes=(), **kw):
        return self.s.dma("pool", out_ap, in_ap, reads=reads, writes=writes, **kw)

    def build(self):
        cfg, nc, s = self.cfg, self.nc, self.s
        L, N, NE = cfg.DEPTH, cfg.N, cfg.NE
        R = self.root
        self.xT0 = self.dram_in("xT0", [D, N])
        self.condT = self.dram_in("condT", [128, 2, NCH])
        self.ada_w = self.dram_in("ada_w", [L, D, 6 * D])
        self.ada_bT = self.dram_in("ada_bT", [L, 128, 48])
        self.normT = self.dram_in("normT", [L, 128, 2, NCH])
        self.cst = self.dram_in("cst", [128, 1024])
        self.router_w = self.dram_in("router_w", [L, 128, NCH, NE])
        self.router_b = self.dram_in("router_b", [L, 128, NE])
        self.w1 = self.dram_in("moe_w1", [L, NE, D, 2 * D])
        self.b1T = self.dram_in("moe_b1T", [L, NE, 128, 16])
        self.w2 = self.dram_in("moe_w2", [L, NE, D, D])
        self.b2 = self.dram_in("moe_b2", [L, NE, D])
        NEV = max(cfg.N_EVEN, 1)
        self.attn_w_in = self.dram_in("attn_w_in", [NEV, D, 1728])
        self.mla_w_uq = self.dram_in("mla_w_uq", [NEV, 384, 768])
        self.mla_w_ukv = self.dram_in("mla_w_ukv", [NEV, 256, 1024])
        self.attn_w_out = self.dram_in("attn_w_out", [NEV, D, D])
        self.attn_g = self.dram_in("attn_g", [NEV, 128, 16])
        self.ropeM = self.dram_in("ropeM", [64, 2, N])
        self.ropeG = self.dram_in("ropeG", [128, 2, N])
        NOD, NVR = max(cfg.N_ODD, 1), max(cfg.N_ODD - 1, 1)
        for nm in ("rw_wr", "rw_wk", "rw_wv", "rw_wo"):
            setattr(self, nm, self.dram_in(nm, [NOD, D, D]))
        self.rw_w1 = self.dram_in("rw_w1", [NOD, 2, D, 64])
        self.rw_w2 = self.dram_in("rw_w2", [NOD, 2, 64, D])
        self.rw_a1 = self.dram_in("rw_a1", [NOD, 2, D, 64])
        self.rw_a2 = self.dram_in("rw_a2", [NOD, 2, 64, D])
        self.rw_g1 = self.dram_in("rw_g1", [NOD, D, 128])
        self.rw_g2 = self.dram_in("rw_g2", [NOD, 128, D])
        self.rw_v1 = self.dram_in("rw_v1", [NVR, D, 32])
        self.rw_v2 = self.dram_in("rw_v2", [NVR, 32, D])
        self.rw_p = self.dram_in("rw_p", [NOD, 128, 16, NCH])
        self.rw_bc = self.dram_in("rw_bc", [NOD, 3, 128, D])
        self.cstR = self.dram_in("cstR", [128, 2176])
        self.cstM_d = self.dram_in("cstM", [128, 384])
        self.outT = self.nc.dram_tensor("outT", [D, cfg.SEQ], F32, kind="ExternalOutput").ap()
        self.XT = self.dram_scratch("XT", [D, N])
        self.C = self.sb(R, "cst", [128, 1024])
        self.ident = self.C[:, 0:128]
        self.ones = self.C[:, 128:256]
        self.blk1 = self.C[:, 256:384]
        self.onesb = self.sb(R, "onesb", [128, 128], BF16)
        self.mod = self.sb(R, "mod", [128, L, 2, 64])
        self.normS = self.sb(R, "normS", [128, L, 2, NCH])
        self.load(self.C[:], self.cst[:, :], writes=[self.C])
        self.cstMb = self.sb(R, "cstM", [128, 384])
        self.cstM = self.cstMb.ap
        self.load(self.cstMb[:], self.cstM_d[:, :], writes=[self.cstMb])
        self.eps_t = self.sb(R, "eps", [128, 1])
        s.op("dve", lambda e: e.memset(self.eps_t[:], 1e-6), writes=[self.eps_t])
        self.eps_ap = self.eps_t[:, 0:1]
        s.op("dve", lambda e: e.tensor_copy(self.onesb[:], self.ones), reads=[self.C], writes=[self.onesb])
        self.load(self.normS[:], self.normT.rearrange("l p a c -> p l a c"), writes=[self.normS])

        self.phase_mods()
        s.dma("sp", self.XT[:, :], self.xT0[:, :])
        s.barrier()
        for l in range(L):
            self.layer(l)
        s.dma("sp", self.outT[:, :], self.XT[:, cfg.CTX:cfg.N])
        s.barrier()
        self.root.close()
        return nc

    def mvec(self, l, col, j, c):
        k = j * 8 + c
        return self.mod[:, l, col, k:k + 1]

    def phase_mods(self):
        cfg, nc, s = self.cfg, self.nc, self.s
        L = cfg.DEPTH
        with ExitStack() as st:
            cond = self.sb(st, "cond", [128, 2, NCH])
            scond = self.sb(st, "scond", [128, NCH, 2])
            abT = self.sb(st, "abT", [128, L, 48])
            wrot = Rot(self, st, "adaw", [128, 6 * D], F32, 2)
            self.load(cond[:], self.condT[:, :, :], writes=[cond])
            self.load(abT[:], self.ada_bT.rearrange("l p k -> p l k"), writes=[abT])
            for col in range(2):
                s.op("act", lambda e, col=col: e.activation(scond[:, :, col], cond[:, col, :], AF.Silu),
                     reads=[cond], writes=[scond])
            for l in range(L):
                ps = self.psum()
                s.op("dve", lambda e: e.memset(ps[:, 0:96], 0.0), writes=[ps])
                for dc in range(NCH):
                    wt = wrot.next()
                    self.load(wt[:], self.ada_w[l, dc * 128:(dc + 1) * 128, :], writes=[wt])
                    for j in range(48):
                        self.mm(ps[:, 2 * j:2 * j + 2], wt[:, j * 128:(j + 1) * 128], scond[:, dc, :],
                                start=False, stop=False, reads=[wt, scond], writes=[ps],
                                inc=(j == 47), skip_group_check=True)
                for col in range(2):
                    s.op("dve", lambda e, col=col: e.tensor_tensor(
                        self.mod[:, l, col, 0:48], ps[:, col:96:2], abT[:, l, :], ALU.add),
                        reads=[ps, abT], writes=[self.mod])
                    for (dst, jsc, nrm) in ((48, 1, 0), (56, 4, 1)):
                        s.op("dve", lambda e, col=col, dst=dst, jsc=jsc, nrm=nrm: e.scalar_tensor_tensor(
                            self.mod[:, l, col, dst:dst + 8], self.mod[:, l, col, jsc * 8:jsc * 8 + 8], 1.0,
                            self.normS[:, l, nrm, :], ALU.add, ALU.mult),
                            reads=[self.mod, self.normS], writes=[self.mod])
            s.barrier()

    def norm_block(self, st_pools, l, which, t0, tb, col, out):
        s = self.s
        xr, sqr, rsr, tr_ = st_pools
        x32 = xr.next()
        self.load(x32[:, :, 0:tb], self.XT.rearrange("(c p) t -> p c t", p=128)[:, :, t0:t0 + tb], writes=[x32])
        sq = sqr.next()
        s.op("act", lambda e: e.activation(sq[:, :, 0:tb], x32[:, :, 0:tb], AF.Square), reads=[x32], writes=[sq])
        ps = self.psum()
        for c in range(NCH):
            self.mm(ps[:, 0:tb], self.onesb[:], sq[:, c, 0:tb], start=(c == 0), stop=(c == NCH - 1),
                    reads=[sq, self.onesb], writes=[ps], inc=(c == NCH - 1))
        rs = rsr.next()
        s.op("act", lambda e: e.activation(rs[:, 0:tb], ps[:, 0:tb], AF.Sqrt, bias=self.eps_ap, scale=1.0 / D),
             reads=[ps], writes=[rs])
        s.op("dve", lambda e: e.reciprocal(rs[:, 0:tb], rs[:, 0:tb]), reads=[rs], writes=[rs])
        jg, jsft = (6, 0) if which == 0 else (7, 3)
        for c in range(NCH):
            tmp = tr_.next()
            s.op("dve", lambda e: e.tensor_tensor(tmp[:, 0:tb], x32[:, c, 0:tb], rs[:, 0:tb], ALU.mult),
                 reads=[x32, rs], writes=[tmp])
            s.op("act", lambda e: e.activation(out[:, c, 0:tb], tmp[:, 0:tb], AF.Identity,
                                               bias=self.mvec(l, col, jsft, c), scale=self.mvec(l, col, jg, c)),
                 reads=[tmp, self.mod], writes=[out])
        return out

    def layer(self, l):
        cfg = self.cfg
        if "nomix" not in self.debug:
            if l % 2 == 0:
                self.phase_attn(l)
            else:
                self.phase_rwkv(l)
        self.phase_moe(l)

    def cast_load(self, dst_buf, dst_ap, src_ap, stg, eng="pool"):
        sg_ = stg.next()
        X = src_ap.shape[-1]
        self.load(sg_[:, 0:X], src_ap, writes=[sg_])
        self.s.op(eng, lambda e: e.tensor_copy(dst_ap, sg_[:, 0:X]), reads=[sg_], writes=[dst_buf])

    def phase_attn(self, l):
        cfg, nc, s = self.cfg, self.nc, self.s
        N, NT, CTX = cfg.N, cfg.NT, cfg.CTX
        ea = l // 2
        QT = self.dram_scratch(f"QT{l}", [12 * 128, N], BF16)
        KT = self.dram_scratch(f"KT{l}", [7 * 128, N], BF16)
        VT = self.dram_scratch(f"VT{l}", [N, 768], BF16)
        QTv = QT.rearrange("(s p) t -> p s t", p=128)
        KTv = KT.rearrange("(s p) t -> p s t", p=128)
        MLA_SCALE = 192.0 ** -0.5
        GQA_SCALE = 128.0 ** -0.5
        with ExitStack() as st:
            win = self.sb(st, "win", [128, NCH, 1728], BF16)
            wuq = self.sb(st, "wuq", [128, 3, 768], BF16)
            wukv = self.sb(st, "wukv", [128, 2, 1024], BF16)
            ag = self.sb(st, "ag", [128, 16])
            self.load(ag[:], self.attn_g[ea], writes=[ag])
            with ExitStack() as st0:
                stg = Rot(self, st0, "stgA", [128, 1728], F32, 2)
                for dc in range(NCH):
                    self.cast_load(win, win[:, dc, :], self.attn_w_in[ea, dc * 128:(dc + 1) * 128, :], stg)
                for rc in range(3):
                    self.cast_load(wuq, wuq[:, rc, :], self.mla_w_uq[ea, rc * 128:(rc + 1) * 128, :], stg)
                for rc in range(2):
                    self.cast_load(wukv, wukv[:, rc, :], self.mla_w_ukv[ea, rc * 128:(rc + 1) * 128, :], stg)
                s.barrier()
            pools = (Rot(self, st, "x32", [128, NCH, 512], F32, 1), Rot(self, st, "sq", [128, NCH, 512], BF16, 1),
                     Rot(self, st, "rs", [128, 512], F32, 2), Rot(self, st, "nt", [128, 512], F32, 2))
            hbr = Rot(self, st, "hb", [128, NCH, 512], BF16, 2)
            zsr = Rot(self, st, "zs", [128, 3, 512], F32, 1)
            zqr = Rot(self, st, "zsq", [128, 3, 512], BF16, 1)
            znr = Rot(self, st, "zn", [128, 3, 512], BF16, 2)
            xsr = Rot(self, st, "xs", [128, 512], F32, 3)
            sqr = Rot(self, st, "sq1", [128, 512], BF16, 2)
            rsr = Rot(self, st, "rs1", [128, 512], F32, 2)
            yr = Rot(self, st, "y", [128, 512], F32, 3)
            t1r = Rot(self, st, "t1", [128, 512], F32, 2)
            t2r = Rot(self, st, "t2", [128, 512], F32, 2)
            rMr = Rot(self, st, "rM", [64, 2, 512], F32, 2)
            rGr = Rot(self, st, "rG", [128, 2, 512], F32, 2)
            qbr = Rot(self, st, "qblk", [128, 12, 512], BF16, 1)
            kbr = Rot(self, st, "kblk", [128, 7, 512], BF16, 1)
            vbr = Rot(self, st, "vblk", [128, 4, 768], BF16, 1)
            for b_ in qbr.bufs + kbr.bufs:
                s.op("pool", lambda e: e.memset(b_[:], 0.0), writes=[b_])
            permM = self.C[0:64, 384:448]
            permG = self.C[:, 512:640]

            for (t0, tb, col) in cfg.blocks:
                hb = self.norm_block(pools, l, 0, t0, tb, col, hbr.next())
                rM, rG = rMr.next(), rGr.next()
                self.load(rM[:, :, 0:tb], self.ropeM[:, :, t0:t0 + tb], writes=[rM])
                self.load(rG[:, :, 0:tb], self.ropeG[:, :, t0:t0 + tb], writes=[rG])
                qblk, kblk, vblk = qbr.next(), kbr.next(), vbr.next()

                def proj(ps, M, w, col0, rhs, nk):
                    for dc in range(nk):
                        self.mm(ps[0:M, 0:tb], w[:, dc, col0:col0 + M], rhs[:, dc, 0:tb], start=(dc == 0),
                                stop=(dc == nk - 1), reads=[w, rhs], writes=[ps], inc=(dc == nk - 1))

                def rms_multi(nchunk, col0, gcol0):
                    zs, zsq, zn = zsr.next(), zqr.next(), znr.next()
                    for c in range(nchunk):
                        ps = self.psum()
                        proj(ps, 128, win, col0 + c * 128, hb, NCH)
                        s.op("act", lambda e: e.copy(zs[:, c, 0:tb], ps[:, 0:tb]), reads=[ps], writes=[zs])
                        s.op("act", lambda e: e.activation(zsq[:, c, 0:tb], ps[:, 0:tb], AF.Square), reads=[ps], writes=[zsq])
                    ps2 = self.psum()
                    for c in range(nchunk):
                        self.mm(ps2[:, 0:tb], self.onesb[:], zsq[:, c, 0:tb], start=(c == 0), stop=(c == nchunk - 1),
                                reads=[zsq, self.onesb], writes=[ps2], inc=(c == nchunk - 1))
                    rs = rsr.next()
                    s.op("act", lambda e: e.activation(rs[:, 0:tb], ps2[:, 0:tb], AF.Sqrt, bias=self.eps_ap,
                                                       scale=1.0 / (nchunk * 128)), reads=[ps2], writes=[rs])
                    s.op("dve", lambda e: e.reciprocal(rs[:, 0:tb], rs[:, 0:tb]), reads=[rs], writes=[rs])
                    for c in range(nchunk):
                        s.op("dve", lambda e: e.scalar_tensor_tensor(zn[:, c, 0:tb], zs[:, c, 0:tb], ag[:, gcol0 + c:gcol0 + c + 1],
                                                                    rs[:, 0:tb], ALU.mult, ALU.mult),
                             reads=[zs, ag, rs], writes=[zn])
                    return zn

                def rms_feat(ps, M, gcol, dst_buf=None, dst_ap=None):
                    xs, sq1, rs = xsr.next(), sqr.next(), rsr.next()
                    s.op("act", lambda e: e.copy(xs[0:M, 0:tb], ps[0:M, 0:tb]), reads=[ps], writes=[xs])
                    s.op("act", lambda e: e.activation(sq1[0:M, 0:tb], ps[0:M, 0:tb], AF.Square), reads=[ps], writes=[sq1])
                    ps2 = self.psum()
                    self.mm(ps2[0:M, 0:tb], self.onesb[0:M, 0:M], sq1[0:M, 0:tb], start=True, stop=True,
                            reads=[sq1, self.onesb], writes=[ps2], inc=True)
                    s.op("act", lambda e: e.activation(rs[0:M, 0:tb], ps2[0:M, 0:tb], AF.Sqrt, bias=self.eps_t[0:M, 0:1],
                                                       scale=1.0 / M), reads=[ps2], writes=[rs])
                    s.op("dve", lambda e: e.reciprocal(rs[0:M, 0:tb], rs[0:M, 0:tb]), reads=[rs], writes=[rs])
                    if dst_buf is None:
                        y = yr.next()
                        dst_buf, dst_ap = y, y[0:M, 0:tb]
                    s.op("dve", lambda e: e.scalar_tensor_tensor(dst_ap, xs[0:M, 0:tb], ag[0:M, gcol:gcol + 1], rs[0:M, 0:tb],
                                                                ALU.mult, ALU.mult), reads=[xs, ag, rs], writes=[dst_buf])
                    return dst_buf

                def rope(y, M, perm, rt, dst_buf, dst_ap):
                    ps = self.psum()
                    self.mm(ps[0:M, 0:tb], perm, y[0:M, 0:tb], start=True, stop=True, reads=[y, self.C], writes=[ps], inc=True)
                    t1, t2 = t1r.next(), t2r.next()
                    s.op("pool", lambda e: e.tensor_tensor(t1[0:M, 0:tb], y[0:M, 0:tb], rt[0:M, 0, 0:tb], ALU.mult),
                         reads=[y, rt], writes=[t1])
                    s.op("dve", lambda e: e.tensor_tensor(t2[0:M, 0:tb], ps[0:M, 0:tb], rt[0:M, 1, 0:tb], ALU.mult),
                         reads=[ps, rt], writes=[t2])
                    s.op("dve", lambda e: e.tensor_tensor(dst_ap, t1[0:M, 0:tb], t2[0:M, 0:tb], ALU.add),
                         reads=[t1, t2], writes=[dst_buf])

                zqn = rms_multi(3, 0, 0)
                for h in range(4):
                    ps = self.psum()
                    proj(ps, 128, wuq, h * 192, zqn, 3)
                    rms_feat(ps, 128, 5, qblk, qblk[:, h, 0:tb])
                    ps = self.psum()
                    proj(ps, 64, wuq, h * 192 + 128, zqn, 3)
                    y = rms_feat(ps, 64, 6)
                    rope(y, 64, permM, rM, qblk, qblk[0:64, 4 + h, 0:tb])
                zkvn = rms_multi(2, 384, 3)
                for h in range(4):
                    ps = self.psum()
                    proj(ps, 128, wukv, h * 256, zkvn, 2)
                    rms_feat(ps, 128, 7, kblk, kblk[:, h, 0:tb])
                ps = self.psum()
                proj(ps, 64, win, 640, hb, NCH)
                y = rms_feat(ps, 64, 8)
                rope(y, 64, permM, rM, kblk, kblk[0:64, 4, 0:tb])
                for h in range(4):
                    ps = self.psum()
                    proj(ps, 128, win, 704 + h * 128, hb, NCH)
                    y = rms_feat(ps, 128, 9)
                    rope(y, 128, permG, rG, qblk, qblk[:, 8 + h, 0:tb])
                for g in range(2):
                    ps = self.psum()
                    proj(ps, 128, win, 1216 + g * 128, hb, NCH)
                    y = rms_feat(ps, 128, 10)
                    rope(y, 128, permG, rG, kblk, kblk[:, 5 + g, 0:tb])
                for sub in range(tb // 128):
                    ps = self.psum()
                    for h in range(4):
                        for rc in range(2):
                            self.mm(ps[:, h * 128:(h + 1) * 128], zkvn[:, rc, sub * 128:(sub + 1) * 128],
                                    wukv[:, rc, h * 256 + 128:h * 256 + 256], start=(rc == 0), stop=(rc == 1),
                                    reads=[zkvn, wukv], writes=[ps], inc=(h == 3 and rc == 1))
                    s.op("act", lambda e: e.copy(vblk[:, sub, 0:512], ps[:, :]), reads=[ps], writes=[vblk])
                    ps = self.psum()
                    for dc in range(NCH):
                        self.mm(ps[:, 0:256], hb[:, dc, sub * 128:(sub + 1) * 128], win[:, dc, 1472:1728],
                                start=(dc == 0), stop=(dc == NCH - 1), reads=[hb, win], writes=[ps], inc=(dc == NCH - 1))
                    s.op("act", lambda e: e.copy(vblk[:, sub, 512:768], ps[:, 0:256]), reads=[ps], writes=[vblk])
                nsub = tb // 128
                self.store(QTv[:, :, t0:t0 + tb], qblk[:, :, 0:tb], reads=[qblk])
                self.store(KTv[:, :, t0:t0 + tb], kblk[:, :, 0:tb], reads=[kblk])
                self.store(VT[t0:t0 + tb, :].rearrange("(a p) f -> p a f", p=128), vblk[:, 0:nsub, :], reads=[vblk])
            s.barrier()
        with ExitStack() as st:
            Kr = self.sb(st, "Kres", [128, 7, N], BF16)
            Vr = self.sb(st, "Vres", [128, NT, 768], BF16)
            wo = self.sb(st, "wo", [128, NCH, D], BF16)
            stg = Rot(self, st, "stgB", [128, D], F32, 2)
            self.load(Kr[:], KTv, writes=[Kr])
            self.load(Vr[:], VT.rearrange("(a p) f -> p a f", p=128), writes=[Vr])
            for hc in range(NCH):
                self.cast_load(wo, wo[:, hc, :], self.attn_w_out[ea, hc * 128:(hc + 1) * 128, :], stg)
            qr_ = Rot(self, st, "qin", [128, 12, 512], BF16, 1)
            ptr = Rot(self, st, "pT", [128, 512], BF16, 4)
            aor = Rot(self, st, "ao", [128, NCH, 512], BF16, 1)
            rdr = Rot(self, st, "rden", [128, 512], F32, 2)
            xur = Rot(self, st, "xa", [128, NCH, 512], F32, 1)
            for (t0, tb, col) in cfg.blocks:
                nkt = (CTX // 128) if col == 1 else NT
                qin = qr_.next()
                self.load(qin[:, :, 0:tb], QTv[:, :, t0:t0 + tb], writes=[qin])
                ao = aor.next()
                for hd in range(8):
                    pso, psd = self.psum_hold(), self.psum_hold()
                    if hd < 4:
                        scale, vs = MLA_SCALE, hd
                    else:
                        scale, vs = GQA_SCALE, 4 + (hd - 4) // 2

                    def scores(kt):
                        ks = slice(kt * 128, (kt + 1) * 128)
                        ps = self.psum()
                        if hd < 4:
                            self.mm(ps[:, 0:tb], Kr[:, hd, ks], qin[:, hd, 0:tb], start=True, stop=False,
                                    reads=[Kr, qin], writes=[ps], inc=False)
                            self.mm(ps[:, 0:tb], Kr[0:64, 4, ks], qin[0:64, 4 + hd, 0:tb], start=False, stop=True,
                                    reads=[Kr, qin], writes=[ps], inc=True)
                        else:
                            self.mm(ps[:, 0:tb], Kr[:, 5 + (hd - 4) // 2, ks], qin[:, 8 + hd - 4, 0:tb], start=True, stop=True,
                                    reads=[Kr, qin], writes=[ps], inc=True)
                        pT = ptr.next()
                        s.op("act", lambda e: e.activation(pT[:, 0:tb], ps[:, 0:tb], AF.Exp, scale=scale), reads=[ps], writes=[pT])
                        return pT

                    pend = [scores(0)]
                    if nkt > 1:
                        pend.append(scores(1))
                    for kt in range(nkt):
                        pT = pend.pop(0)
                        if kt + 2 < nkt:
                            pend.append(scores(kt + 2))
                        self.mm(pso[:, 0:tb], Vr[:, kt, vs * 128:(vs + 1) * 128], pT[:, 0:tb], start=(kt == 0), stop=(kt == nkt - 1),
                                reads=[Vr, pT], writes=[pso], inc=False)
                        self.mm(psd[:, 0:tb], self.onesb[:], pT[:, 0:tb], start=(kt == 0), stop=(kt == nkt - 1),
                                reads=[self.onesb, pT], writes=[psd], inc=True)
                    rd = rdr.next()
                    s.op("dve", lambda e: e.reciprocal(rd[:, 0:tb], psd[:, 0:tb]), reads=[psd], writes=[rd])
                    s.op("dve", lambda e: e.tensor_tensor(ao[:, hd, 0:tb], pso[:, 0:tb], rd[:, 0:tb], ALU.mult),
                         reads=[pso, rd], writes=[ao])
                    self.psum_release(pso)
                    self.psum_release(psd)
                xa = xur.next()
                xv = self.XT.rearrange("(c p) t -> p c t", p=128)[:, :, t0:t0 + tb]
                self.load(xa[:, :, 0:tb], xv, writes=[xa])
                for fc in range(NCH):
                    ps = self.psum()
                    for hd in range(8):
                        self.mm(ps[:, 0:tb], wo[:, hd, fc * 128:(fc + 1) * 128], ao[:, hd, 0:tb], start=(hd == 0), stop=(hd == 7),
                                reads=[wo, ao], writes=[ps], inc=(hd == 7))
                    s.op("dve", lambda e: e.scalar_tensor_tensor(xa[:, fc, 0:tb], ps[:, 0:tb], self.mvec(l, col, 2, fc),
                                                                xa[:, fc, 0:tb], ALU.mult, ALU.add),
                         reads=[ps, xa, self.mod], writes=[xa])
                self.store(xv, xa[:, :, 0:tb], reads=[xa])
            s.barrier()

    def phase_rwkv(self, l):
        cfg, nc, s = self.cfg, self.nc, self.s
        N, NT, CTX = cfg.N, cfg.NT, cfg.CTX
        j = l // 2
        C0 = 0.6065306597126334
        fmv = lambda a: a.rearrange("(c p) t -> p c t", p=128)
        HT = self.dram_scratch(f"HTr{l}", [D, N])
        RT = self.dram_scratch(f"RT{l}", [D, N])
        SG = [self.dram_scratch(f"SG{l}_{d}", [D, N]) for d in range(2)]
        KD = [self.dram_scratch(f"KD{l}_{d}", [D, N]) for d in range(2)]
        BD = [self.dram_scratch(f"BD{l}_{d}", [D, N]) for d in range(2)]
        NKK = self.dram_scratch(f"NKK{l}", [D, N])
        GT = self.dram_scratch(f"GT{l}", [D, N], BF16)
        VTOK = self.dram_scratch(f"VTOK{l}", [N, D])
        RK = self.dram_scratch(f"RK{l}", [N, 16])
        YD = [self.dram_scratch(f"YD{l}_{d}", [N, D]) for d in range(2)]
        if j == 0:
            self.VRAW = self.dram_scratch("VRAW", [N, D])
        with ExitStack() as st:
            pools = (Rot(self, st, "x32", [128, NCH, 512], F32, 2), Rot(self, st, "sq", [128, NCH, 512], BF16, 2),
                     Rot(self, st, "rs", [128, 512], F32, 2), Rot(self, st, "nt", [128, 512], F32, 3))
            h32r = Rot(self, st, "h32", [128, NCH, 512], F32, 2)
            for (t0, tb, col) in cfg.blocks:
                h32 = self.norm_block(pools, l, 0, t0, tb, col, h32r.next())
                self.store(fmv(HT)[:, :, t0:t0 + tb], h32[:, :, 0:tb], reads=[h32])
            s.barrier()
        if "stopA0" in self.debug:
            return
        with ExitStack() as st:
            wr = self.sb(st, "wr", [128, NCH, D], BF16)
            wk = self.sb(st, "wk", [128, NCH, D], BF16)
            wv = self.sb(st, "wv", [128, NCH, D], BF16)
            w1b = [self.sb(st, f"w1b{d}", [128, NCH, 64], BF16) for d in range(2)]
            a1b = [self.sb(st, f"a1b{d}", [128, NCH, 64], BF16) for d in range(2)]
            w2b = [self.sb(st, f"w2b{d}", [64, D], BF16) for d in range(2)]
            a2b = [self.sb(st, f"a2b{d}", [64, D], BF16) for d in range(2)]
            g1b = self.sb(st, "g1b", [128, NCH, 128], BF16)
            g2b = self.sb(st, "g2b", [128, D], BF16)
            v1b = self.sb(st, "v1b", [128, NCH, 32], BF16)
            v2b = self.sb(st, "v2b", [32, D], BF16)
            pp = self.sb(st, "rwp", [128, 16, NCH])
            v0bc = self.sb(st, "v0bc", [128, D])
            hsel = self.sb(st, "hsel", [128, NCH, 16])
            self.load(pp[:], self.rw_p[j], writes=[pp])
            self.load(hsel[:], self.cstR[:, 2048:2176].rearrange("p (c h) -> p c h", c=NCH), writes=[hsel])
            s.op("dve", lambda e: e.tensor_scalar(pp[:, 12, :], pp[:, 11, :], -1.0, 1.0, ALU.mult, ALU.add), reads=[pp], writes=[pp])
            s.op("dve", lambda e: e.tensor_scalar(pp[:, 14, :], pp[:, 10, :], -1.0, None, ALU.mult), reads=[pp], writes=[pp])
            pv = lambda i, c: pp[:, i, c:c + 1]
            with ExitStack() as st0:
                stg = Rot(self, st0, "stgR", [128, D], F32, 2)
                for (dst, src) in ((wr, self.rw_wr), (wk, self.rw_wk), (wv, self.rw_wv)):
                    for dc in range(NCH):
                        self.cast_load(dst, dst[:, dc, :], src[j, dc * 128:(dc + 1) * 128, :], stg)
                for d in range(2):
                    for (dst, src) in ((w1b[d], self.rw_w1), (a1b[d], self.rw_a1)):
                        sg_ = stg.next()
                        self.load(sg_[:, 0:512].rearrange("p (c m) -> p c m", c=NCH),
                                  src[j, d].rearrange("(c p) m -> p c m", p=128), writes=[sg_])
                        s.op("pool", lambda e: e.tensor_copy(dst[:], sg_[:, 0:512].rearrange("p (c m) -> p c m", c=NCH)),
                             reads=[sg_], writes=[dst])
                    for (dst, src) in ((w2b[d], self.rw_w2), (a2b[d], self.rw_a2)):
                        sg_ = stg.next()
                        self.load(sg_[0:64, :], src[j, d], writes=[sg_])
                        s.op("pool", lambda e: e.tensor_copy(dst[:], sg_[0:64, :]), reads=[sg_], writes=[dst])
                sg_ = stg.next()
                self.load(sg_[:, :].rearrange("p (c m) -> p c m", c=NCH), self.rw_g1[j].rearrange("(c p) m -> p c m", p=128), writes=[sg_])
                s.op("pool", lambda e: e.tensor_copy(g1b[:], sg_[:, :].rearrange("p (c m) -> p c m", c=NCH)), reads=[sg_], writes=[g1b])
                self.cast_load(g2b, g2b[:], self.rw_g2[j], stg)
                if j > 0:
                    sg_ = stg.next()
                    self.load(sg_[:, 0:256].rearrange("p (c m) -> p c m", c=NCH),
                              self.rw_v1[j - 1].rearrange("(c p) m -> p c m", p=128), writes=[sg_])
                    s.op("pool", lambda e: e.tensor_copy(v1b[:], sg_[:, 0:256].rearrange("p (c m) -> p c m", c=NCH)),
                         reads=[sg_], writes=[v1b])
                    sg_ = stg.next()
                    self.load(sg_[0:32, :], self.rw_v2[j - 1], writes=[sg_])
                    s.op("pool", lambda e: e.tensor_copy(v2b[:], sg_[0:32, :]), reads=[sg_], writes=[v2b])
                    self.load(v0bc[:], self.rw_bc[j, 2], writes=[v0bc])
                s.barrier()
            TB = 256
            hhr = Rot(self, st, "hh", [128, NCH, TB + 2], F32, 2)
            xxr = Rot(self, st, "xx", [128, NCH, TB], F32, 1)
            xmr = Rot(self, st, "xm", [128, NCH, TB], BF16, 2)
            rTr = Rot(self, st, "rT", [128, NCH, TB], F32, 1)
            kTr = Rot(self, st, "kT", [128, NCH, TB], F32, 1)
            ksr = Rot(self, st, "ksum", [128, NCH, TB], F32, 1)
            o2r = Rot(self, st, "o2", [128, TB], F32, 6)
            nkr = Rot(self, st, "nkk", [128, TB], F32, NCH)
            lor = Rot(self, st, "lo", [128, TB], BF16, 3)
            vtr = Rot(self, st, "vt", [128, 2, D], F32, 1)
            vfr = Rot(self, st, "vf", [128, 2, D], F32, 1)
            sgr = Rot(self, st, "vsg", [128, 512], F32, 2)
            gbr = Rot(self, st, "gb", [128, NCH, TB], BF16, 1)
            rkr = Rot(self, st, "rk", [128, 2, 16], F32, 2)
            blocks = []
            for (s0, s1, col) in ((0, CTX, 1), (CTX, N, 0)):
                t = s0
                while t < s1:
                    tb = min(TB, s1 - t)
                    blocks.append((t, tb, col, s0, s1))
                    t += tb
            for (t0, tb, col, s0, s1) in blocks:
                hh = hhr.next()
                s.op("pool", lambda e: e.memset(hh[:, :, :], 0.0), writes=[hh])
                lo, hi = max(t0 - 1, s0), min(t0 + tb + 1, s1)
                off = lo - (t0 - 1)
                self.load(hh[:, :, off:off + hi - lo], fmv(HT)[:, :, lo:hi], writes=[hh])
                hc = hh[:, :, 1:tb + 1]
                xx = xxr.next()
                s.op("dve", lambda e: e.tensor_tensor(xx[:, :, 0:tb], hh[:, :, 0:tb], hh[:, :, 2:tb + 2], ALU.add), reads=[hh], writes=[xx])
                s.op("dve", lambda e: e.scalar_tensor_tensor(xx[:, :, 0:tb], xx[:, :, 0:tb], 0.5, hc, ALU.mult, ALU.subtract),
                     reads=[xx, hh], writes=[xx])

                def mix(m):
                    xm = xmr.next()
                    for c in range(NCH):
                        s.op("dve", lambda e: e.scalar_tensor_tensor(xm[:, c, 0:tb], xx[:, c, 0:tb], pv(m, c), hh[:, c, 1:tb + 1],
                                                                    ALU.mult, ALU.add), reads=[xx, pp, hh], writes=[xm])
                    return xm

                def proj_fm(w, xm, fc, M=128, col0=None):
                    ps = self.psum()
                    c0_ = fc * 128 if col0 is None else col0
                    for dc in range(NCH):
                        self.mm(ps[0:M, 0:tb], w[:, dc, c0_:c0_ + M], xm[:, dc, 0:tb], start=(dc == 0), stop=(dc == NCH - 1),
                                reads=[w, xm], writes=[ps], inc=(dc == NCH - 1))
                    return ps

                xm = mix(0)
                rT = rTr.next()
                for fc in range(NCH):
                    ps = proj_fm(wr, xm, fc)
                    s.op("act", lambda e: e.copy(rT[:, fc, 0:tb], ps[:, 0:tb]), reads=[ps], writes=[rT])
                self.store(fmv(RT)[:, :, t0:t0 + tb], rT[:, :, 0:tb], reads=[rT])
                xm = mix(2)
                kT = kTr.next()
                for fc in range(NCH):
                    ps = proj_fm(wk, xm, fc)
                    s.op("act", lambda e: e.copy(kT[:, fc, 0:tb], ps[:, 0:tb]), reads=[ps], writes=[kT])
                nkk = []
                for fc in range(NCH):
                    sq = o2r.next()
                    s.op("act", lambda e: e.activation(sq[:, 0:tb], kT[:, fc, 0:tb], AF.Square, scale=pv(10, fc)), reads=[kT, pp], writes=[sq])
                    ps = self.psum()
                    self.mm(ps[:, 0:tb], self.blk1, sq[:, 0:tb], start=True, stop=True, reads=[self.C, sq], writes=[ps], inc=True)
                    rn = o2r.next()
                    s.op("dve", lambda e: e.tensor_scalar(rn[:, 0:tb], ps[:, 0:tb], 1e-24, None, ALU.max), reads=[ps], writes=[rn])
                    s.op("act", lambda e: e.activation(rn[:, 0:tb], rn[:, 0:tb], AF.Sqrt), reads=[rn], writes=[rn])
                    s.op("dve", lambda e: e.reciprocal(rn[:, 0:tb], rn[:, 0:tb]), reads=[rn], writes=[rn])
                    nk = nkr.next()
                    s.op("dve", lambda e: e.scalar_tensor_tensor(nk[:, 0:tb], kT[:, fc, 0:tb], pv(14, fc), rn[:, 0:tb], ALU.mult, ALU.mult),
                         reads=[kT, pp, rn], writes=[nk])
                    self.store(NKK[fc * 128:(fc + 1) * 128, t0:t0 + tb], nk[:, 0:tb], reads=[nk])
                    nkk.append(nk)
                    for d in range(2):
                        pass
                xm = mix(1)
                for d in range(2):
                    ps = proj_fm(w1b[d], xm, 0, M=64, col0=0)
                    lw = lor.next()
                    s.op("act", lambda e: e.activation(lw[0:64, 0:tb], ps[0:64, 0:tb], AF.Tanh), reads=[ps], writes=[lw])
                    for fc in range(NCH):
                        ps2 = self.psum()
                        self.mm(ps2[:, 0:tb], w2b[d][0:64, fc * 128:(fc + 1) * 128], lw[0:64, 0:tb], start=True, stop=True,
                                reads=[w2b[d], lw], writes=[ps2], inc=True)
                        sg = o2r.next()
                        s.op("act", lambda e: e.activation(sg[:, 0:tb], ps2[:, 0:tb], AF.Sigmoid, bias=pv(6 + d, fc)), reads=[ps2, pp], writes=[sg])
                        self.store(SG[d][fc * 128:(fc + 1) * 128, t0:t0 + tb], sg[:, 0:tb], reads=[sg])
                xm = mix(4)
                ksum = ksr.next()
                for d in range(2):
                    ps = proj_fm(a1b[d], xm, 0, M=64, col0=0)
                    la = lor.next()
                    s.op("act", lambda e: e.copy(la[0:64, 0:tb], ps[0:64, 0:tb]), reads=[ps], writes=[la])
                    for fc in range(NCH):
                        ps2 = self.psum()
                        self.mm(ps2[:, 0:tb], a2b[d][0:64, fc * 128:(fc + 1) * 128], la[0:64, 0:tb], start=True, stop=True,
                                reads=[a2b[d], la], writes=[ps2], inc=True)
                        a_ = o2r.next()
                        s.op("act", lambda e: e.activation(a_[:, 0:tb], ps2[:, 0:tb], AF.Sigmoid, bias=pv(8 + d, fc)), reads=[ps2, pp], writes=[a_])
                        kd = o2r.next()
                        s.op("dve", lambda e: e.tensor_scalar(kd[:, 0:tb], a_[:, 0:tb], pv(11, fc), pv(12, fc), ALU.mult, ALU.add),
                             reads=[a_, pp], writes=[kd])
                        s.op("dve", lambda e: e.tensor_tensor(kd[:, 0:tb], kd[:, 0:tb], kT[:, fc, 0:tb], ALU.mult), reads=[kd, kT], writes=[kd])
                        self.store(KD[d][fc * 128:(fc + 1) * 128, t0:t0 + tb], kd[:, 0:tb], reads=[kd])
                        if d == 0:
                            s.op("pool", lambda e: e.tensor_copy(ksum[:, fc, 0:tb], kd[:, 0:tb]), reads=[kd], writes=[ksum])
                        else:
                            s.op("pool", lambda e: e.tensor_tensor(ksum[:, fc, 0:tb], ksum[:, fc, 0:tb], kd[:, 0:tb], ALU.add),
                                 reads=[kd, ksum], writes=[ksum])
                        bt_ = o2r.next()
                        s.op("dve", lambda e: e.scalar_tensor_tensor(bt_[:, 0:tb], nkk[fc][:, 0:tb], -1.0, a_[:, 0:tb], ALU.mult, ALU.mult),
                             reads=[nkk[fc], a_], writes=[bt_])
                        self.store(BD[d][fc * 128:(fc + 1) * 128, t0:t0 + tb], bt_[:, 0:tb], reads=[bt_])
                for fc in range(NCH):
                    s.op("dve", lambda e: e.scalar_tensor_tensor(ksum[:, fc, 0:tb], ksum[:, fc, 0:tb], pv(13, fc), rT[:, fc, 0:tb],
                                                                ALU.mult, ALU.mult), reads=[ksum, pp, rT], writes=[ksum])
                rk = rkr.next()
                for sub in range(tb // 128):
                    ps = self.psum()
                    for fc in range(NCH):
                        self.mm(ps[:, 0:16], ksum[:, fc, sub * 128:(sub + 1) * 128], hsel[:, fc, :], start=(fc == 0), stop=(fc == NCH - 1),
                                reads=[ksum, hsel], writes=[ps], inc=(fc == NCH - 1))
                    s.op("act", lambda e: e.copy(rk[:, sub, :], ps[:, 0:16]), reads=[ps], writes=[rk])
                self.store(RK[t0:t0 + tb, :].rearrange("(a p) h -> p a h", p=128), rk[:, 0:tb // 128, :], reads=[rk])
                xm = mix(3)
                vt = vtr.next()
                for sub in range(tb // 128):
                    for hf in range(2):
                        ps = self.psum()
                        for dc in range(NCH):
                            self.mm(ps[:, :], xm[:, dc, sub * 128:(sub + 1) * 128], wv[:, dc, hf * 512:(hf + 1) * 512],
                                    start=(dc == 0), stop=(dc == NCH - 1), reads=[xm, wv], writes=[ps], inc=(dc == NCH - 1))
                        s.op("act", lambda e: e.copy(vt[:, sub, hf * 512:(hf + 1) * 512], ps[:, :]), reads=[ps], writes=[vt])
                nsub = tb // 128
                tokv = lambda a: a[t0:t0 + tb, :].rearrange("(a p) f -> p a f", p=128)
                if j == 0:
                    self.store(tokv(self.VRAW), vt[:, 0:nsub, :], reads=[vt])
                else:
                    vf = vfr.next()
                    self.load(vf[:, 0:nsub, :], tokv(self.VRAW), writes=[vf])
                    ps = proj_fm(v1b, xm, 0, M=32, col0=0)
                    l32 = lor.next()
                    s.op("act", lambda e: e.copy(l32[0:32, 0:tb], ps[0:32, 0:tb]), reads=[ps], writes=[l32])
                    for sub in range(nsub):
                        for hf in range(2):
                            hs = slice(hf * 512, (hf + 1) * 512)
                            ps2 = self.psum()
                            self.mm(ps2[:, :], l32[0:32, sub * 128:(sub + 1) * 128], v2b[0:32, hs], start=True, stop=True,
                                    reads=[l32, v2b], writes=[ps2], inc=True)
                            sg = sgr.next()
                            s.op("dve", lambda e: e.tensor_tensor(sg[:, :], ps2[:, :], v0bc[:, hs], ALU.add), reads=[ps2, v0bc], writes=[sg])
                            s.op("act", lambda e: e.activation(sg[:, :], sg[:, :], AF.Sigmoid), reads=[sg], writes=[sg])
                            s.op("dve", lambda e: e.tensor_tensor(vf[:, sub, hs], vf[:, sub, hs], vt[:, sub, hs], ALU.subtract), reads=[vf, vt], writes=[vf])
                            s.op("dve", lambda e: e.tensor_tensor(vf[:, sub, hs], vf[:, sub, hs], sg[:, :], ALU.mult), reads=[vf, sg], writes=[vf])
                            s.op("dve", lambda e: e.tensor_tensor(vt[:, sub, hs], vt[:, sub, hs], vf[:, sub, hs], ALU.add), reads=[vf, vt], writes=[vt])
                self.store(tokv(VTOK), vt[:, 0:nsub, :], reads=[vt])
                xm = mix(5)
                ps = proj_fm(g1b, xm, 0, M=128, col0=0)
                lg = lor.next()
                s.op("act", lambda e: e.activation(lg[:, 0:tb], ps[:, 0:tb], AF.Sigmoid), reads=[ps], writes=[lg])
                gb = gbr.next()
                for fc in range(NCH):
                    ps2 = self.psum()
                    self.mm(ps2[:, 0:tb], g2b[:, fc * 128:(fc + 1) * 128], lg[:, 0:tb], start=True, stop=True, reads=[g2b, lg], writes=[ps2], inc=True)
                    s.op("act", lambda e: e.copy(gb[:, fc, 0:tb], ps2[:, 0:tb]), reads=[ps2], writes=[gb])
                self.store(fmv(GT)[:, :, t0:t0 + tb], gb[:, :, 0:tb], reads=[gb])
            s.barrier()
        if "stopA1" in self.debug:
            return
        self.f32r = "f32r" in self.debug
        with ExitStack() as st:
            msk = self.sb(st, "msk", [128, 2048])
            self.load(msk[:], self.cstR[:, 0:2048], writes=[msk])
            ST = self.sb(st, "ST", [128, NCH, 64])
            onesf = self.ones
            ldr = {k: Rot(self, st, f"ld{k}", [128, NCH, 128], F32, 2) for k in ("r", "sg", "kd", "nkk", "bd")}
            vr_ = Rot(self, st, "ldv", [128, D], F32, 2)
            csr = Rot(self, st, "cs", [128, NCH, 128], F32, 1)
            Pr = Rot(self, st, "P", [128, NCH, 128], F32, 1)
            Pir = Rot(self, st, "Pi", [128, NCH, 128], F32, 1)
            Pxr = Rot(self, st, "Px", [128, NCH, 128], F32, 1)
            ARr = Rot(self, st, "AR", [128, NCH, 256], F32, 1)
            BTr = Rot(self, st, "BT", [128, NCH, 128], F32, 1)
            KTr = Rot(self, st, "KTt", [128, NCH, 128], F32, 1)
            Btr = Rot(self, st, "Btok", [128, D], F32, 1)
            Ktr = Rot(self, st, "Ktok", [128, D], F32, 1)
            Mh = [self.sb(st, f"Mh{h}", [128, 512]) for h in range(16)]
            A4 = [[self.sb(st, f"A4_{g}_{i}", [128, 512]) for i in range(2)] for g in range(4)]
            AT4 = [[self.sb(st, f"AT4_{g}_{i}", [128, 512]) for i in range(2)] for g in range(4)]
            TT4 = [self.sb(st, f"TT4_{g}", [128, 512]) for g in range(4)]
            Wsb = self.sb(st, "Wsb", [128, D])
            Usb = self.sb(st, "Usb", [128, D])
            Ysb = self.sb(st, "Ysb", [128, D])
            pcb = self.sb(st, "pcb", [128, NCH, 64])
            tot = self.sb(st, "tot", [128, NCH])
            stmp = self.sb(st, "stmp", [128, NCH, 64])
            nctx = CTX // 128
            for d in range(2):
                order = list(range(NT)) if d == 0 else (list(range(nctx - 1, -1, -1)) + list(range(NT - 1, nctx - 1, -1)))
                s.op("dve", lambda e: e.memset(ST[:], 0.0), writes=[ST])
                m512 = msk[:, d * 512:(d + 1) * 512]
                mA4 = msk[:, 1024 + d * 512:1024 + (d + 1) * 512]
                for ci in order:
                    ts_ = slice(ci * 128, (ci + 1) * 128)
                    ld = {k: ldr[k].next() for k in ldr}
                    for k, src in (("r", RT), ("sg", SG[d]), ("kd", KD[d]), ("nkk", NKK), ("bd", BD[d])):
                        self.load(ld[k][:], fmv(src)[:, :, ts_], writes=[ld[k]])
                    V = vr_.next()
                    self.load(V[:], VTOK[ts_, :], writes=[V])
                    cs, P_, Pi, Px = csr.next(), Pr.next(), Pir.next(), Pxr.next()
                    for fc in range(NCH):
                        s.op("dve", lambda e: e.tensor_tensor_scan(cs[:, fc, :], onesf, ld["sg"][:, fc, :], 0.0, ALU.mult, ALU.add),
                             reads=[self.C, ld["sg"]], writes=[cs])
                    if d == 1:
                        s.op("dve", lambda e: e.tensor_copy(tot[:], cs[:, :, 127]), reads=[cs], writes=[tot])
                        for fc in range(NCH):
                            s.op("dve", lambda e: e.scalar_tensor_tensor(cs[:, fc, :], cs[:, fc, :], -1.0, ld["sg"][:, fc, :], ALU.mult, ALU.add),
                                 reads=[cs, ld["sg"]], writes=[cs])
                            s.op("dve", lambda e: e.tensor_scalar(cs[:, fc, :], cs[:, fc, :], tot[:, fc:fc + 1], None, ALU.add),
                                 reads=[cs, tot], writes=[cs])
                    last = 127 if d == 0 else 0
                    s.op("act", lambda e: e.activation(P_[:], cs[:], AF.Exp, scale=-C0), reads=[cs], writes=[P_])
                    s.op("act", lambda e: e.activation(Pi[:], cs[:], AF.Exp, scale=C0), reads=[cs], writes=[Pi])
                    s.op("dve", lambda e: e.tensor_tensor(Px[:], cs[:], ld["sg"][:], ALU.subtract), reads=[cs, ld["sg"]], writes=[Px])
                    s.op("act", lambda e: e.activation(Px[:], Px[:], AF.Exp, scale=-C0), reads=[Px], writes=[Px])
                    AR, BT, KTt = ARr.next(), BTr.next(), KTr.next()
                    s.op("dve", lambda e: e.tensor_tensor(AR[:, :, 0:128], ld["nkk"][:], Px[:], ALU.mult), reads=[ld["nkk"], Px], writes=[AR])
                    s.op("pool", lambda e: e.tensor_tensor(AR[:, :, 128:256], ld["r"][:], P_[:], ALU.mult), reads=[ld["r"], P_], writes=[AR])
                    s.op("dve", lambda e: e.tensor_tensor(BT[:], ld["bd"][:], Pi[:], ALU.mult), reads=[ld["bd"], Pi], writes=[BT])
                    s.op("pool", lambda e: e.tensor_tensor(KTt[:], ld["kd"][:], Pi[:], ALU.mult), reads=[ld["kd"], Pi], writes=[KTt])
                    for fc in range(NCH):
                        s.op("dve", lambda e: e.tensor_scalar(pcb[:, fc, :], onesf[:, 0:64], P_[:, fc, last:last + 1], None, ALU.mult),
                             reads=[self.C, P_], writes=[pcb])
                    if "B1" in self.debug:
                        continue
                    Btok, Ktok = Btr.next(), Ktr.next()
                    for (src, dst) in ((BT, Btok), (KTt, Ktok)):
                        for half in range(2):
                            ps = self.psum()
                            for q in range(4):
                                self.tr(ps[:, q * 128:(q + 1) * 128], src[:, half * 4 + q, :], self.ident, reads=[src, self.C], writes=[ps], inc=(q == 3))
                            s.op("act", lambda e: e.copy(dst[:, half * 512:(half + 1) * 512], ps[:, :]), reads=[ps], writes=[dst])
                    if "B2" in self.debug:
                        continue
                    for h in range(16):
                        fc, sl = h // 2, slice((h % 2) * 64, (h % 2) * 64 + 64)
                        ps = self.psum()
                        self.mm(ps[:, 0:256], BT[sl, fc, :], AR[sl, fc, :], start=True, stop=True, reads=[BT, AR], writes=[ps], inc=False)
                        self.mm(ps[:, 256:512], KTt[sl, fc, :], AR[sl, fc, :], start=True, stop=True, reads=[KTt, AR], writes=[ps], inc=True)
                        s.op("dve", lambda e: e.tensor_tensor(Mh[h][:], ps[:, :], m512, ALU.mult), reads=[ps, msk], writes=[Mh[h]])
                    if "B3" in self.debug:
                        continue
                    cur = [0, 0, 0, 0]
                    hof = lambda g, q: (g // 2) * 8 + 2 * q + (g % 2)
                    for g in range(4):
                        ps = self.psum()
                        for q in range(4):
                            h = hof(g, q)
                            fc, sl = h // 2, slice((h % 2) * 64, (h % 2) * 64 + 64)
                            self.mm(ps[:, q * 128:(q + 1) * 128], AR[sl, fc, 0:128], BT[sl, fc, :], start=True, stop=True,
                                    reads=[AR, BT], writes=[ps], inc=(q == 3))
                        s.op("dve", lambda e: e.tensor_tensor(A4[g][0][:], ps[:, :], mA4, ALU.mult), reads=[ps, msk], writes=[A4[g][0]])
                        for q in range(4):
                            h = hof(g, q)
                            s.op("pool", lambda e: e.tensor_copy(AT4[g][0][:, q * 128:(q + 1) * 128], Mh[h][:, 0:128]), reads=[Mh[h]], writes=[AT4[g][0]])
                            s.op("pool", lambda e: e.tensor_tensor(TT4[g][:, q * 128:(q + 1) * 128], Mh[h][:, 0:128], self.ident, ALU.add),
                                 reads=[Mh[h], self.C], writes=[TT4[g]])
                    if "B4" in self.debug:
                        continue
                    for lev in range(6):
                        for g in range(4):
                            A_, AT_ = A4[g][cur[g]], AT4[g][cur[g]]
                            An, ATn = A4[g][1 - cur[g]], AT4[g][1 - cur[g]]
                            psA = self.psum()
                            for q in range(4):
                                qs = slice(q * 128, (q + 1) * 128)
                                self.mm(psA[:, qs], AT_[:, qs], A_[:, qs], start=True, stop=True, reads=[A_, AT_], writes=[psA], inc=(q == 3))
                            s.op("act", lambda e: e.copy(An[:], psA[:, :]), reads=[psA], writes=[An])
                            if lev < 5:
                                psT = self.psum()
                                for q in range(4):
                                    qs = slice(q * 128, (q + 1) * 128)
                                    self.mm(psT[:, qs], A_[:, qs], AT_[:, qs], start=True, stop=True, reads=[A_, AT_], writes=[psT], inc=(q == 3))
                                s.op("act", lambda e: e.copy(ATn[:], psT[:, :]), reads=[psT], writes=[ATn])
                            psU = self.psum()
                            for q in range(4):
                                qs = slice(q * 128, (q + 1) * 128)
                                self.mm(psU[:, qs], An[:, qs], TT4[g][:, qs], start=True, stop=True, reads=[An, TT4[g]], writes=[psU], inc=(q == 3))
                            s.op("dve", lambda e: e.tensor_tensor(TT4[g][:], psU[:, :], TT4[g][:], ALU.add), reads=[psU, TT4[g]], writes=[TT4[g]])
                            cur[g] = 1 - cur[g]
                    if "B5" in self.debug:
                        continue
                    hp = lambda h: (h // 2, slice((h % 2) * 64, (h % 2) * 64 + 64), slice(h * 64, (h + 1) * 64))
                    for b in range(2):
                        ps = self.psum()
                        for q in range(8):
                            h = b * 8 + q
                            fc, sl, hs = hp(h)
                            qs = slice(q * 64, (q + 1) * 64)
                            self.mm(ps[:, qs], AR[sl, fc, 0:128], ST[sl, fc, :], start=True, stop=False, reads=[AR, ST], writes=[ps], inc=False)
                            self.mm(ps[:, qs], Mh[h][:, 256:384], V[:, hs], start=False, stop=True, reads=[Mh[h], V], writes=[ps], inc=(q == 7))
                        s.op("act", lambda e: e.copy(Wsb[:, b * 512:(b + 1) * 512], ps[:, :]), reads=[ps], writes=[Wsb])
                    for b in range(2):
                        ps = self.psum()
                        for q in range(8):
                            h = b * 8 + q
                            g, qq = 2 * (h // 8) + (h % 2), (h % 8) // 2
                            self.mm(ps[:, q * 64:(q + 1) * 64], TT4[g][:, qq * 128:(qq + 1) * 128], Wsb[:, h * 64:(h + 1) * 64],
                                    start=True, stop=True, reads=[TT4[g], Wsb], writes=[ps], inc=(q == 7))
                        s.op("act", lambda e: e.copy(Usb[:, b * 512:(b + 1) * 512], ps[:, :]), reads=[ps], writes=[Usb])
                    for b in range(2):
                        ps = self.psum()
                        for q in range(8):
                            h = b * 8 + q
                            fc, sl, hs = hp(h)
                            qs = slice(q * 64, (q + 1) * 64)
                            self.mm(ps[:, qs], AR[sl, fc, 128:256], ST[sl, fc, :], start=True, stop=False, reads=[AR, ST], writes=[ps], inc=False)
                            self.mm(ps[:, qs], Mh[h][:, 128:256], Usb[:, hs], start=False, stop=False, reads=[Mh[h], Usb], writes=[ps], inc=False)
                            self.mm(ps[:, qs], Mh[h][:, 384:512], V[:, hs], start=False, stop=True, reads=[Mh[h], V], writes=[ps], inc=(q == 7))
                        s.op("act", lambda e: e.copy(Ysb[:, b * 512:(b + 1) * 512], ps[:, :]), reads=[ps], writes=[Ysb])
                    self.store(YD[d][ts_, :], Ysb[:], reads=[Ysb])
                    for b in range(2):
                        ps = self.psum()
                        for q in range(8):
                            h = b * 8 + q
                            fc, sl, hs = hp(h)
                            qs = slice(q * 64, (q + 1) * 64)
                            self.mm(ps[:, qs], Btok[:, fc * 128:(fc + 1) * 128], Usb[:, hs], start=True, stop=False, reads=[Btok, Usb], writes=[ps], inc=False)
                            self.mm(ps[:, qs], Ktok[:, fc * 128:(fc + 1) * 128], V[:, hs], start=False, stop=True, reads=[Ktok, V], writes=[ps], inc=(q == 7))
                        psv = ps[:, :].rearrange("p (f two v) -> p f two v", two=2, v=64)
                        for par in range(2):
                            sl = slice(par * 64, par * 64 + 64)
                            fcs = slice(b * 4, b * 4 + 4)
                            s.op("dve", lambda e: e.tensor_tensor(stmp[sl, fcs, :], psv[sl, :, par, :], ST[sl, fcs, :], ALU.add),
                                 reads=[ps, ST], writes=[stmp])
                            s.op("dve", lambda e: e.tensor_tensor(ST[sl, fcs, :], stmp[sl, fcs, :], pcb[sl, fcs, :], ALU.mult),
                                 reads=[stmp, pcb], writes=[ST])
            s.barrier()
        self.f32r = False
        if "stopB" in self.debug:
            return
        with ExitStack() as st:
            wo = self.sb(st, "rwo", [128, NCH, D], BF16)
            lnw = self.sb(st, "lnw", [128, D])
            lnb = self.sb(st, "lnb", [128, D])
            self.load(lnw[:], self.rw_bc[j, 0], writes=[lnw])
            self.load(lnb[:], self.rw_bc[j, 1], writes=[lnb])
            with ExitStack() as st0:
                stg = Rot(self, st0, "stgO", [128, D], F32, 2)
                for dc in range(NCH):
                    self.cast_load(wo, wo[:, dc, :], self.rw_wo[j, dc * 128:(dc + 1) * 128, :], stg)
                s.barrier()
            y0r = Rot(self, st, "y0", [128, D], F32, 2)
            y1r = Rot(self, st, "y1", [128, D], F32, 2)
            vr_ = Rot(self, st, "vC", [128, D], F32, 2)
            rkr = Rot(self, st, "rkC", [128, 16], F32, 2)
            str_ = Rot(self, st, "stat", [128, 64], F32, 2)
            sqr = Rot(self, st, "sqC", [128, D], F32, 1)
            gtr = Rot(self, st, "gtC", [128, NCH, 512], BF16, 1)
            zgr = Rot(self, st, "zgT", [128, NCH, 512], BF16, 1)
            xur = Rot(self, st, "xC", [128, NCH, 512], F32, 1)
            h3 = lambda a: a.rearrange("p (h n) -> p h n", n=64)
            for (t0, tb, col) in cfg.blocks:
                gt, zg = gtr.next(), zgr.next()
                self.load(gt[:, :, 0:tb], fmv(GT)[:, :, t0:t0 + tb], writes=[gt])
                for sub in range(tb // 128):
                    ts_ = slice(t0 + sub * 128, t0 + (sub + 1) * 128)
                    y0, y1, v, rk, sa, sq = y0r.next(), y1r.next(), vr_.next(), rkr.next(), str_.next(), sqr.next()
                    self.load(y0[:], YD[0][ts_, :], writes=[y0])
                    self.load(y1[:], YD[1][ts_, :], writes=[y1])
                    self.load(v[:], VTOK[ts_, :], writes=[v])
                    self.load(rk[:], RK[ts_, :], writes=[rk])
                    s.op("dve", lambda e: e.tensor_tensor(y0[:], y0[:], y1[:], ALU.add), reads=[y0, y1], writes=[y0])
                    s.op("dve", lambda e: e.tensor_reduce(sa[:, 0:16], h3(y0[:]), AX.X, ALU.add), reads=[y0], writes=[sa])
                    s.op("dve", lambda e: e.tensor_scalar(sa[:, 0:16], sa[:, 0:16], 1.0 / 64, None, ALU.mult), reads=[sa], writes=[sa])
                    s.op("dve", lambda e: e.tensor_tensor(h3(y0[:]), h3(y0[:]), sa[:, 0:16].unsqueeze(2).broadcast_to([128, 16, 64]), ALU.subtract),
                         reads=[y0, sa], writes=[y0])
                    s.op("pool", lambda e: e.tensor_tensor(sq[:], y0[:], y0[:], ALU.mult), reads=[y0], writes=[sq])
                    s.op("dve", lambda e: e.tensor_reduce(sa[:, 16:32], h3(sq[:]), AX.X, ALU.add), reads=[sq], writes=[sa])
                    s.op("dve", lambda e: e.tensor_scalar(sa[:, 16:32], sa[:, 16:32], 1.0 / 64, 64e-5, ALU.mult, ALU.add), reads=[sa], writes=[sa])
                    s.op("act", lambda e: e.activation(sa[:, 16:32], sa[:, 16:32], AF.Sqrt), reads=[sa], writes=[sa])
                    s.op("dve", lambda e: e.reciprocal(sa[:, 16:32], sa[:, 16:32]), reads=[sa], writes=[sa])
                    s.op("dve", lambda e: e.tensor_tensor(h3(y0[:]), h3(y0[:]), sa[:, 16:32].unsqueeze(2).broadcast_to([128, 16, 64]), ALU.mult),
                         reads=[y0, sa], writes=[y0])
                    s.op("pool", lambda e: e.tensor_tensor(y0[:], y0[:], lnw[:], ALU.mult), reads=[y0, lnw], writes=[y0])
                    s.op("pool", lambda e: e.tensor_tensor(y0[:], y0[:], lnb[:], ALU.add), reads=[y0, lnb], writes=[y0])
                    s.op("dve", lambda e: e.tensor_tensor(h3(v[:]), h3(v[:]), rk[:, 0:16].unsqueeze(2).broadcast_to([128, 16, 64]), ALU.mult),
                         reads=[v, rk], writes=[v])
                    s.op("dve", lambda e: e.tensor_tensor(y0[:], y0[:], v[:], ALU.add), reads=[y0, v], writes=[y0])
                    for half in range(2):
                        ps = self.psum()
                        for q in range(4):
                            c = half * 4 + q
                            self.tr(ps[:, q * 128:(q + 1) * 128], y0[:, c * 128:(c + 1) * 128], self.ident, reads=[y0, self.C], writes=[ps], inc=(q == 3))
                        s.op("dve", lambda e: e.tensor_tensor(zg[:, half * 4:half * 4 + 4, sub * 128:(sub + 1) * 128],
                                                             ps[:, :].rearrange("p (q t) -> p q t", q=4),
                                                             gt[:, half * 4:half * 4 + 4, sub * 128:(sub + 1) * 128], ALU.mult),
                             reads=[ps, gt], writes=[zg])
                xa = xur.next()
                xv = fmv(self.XT)[:, :, t0:t0 + tb]
                self.load(xa[:, :, 0:tb], xv, writes=[xa])
                for fo in range(NCH):
                    ps = self.psum()
                    for fc in range(NCH):
                        self.mm(ps[:, 0:tb], wo[:, fc, fo * 128:(fo + 1) * 128], zg[:, fc, 0:tb], start=(fc == 0), stop=(fc == NCH - 1),
                                reads=[wo, zg], writes=[ps], inc=(fc == NCH - 1))
                    s.op("dve", lambda e: e.scalar_tensor_tensor(xa[:, fo, 0:tb], ps[:, 0:tb], self.mvec(l, col, 2, fo), xa[:, fo, 0:tb],
                                                                ALU.mult, ALU.add), reads=[ps, xa, self.mod], writes=[xa])
                self.store(xv, xa[:, :, 0:tb], reads=[xa])
            s.barrier()

    def phase_moe(self, l):
        cfg, nc, s = self.cfg, self.nc, self.s
        NE, NT, N, TOPK = cfg.NE, cfg.NT, cfg.N, cfg.TOPK
        T = 512
        NSLOT = (N * TOPK + NE * (T - 1)) // T
        I32 = mybir.dt.int32
        BIG = 1.0e7
        fmv = lambda a: a.rearrange("(c p) t -> p c t", p=128)
        HTOK = self.dram_scratch(f"HTOK{l}", [N, D])
        HS = self.dram_scratch("HS", [NSLOT * T, D]) if l == 0 else self.HS
        YS = self.dram_scratch("YS", [NSLOT * T, D]) if l == 0 else self.YS
        self.HS, self.YS = HS, YS
        w1rows = self.w1.rearrange("l e d f -> (l e d) f")
        w2rows = self.w2.rearrange("l e d f -> (l e d) f")
        b1rows = self.b1T.rearrange("l e p k -> (l e p) k")
        with ExitStack() as st:
            gates = self.sb(st, "gates", [128, NT, NE])
            Mall = self.sb(st, "Mall", [128, NT, NE])
            POS = self.sb(st, "POS", [128, NT, 4], I32)
            GK = self.sb(st, "GK", [128, NT, 4])
            idxw = self.sb(st, "idxw", [128, NSLOT, NCH], I32)
            idxb = self.sb(st, "idxb", [128, NSLOT], I32)
            rw = self.sb(st, "rw", [128, NCH, NE])
            rb = self.sb(st, "rb", [128, NE])
            b2 = self.sb(st, "b2", [NE, D])
            off = self.sb(st, "off", [128, NE])
            self.load(rw[:], self.router_w[l], writes=[rw])
            self.load(rb[:], self.router_b[l], writes=[rb])
            self.load(b2[:], self.b2[l], writes=[b2])
            with ExitStack() as st2:
                pools = (Rot(self, st2, "x32", [128, NCH, 512], F32, 2), Rot(self, st2, "sq", [128, NCH, 512], BF16, 2),
                         Rot(self, st2, "rs", [128, 512], F32, 2), Rot(self, st2, "nt", [128, 512], F32, 3))
                h32r = Rot(self, st2, "h32", [128, NCH, 512], F32, 2)
                hrr = Rot(self, st2, "hrow", [128, D], F32, 3)
                smr = Rot(self, st2, "sm", [128, 64], F32, 4)
                cnt = self.psum_hold()
                htok_b = [Buf(f"htok{ti}") for ti in range(NT)]
                first = True
                for (t0, tb, col) in cfg.blocks:
                    h32 = self.norm_block(pools, l, 1, t0, tb, col, h32r.next())
                    for sub in range(tb // 128):
                        ti = t0 // 128 + sub
                        ps = self.psum()
                        for c in range(NCH):
                            self.mm(ps[:, 0:NE], h32[:, c, sub * 128:(sub + 1) * 128], rw[:, c, :],
                                    start=(c == 0), stop=(c == NCH - 1), reads=[h32, rw], writes=[ps],
                                    inc=(c == NCH - 1))
                        sm = smr.next()
                        lg = sm[:, 0:NE]
                        g = gates[:, ti, :]
                        M = Mall[:, ti, :]
                        s.op("dve", lambda e: e.tensor_tensor(lg, ps[:, 0:NE], rb[:], ALU.add), reads=[ps, rb], writes=[sm])
                        s.op("dve", lambda e: e.max(sm[:, 32:40], lg), reads=[sm], writes=[sm])
                        s.op("dve", lambda e: e.tensor_scalar(sm[:, 40:41], sm[:, 32:33], -1.0, None, ALU.mult),
                             reads=[sm], writes=[sm])
                        s.op("act", lambda e: e.activation(g, lg, AF.Exp, bias=sm[:, 40:41], scale=1.0),
                             reads=[sm], writes=[gates])
                        k = TOPK - 1
                        s.op("dve", lambda e: e.tensor_scalar(M, lg, sm[:, 32 + k:33 + k], None, ALU.is_ge), reads=[sm], writes=[Mall])
                        s.op("dve", lambda e: e.tensor_tensor(g, g, M, ALU.mult), reads=[Mall, gates], writes=[gates])
                        s.op("dve", lambda e: e.tensor_reduce(sm[:, 41:42], g, AX.X, ALU.add), reads=[gates], writes=[sm])
                        s.op("dve", lambda e: e.reciprocal(sm[:, 41:42], sm[:, 41:42]), reads=[sm], writes=[sm])
                        s.op("dve", lambda e: e.tensor_scalar(g, g, sm[:, 41:42], None, ALU.mult),
                             reads=[sm, gates], writes=[gates])
                        last = (t0 + tb == N) and (sub == tb // 128 - 1)
                        self.mm(cnt[:, 0:NE], self.ones, M, start=first, stop=last, reads=[self.C, Mall], writes=[cnt], inc=True)
                        first = False
                        hrow = hrr.next()
                        for half in range(2):
                            ps2 = self.psum()
                            for q in range(4):
                                c = half * 4 + q
                                self.tr(ps2[:, q * 128:(q + 1) * 128], h32[:, c, sub * 128:(sub + 1) * 128], self.ident,
                                        reads=[h32, self.C], writes=[ps2], inc=(q == 3))
                            s.op("act", lambda e: e.copy(hrow[:, half * 512:(half + 1) * 512], ps2[:, :]), reads=[ps2], writes=[hrow])
                        self.store(HTOK[ti * 128:(ti + 1) * 128, :], hrow[:], reads=[hrow], writes=[htok_b[ti]])
                nf = self.sb(st2, "nf", [128, NE])
                ni = self.sb(st2, "ni", [128, NE], I32)
                npf = self.sb(st2, "npf", [128, NE])
                ends = self.sb(st2, "ends", [128, NE])
                cmp_ = self.sb(st2, "cmp", [128, NSLOT, NE])
                ejf = self.sb(st2, "ejf", [128, NSLOT])
                ixf = self.sb(st2, "ixf", [128, NSLOT, NCH])
                s.op("dve", lambda e: e.tensor_scalar(nf[:], cnt[:, 0:NE], float(T - 1), None, ALU.add), reads=[cnt], writes=[nf])
                self.psum_release(cnt)
                s.op("dve", lambda e: e.tensor_copy(ni[:], nf[:]), reads=[nf], writes=[ni])
                s.op("dve", lambda e: e.tensor_scalar(ni[:], ni[:], 9, 9, ALU.arith_shift_right, ALU.logical_shift_left), reads=[ni], writes=[ni])
                s.op("dve", lambda e: e.tensor_copy(npf[:], ni[:]), reads=[ni], writes=[npf])
                s.op("dve", lambda e: e.tensor_tensor_scan(ends[:], self.ones[:, 0:NE], npf[:], 0.0, ALU.mult, ALU.add),
                     reads=[self.C, npf], writes=[ends])
                s.op("dve", lambda e: e.tensor_tensor(off[:], ends[:], npf[:], ALU.subtract), reads=[ends, npf], writes=[off])
                starts = self.cstM[:, 0:NSLOT]
                s.op("dve", lambda e: e.tensor_tensor(cmp_[:], ends[:].unsqueeze(1).broadcast_to([128, NSLOT, NE]),
                                                     starts.unsqueeze(2).broadcast_to([128, NSLOT, NE]), ALU.is_le),
                     reads=[ends, self.cstMb], writes=[cmp_])
                s.op("dve", lambda e: e.tensor_reduce(ejf[:], cmp_[:], AX.X, ALU.add), reads=[cmp_], writes=[ejf])
                skp = self.sb(st2, "skp", [128, NSLOT])
                s.op("dve", lambda e: e.tensor_scalar(skp[:], ejf[:], float(NE) - 0.5, 4.0e6, ALU.is_ge, ALU.mult), reads=[ejf], writes=[skp])
                s.op("dve", lambda e: e.tensor_scalar(ejf[:], ejf[:], float(NE - 1), float(l * NE), ALU.min, ALU.add), reads=[ejf], writes=[ejf])
                pb = self.cstM[:, 128:136]
                s.op("dve", lambda e: e.tensor_scalar(ixf[:, :, 0], ejf[:], 128.0, self.cstM[:, 128:129], ALU.mult, ALU.add),
                     reads=[ejf, self.cstMb], writes=[ixf])
                s.op("dve", lambda e: e.tensor_copy(idxb[:], ixf[:, :, 0]), reads=[ixf], writes=[idxb])
                s.op("dve", lambda e: e.tensor_scalar(ejf[:], ejf[:], float(D), None, ALU.mult), reads=[ejf], writes=[ejf])
                if l > 0:
                    s.op("dve", lambda e: e.tensor_tensor(ejf[:], ejf[:], skp[:], ALU.add), reads=[ejf, skp], writes=[ejf])
                s.op("dve", lambda e: e.tensor_tensor(ixf[:], ejf[:].unsqueeze(2).broadcast_to([128, NSLOT, NCH]),
                                                     pb.unsqueeze(1).broadcast_to([128, NSLOT, NCH]), ALU.add),
                     reads=[ejf, self.cstMb], writes=[ixf])
                s.op("dve", lambda e: e.tensor_copy(idxw[:], ixf[:]), reads=[ixf], writes=[idxw])
                tpr = Rot(self, st2, "tp", [128, 4 * NE], F32, 2)
                pfr = Rot(self, st2, "pf", [128, 16], F32, 2)
                SUf = self.cstM[:, 256:384]
                for ti in range(NT):
                    M = Mall[:, ti, :]
                    ps = self.psum()
                    self.mm(ps[:, 0:NE], SUf, M, start=True, stop=True, reads=[self.cstMb, Mall], writes=[ps], inc=False)
                    self.mm(ps[:, 64:64 + NE], self.ones, M, start=True, stop=True, reads=[self.C, Mall], writes=[ps], inc=True)
                    tp, pf = tpr.next(), pfr.next()
                    posf, t1, npm, tq = tp[:, 0:NE], tp[:, NE:2 * NE], tp[:, 2 * NE:3 * NE], tp[:, 3 * NE:4 * NE]
                    s.op("dve", lambda e: e.tensor_tensor(posf, ps[:, 0:NE], off[:], ALU.add), reads=[ps, off], writes=[tp])
                    s.op("dve", lambda e: e.tensor_tensor(off[:], ps[:, 64:64 + NE], off[:], ALU.add), reads=[ps, off], writes=[off])
                    s.op("dve", lambda e: e.tensor_tensor(t1, posf, M, ALU.mult), reads=[tp, Mall], writes=[tp])
                    s.op("dve", lambda e: e.tensor_scalar(npm, M, -1.0, BIG, ALU.add, ALU.mult), reads=[Mall], writes=[tp])
                    s.op("dve", lambda e: e.tensor_tensor(npm, npm, t1, ALU.subtract), reads=[tp], writes=[tp])
                    s.op("dve", lambda e: e.max(pf[:, 0:8], npm), reads=[tp], writes=[pf])
                    s.op("dve", lambda e: e.tensor_scalar(pf[:, 8:12], pf[:, 0:4], -1.0, None, ALU.mult), reads=[pf], writes=[pf])
                    s.op("dve", lambda e: e.tensor_copy(POS[:, ti, :], pf[:, 8:12]), reads=[pf], writes=[POS])
                    for k in range(TOPK):
                        s.op("dve", lambda e: e.scalar_tensor_tensor(tq, npm, pf[:, k:k + 1], gates[:, ti, :], ALU.is_equal, ALU.mult),
                             reads=[tp, pf, gates], writes=[tp])
                        s.op("dve", lambda e: e.tensor_reduce(GK[:, ti, k:k + 1], tq, AX.X, ALU.add), reads=[tp], writes=[GK])
                    hrow = hrr.next()
                    self.load(hrow[:], HTOK[ti * 128:(ti + 1) * 128, :], reads=[htok_b[ti]], writes=[hrow])
                    for k in range(TOPK):
                        s.idma(HS[:, :], bass.IndirectOffsetOnAxis(ap=POS[:, ti, k:k + 1], axis=0), hrow[:], None,
                               reads=[hrow, POS], nrows=NSLOT * T)
                s.barrier()
            with ExitStack() as st2:
                w1b = [[self.sb(st2, f"w1b{b}_{dc}", [128, 2 * D], BF16) for dc in range(NCH)] for b in range(2)]
                w2b = [[self.sb(st2, f"w2b{b}_{fc}", [128, D], BF16) for fc in range(NCH)] for b in range(2)]
                b1s = [self.sb(st2, f"b1s{b}", [128, 16]) for b in range(2)]
                Xr = Rot(self, st2, "Xs", [128, 4, D], F32, 2)
                hTr = Rot(self, st2, "hTs", [128, NCH, 512], BF16, 1)
                ysr = Rot(self, st2, "ys", [128, 4, D], F32, 1)
                ur = Rot(self, st2, "u", [128, 512], F32, 2)
                sgr = Rot(self, st2, "sg", [128, 512], F32, 2)
                lnr = Rot(self, st2, "ln", [128, 512], F32, 2)
                actr = Rot(self, st2, "actT", [128, NCH, 512], BF16, 1)
                for j in range(NSLOT):
                    bsel = j % 2
                    s.idma(b1s[bsel][:], None, b1rows[:, :], bass.IndirectOffsetOnAxis(ap=idxb[:, j:j + 1], axis=0),
                           reads=[idxb], writes=[b1s[bsel]], nrows=cfg.DEPTH * NE * 128)
                    for dc in range(NCH):
                        s.idma(w1b[bsel][dc][:], None, w1rows[:, :], bass.IndirectOffsetOnAxis(ap=idxw[:, j, dc:dc + 1], axis=0),
                               reads=[idxw], writes=[w1b[bsel][dc]], nrows=cfg.DEPTH * NE * D)
                    for fc in range(NCH):
                        s.idma(w2b[bsel][fc][:], None, w2rows[:, :], bass.IndirectOffsetOnAxis(ap=idxw[:, j, fc:fc + 1], axis=0),
                               reads=[idxw], writes=[w2b[bsel][fc]], nrows=cfg.DEPTH * NE * D)
                    X = Xr.next()
                    self.load(X[:], HS[j * T:(j + 1) * T, :].rearrange("(a p) f -> p a f", p=128), writes=[X])
                    hT = hTr.next()
                    for c in range(NCH):
                        ps = self.psum()
                        for sub in range(4):
                            self.tr(ps[:, sub * 128:(sub + 1) * 128], X[:, sub, c * 128:(c + 1) * 128], self.ident,
                                    reads=[X, self.C], writes=[ps], inc=(sub == 3))
                        s.op("act", lambda e: e.copy(hT[:, c, :], ps[:, :]), reads=[ps], writes=[hT])
                    actT = actr.next()
                    tb = T
                    for i in range(NCH):
                        psg, psl = self.psum(), self.psum()
                        for (pp, joff) in ((psg, 0), (psl, D)):
                            for dc in range(NCH):
                                self.mm(pp[:, 0:tb], w1b[bsel][dc][:, joff + i * 128: joff + (i + 1) * 128],
                                        hT[:, dc, 0:tb], start=(dc == 0), stop=(dc == NCH - 1),
                                        reads=[w1b[bsel][dc], hT], writes=[pp], inc=(dc == NCH - 1))
                        u, sg, ln = ur.next(), sgr.next(), lnr.next()
                        s.op("dve", lambda e: e.tensor_scalar(u[:, 0:tb], psg[:, 0:tb], b1s[bsel][:, i:i + 1], 7.0, ALU.add, ALU.min),
                             reads=[psg, b1s[bsel]], writes=[u])
                        s.op("act", lambda e: e.activation(sg[:, 0:tb], u[:, 0:tb], AF.Sigmoid, scale=1.702), reads=[u], writes=[sg])
                        s.op("dve", lambda e: e.tensor_scalar(ln[:, 0:tb], psl[:, 0:tb], b1s[bsel][:, 8 + i:9 + i], 7.0, ALU.add, ALU.min),
                             reads=[psl, b1s[bsel]], writes=[ln])
                        s.op("dve", lambda e: e.tensor_scalar(ln[:, 0:tb], ln[:, 0:tb], -7.0, 1.0, ALU.max, ALU.add), reads=[ln], writes=[ln])
                        s.op("dve", lambda e: e.tensor_tensor(u[:, 0:tb], u[:, 0:tb], sg[:, 0:tb], ALU.mult), reads=[u, sg], writes=[u])
                        s.op("dve", lambda e: e.tensor_tensor(actT[:, i, 0:tb], u[:, 0:tb], ln[:, 0:tb], ALU.mult), reads=[u, ln], writes=[actT])
                    ys = ysr.next()
                    for sub in range(4):
                        for hf in range(2):
                            ps = self.psum()
                            for fc in range(NCH):
                                self.mm(ps[:, :], actT[:, fc, sub * 128:(sub + 1) * 128], w2b[bsel][fc][:, hf * 512:(hf + 1) * 512],
                                        start=(fc == 0), stop=(fc == NCH - 1), reads=[actT, w2b[bsel][fc]], writes=[ps],
                                        inc=(fc == NCH - 1))
                            s.op("act", lambda e: e.copy(ys[:, sub, hf * 512:(hf + 1) * 512], ps[:, :]), reads=[ps], writes=[ys])
                    self.store(YS[j * T:(j + 1) * T, :].rearrange("(a p) f -> p a f", p=128), ys[:], reads=[ys])
                s.barrier()
            with ExitStack() as st2:
                accr = Rot(self, st2, "acc", [128, D], F32, 2)
                rwr = Rot(self, st2, "yrow", [128, D], F32, 4)
                gT = self.sb(st2, "gT", [NE, 128])
                xur = Rot(self, st2, "xu", [128, NCH, 128], F32, 2)
                for ti in range(NT):
                    acc = accr.next()
                    ps = self.psum()
                    self.tr(ps[0:NE, 0:128], gates[:, ti, :], self.ident, reads=[gates, self.C], writes=[ps], inc=True)
                    s.op("dve", lambda e: e.tensor_copy(gT[:], ps[0:NE, 0:128]), reads=[ps], writes=[gT])
                    for hf in range(2):
                        ps2 = self.psum()
                        self.mm(ps2[:, :], gT[:], b2[:, hf * 512:(hf + 1) * 512], start=True, stop=True,
                                reads=[gT, b2], writes=[ps2], inc=True)
                        s.op("act", lambda e: e.copy(acc[:, hf * 512:(hf + 1) * 512], ps2[:, :]), reads=[ps2], writes=[acc])
                    for k in range(TOPK):
                        yr_ = rwr.next()
                        s.idma(yr_[:], None, YS[:, :], bass.IndirectOffsetOnAxis(ap=POS[:, ti, k:k + 1], axis=0),
                               reads=[POS], writes=[yr_], nrows=NSLOT * T)
                        s.op("dve", lambda e: e.scalar_tensor_tensor(acc[:], yr_[:], GK[:, ti, k:k + 1], acc[:], ALU.mult, ALU.add),
                             reads=[yr_, GK, acc], writes=[acc])
                    col = 1 if ti * 128 < cfg.CTX else 0
                    xu = xur.next()
                    xv = fmv(self.XT)[:, :, ti * 128:(ti + 1) * 128]
                    self.load(xu[:], xv, writes=[xu])
                    for half in range(2):
                        ps = self.psum()
                        for q in range(4):
                            c = half * 4 + q
                            self.tr(ps[:, q * 128:(q + 1) * 128], acc[:, c * 128:(c + 1) * 128], self.ident,
                                    reads=[acc, self.C], writes=[ps], inc=(q == 3))
                        for q in range(4):
                            c = half * 4 + q
                            s.op("dve", lambda e: e.scalar_tensor_tensor(xu[:, c, :], ps[:, q * 128:(q + 1) * 128], self.mvec(l, col, 5, c),
                                                                        xu[:, c, :], ALU.mult, ALU.add), reads=[ps, xu, self.mod], writes=[xu])
                    self.store(xv, xu[:], reads=[xu])
                s.barrier()


def make_consts():
    c = np.zeros((128, 1024), np.float32)
    c[:, 0:128] = np.eye(128, dtype=np.float32)
    c[:, 128:256] = 1.0
    c[0:64, 256:320] = 1.0
    c[64:128, 320:384] = 1.0
    for m in range(64):
        c[(m + 16) if (m % 32) < 16 else (m - 16), 384 + m] = 1.0
    for m in range(128):
        c[(m + 32) if (m % 64) < 32 else (m - 32), 512 + m] = 1.0
    return c


def make_consts_r():
    c = np.zeros((128, 2176), np.float32)
    i = np.arange(128)
    SU = (i[:, None] < i[None, :]).astype(np.float32)
    UI = (i[:, None] <= i[None, :]).astype(np.float32)
    SL = (i[:, None] > i[None, :]).astype(np.float32)
    LI = (i[:, None] >= i[None, :]).astype(np.float32)
    c[:, 0:512] = np.concatenate([SU, UI, SU, UI], axis=1)
    c[:, 512:1024] = np.concatenate([SL, LI, SL, LI], axis=1)
    c[:, 1024:1536] = np.concatenate([SL] * 4, axis=1)
    c[:, 1536:2048] = np.concatenate([SU] * 4, axis=1)
    hs = np.zeros((128, NCH, 16), np.float32)
    for p in range(128):
        for fc in range(NCH):
            hs[p, fc, fc * 2 + p // 64] = 1.0
    c[:, 2048:2176] = hs.reshape(128, 128)
    return c


def rope_table(cfg, rot_dim):
    n, gw = cfg.SEQ, cfg.GRID_W
    rows = n // gw
    row = np.repeat(np.arange(rows, dtype=np.float32), gw)
    colp = np.tile(np.arange(gw, dtype=np.float32), rows)
    half = rot_dim // 2
    inv = (np.float32(10000.0) ** (-np.arange(0, half, 2, dtype=np.float32) / np.float32(half))).astype(np.float32)
    ar = (row[:, None] * inv[None, :]).astype(np.float32)
    ac = (colp[:, None] * inv[None, :]).astype(np.float32)
    q = rot_dim // 4
    t = np.zeros((rot_dim, 2, cfg.N), np.float32)
    t[:, 0, :cfg.CTX] = 1.0
    for blk, ang in ((0, ar), (1, ac)):
        cs, sn = np.cos(ang).T.astype(np.float32), np.sin(ang).T.astype(np.float32)
        b0 = blk * half
        t[b0:b0 + q, 0, cfg.CTX:] = cs
        t[b0 + q:b0 + 2 * q, 0, cfg.CTX:] = cs
        t[b0:b0 + q, 1, cfg.CTX:] = -sn
        t[b0 + q:b0 + 2 * q, 1, cfg.CTX:] = sn
    return t


def fm(v):
    v = np.asarray(v, np.float32)
    return np.ascontiguousarray(np.swapaxes(v.reshape(v.shape[:-1] + (NCH, 128)), -1, -2))


def prep_shared(cfg, inp):
    L, NE = cfg.DEPTH, cfg.NE
    sh = {}
    sh["ada_w"] = np.ascontiguousarray(inp["ada_w"], np.float32)
    sh["ada_bT"] = np.ascontiguousarray(np.swapaxes(np.asarray(inp["ada_b"], np.float32).reshape(L, 48, 128), 1, 2))
    sh["normT"] = np.ascontiguousarray(np.stack([fm(inp["norm_mix"]), fm(inp["norm_ffn"])], axis=2))
    sh["cst"] = make_consts()
    rw = np.asarray(inp["moe_router_w"], np.float32).reshape(L, NCH, 128, NE)
    sh["router_w"] = np.ascontiguousarray(rw.transpose(0, 2, 1, 3))
    sh["router_b"] = np.ascontiguousarray(np.broadcast_to(np.asarray(inp["moe_router_b"], np.float32)[:, None, :], (L, 128, NE)))
    w1 = np.asarray(inp["moe_w1"], np.float32)
    sh["moe_w1"] = np.ascontiguousarray(np.concatenate([w1[..., 0::2], w1[..., 1::2]], axis=-1))
    b1 = np.asarray(inp["moe_b1"], np.float32)
    b1d = np.concatenate([b1[..., 0::2], b1[..., 1::2]], axis=-1).reshape(L, NE, 16, 128)
    sh["moe_b1T"] = np.ascontiguousarray(np.swapaxes(b1d, -1, -2))
    sh["moe_w2"] = np.ascontiguousarray(inp["moe_w2"], np.float32)
    sh["moe_b2"] = np.ascontiguousarray(inp["moe_b2"], np.float32)
    NEV = max(cfg.N_EVEN, 1)
    for k in ("attn_w_in", "mla_w_uq", "mla_w_ukv", "attn_w_out"):
        sh[k] = np.ascontiguousarray(inp[k], np.float32)
    ag = np.zeros((NEV, 128, 16), np.float32)
    f32 = lambda k: np.asarray(inp[k], np.float32)
    ag[:, :, 0:3] = np.swapaxes(f32("mla_q_norm").reshape(NEV, 3, 128), 1, 2)
    ag[:, :, 3:5] = np.swapaxes(f32("mla_kv_norm").reshape(NEV, 2, 128), 1, 2)
    ag[:, :, 5] = f32("mla_qn_g")
    ag[:, 0:64, 6] = f32("mla_qr_g")
    ag[:, :, 7] = f32("mla_kn_g")
    ag[:, 0:64, 8] = f32("mla_kr_g")
    ag[:, :, 9] = f32("gqa_q_g")
    ag[:, :, 10] = f32("gqa_k_g")
    sh["attn_g"] = ag
    sh["ropeM"] = rope_table(cfg, 64)
    sh["ropeG"] = rope_table(cfg, 128)
    NOD, NVR = max(cfg.N_ODD, 1), max(cfg.N_ODD - 1, 1)

    def pad0(a, n):
        a = np.asarray(a, np.float32)
        if a.shape[0] >= n:
            return np.ascontiguousarray(a)
        return np.ascontiguousarray(np.concatenate([a, np.zeros((n - a.shape[0],) + a.shape[1:], np.float32)], axis=0))
    for dst, src in (("rw_wr", "rwkv_w_r"), ("rw_wk", "rwkv_w_k"), ("rw_wv", "rwkv_w_v"), ("rw_wo", "rwkv_w_o"),
                     ("rw_w1", "rwkv_w1"), ("rw_w2", "rwkv_w2"), ("rw_a1", "rwkv_a1"), ("rw_a2", "rwkv_a2"),
                     ("rw_g1", "rwkv_g1"), ("rw_g2", "rwkv_g2")):
        sh[dst] = pad0(inp[src], NOD)
    sh["rw_v1"] = pad0(inp["rwkv_v1"], NVR)
    sh["rw_v2"] = pad0(inp["rwkv_v2"], NVR)
    J = cfg.N_ODD
    rp = np.zeros((NOD, 128, 16, NCH), np.float32)
    bc = np.zeros((NOD, 3, 128, D), np.float32)
    if J > 0:
        rp[:J, :, 0:6, :] = np.swapaxes(fm(inp["rwkv_mix"]), 1, 2)
        rp[:J, :, 6:8, :] = np.swapaxes(fm(inp["rwkv_w0"]), 1, 2)
        rp[:J, :, 8:10, :] = np.swapaxes(fm(inp["rwkv_a0"]), 1, 2)
        rp[:J, :, 10, :] = fm(inp["rwkv_k_k"])
        rp[:J, :, 11, :] = fm(inp["rwkv_k_a"])
        rp[:J, :, 13, :] = fm(np.asarray(inp["rwkv_r_k"], np.float32).reshape(J, D))
        bc[:J, 0] = np.asarray(inp["rwkv_ln_w"], np.float32)[:, None, :]
        bc[:J, 1] = np.asarray(inp["rwkv_ln_b"], np.float32)[:, None, :]
        for jj in range(1, J):
            bc[jj, 2] = np.asarray(inp["rwkv_v0"], np.float32)[jj - 1][None, :]
    sh["rw_p"] = rp
    sh["rw_bc"] = bc
    sh["cstR"] = make_consts_r()
    cm = np.zeros((128, 384), np.float32)
    cm[:, 0:128] = (np.arange(128, dtype=np.float32) * 512.0)[None, :]
    cm[:, 128:136] = np.arange(128, dtype=np.float32)[:, None] + 128.0 * np.arange(8, dtype=np.float32)[None, :]
    ii = np.arange(128)
    cm[:, 256:384] = (ii[:, None] < ii[None, :]).astype(np.float32)
    sh["cstM"] = cm
    return sh


def prep_core(cfg, inp, b):
    pc = {}
    xT = np.concatenate([np.asarray(inp["ctx"][b], np.float32).T, np.asarray(inp["x"][b], np.float32).T], axis=1)
    pc["xT0"] = np.ascontiguousarray(xT)
    cond = np.stack([fm(inp["c"][b]), fm(inp["c_ctx"])], axis=1)
    pc["condT"] = np.ascontiguousarray(cond)
    return pc


def run(cfg, inp, n_cores, debug=()):
    P = Prog(cfg, debug=debug)
    nc = P.build()
    sh = prep_shared(cfg, inp)
    in_maps = []
    for b in range(n_cores):
        m = dict(sh)
        m.update(prep_core(cfg, inp, b))
        for k, (shape, dt) in P.inputs.items():
            assert tuple(m[k].shape) == shape, (k, m[k].shape, shape)
        in_maps.append({k: m[k] for k in P.inputs})
    res = run_bass_kernel_spmd(nc, in_maps, core_ids=list(range(n_cores)))
    return res, P


def kernel(**inputs):
    cfg = Cfg()
    res, P = run(cfg, inputs, 8)
    out = np.stack([np.ascontiguousarray(r["outT"].T) for r in res.results], axis=0)
    return out.astype(np.float32)
```
